# Optimizing a Trainium2 kernel written in Bass

```python
import jax, jax.numpy as jnp
from jax import lax
import numpy as np

D_MODEL = 1024
BATCH = 1
SEQ = 16384
DEPTH = 4

GRID_W = 64
CTX_LEN = 256
NA_HEADS = 8
NA_HEAD_DIM = 64
NA_KH = 8
NA_KW = 16
NA_W = NA_HEADS * NA_HEAD_DIM
MLA_HEADS = 8
MLA_NOPE = 64
MLA_ROPE = 32
MLA_V = 64
MLA_Q_LORA = 256
MLA_KV_LORA = 128
MLA_OUT_W = MLA_HEADS * MLA_V
ROPE_THETA = 10000.0
Q_BLOCK = 128
MIX_W = NA_W + MLA_OUT_W
IN_COLS = 3 * NA_W + MLA_Q_LORA + MLA_KV_LORA + MLA_ROPE
SPLITS = [NA_W, 2 * NA_W, 3 * NA_W, 3 * NA_W + MLA_Q_LORA, 3 * NA_W + MLA_Q_LORA + MLA_KV_LORA]
POOL_WINDOWS = (2, 4, 8, 16)
POOL_GROUPS = len(POOL_WINDOWS)
POOL_GROUP = D_MODEL // POOL_GROUPS
FFN_DIM = 2816
N_EXPERTS = 8
TOP_K = 2
EXPERT_DIM = 3584
N_ATTN_LAYERS = (DEPTH + 1) // 2
N_POOL_LAYERS = DEPTH // 2
DEEPNORM_ALPHA = (2 * DEPTH) ** 0.25
DEEPNORM_BETA = (8 * DEPTH) ** -0.25
LN_EPS = 1e-5
RMS_EPS = 1e-6

kernel_name = 'hybrid_na_mla_pool_moe_trunk'


def layer_norm(x, g, b):
    xf = x.astype(jnp.float32)
    mu = jnp.mean(xf, -1, keepdims=True)
    var = jnp.mean(jnp.square(xf - mu), -1, keepdims=True)
    return ((xf - mu) * lax.rsqrt(var + LN_EPS)).astype(x.dtype) * g + b


def rms_norm(x, g):
    xf = x.astype(jnp.float32)
    return (xf * lax.rsqrt(jnp.mean(xf * xf, -1, keepdims=True) + RMS_EPS)).astype(x.dtype) * g


def modulate(h, shift, scale):
    return h * (1 + scale) + shift


def softmax_f32(s, like):
    return jax.nn.softmax(s.astype(jnp.float32), axis=-1).astype(like.dtype)


def _rotate(x, ang):
    half = x.shape[-1] // 2
    x1, x2 = x[..., :half], x[..., half:]
    cos = jnp.cos(ang).astype(x.dtype)
    sin = jnp.sin(ang).astype(x.dtype)
    return jnp.concatenate([x1 * cos - x2 * sin, x1 * sin + x2 * cos], -1)


def axial_rope(x, ang_row, ang_col):
    if x.ndim == 4:
        ang_row, ang_col = ang_row[:, None], ang_col[:, None]
    half = x.shape[-1] // 2
    return jnp.concatenate([_rotate(x[..., :half], ang_row), _rotate(x[..., half:], ang_col)], -1)


def grid_angles(n):
    t = jnp.arange(n, dtype=jnp.int32)
    row = (t // GRID_W).astype(jnp.float32)
    col = (t % GRID_W).astype(jnp.float32)
    n_freq = MLA_ROPE // 4
    inv = 1.0 / (ROPE_THETA ** (jnp.arange(n_freq, dtype=jnp.float32) / n_freq))
    return row[:, None] * inv, col[:, None] * inv


def neighbourhood_attention(q, k, v, kc, vc, rel_bias):
    b, s, h, dh = q.shape
    rows = s // GRID_W
    kh = min(NA_KH, rows)
    n_loc = kh * NA_KW
    scale = dh ** -0.5
    cols = jnp.arange(GRID_W)
    col_start = jnp.clip(cols - NA_KW // 2, 0, GRID_W - NA_KW)
    key_cols = col_start[:, None] + jnp.arange(NA_KW)
    dcol = key_cols - cols[:, None] + NA_KW - 1

    def row_fn(r):
        row_start = jnp.clip(r - kh // 2, 0, rows - kh)
        key_rows = row_start + jnp.arange(kh)
        idx = (key_rows[None, :, None] * GRID_W + key_cols[:, None, :]).reshape(GRID_W, n_loc)
        drow = key_rows - r + NA_KH - 1
        bias = rel_bias[:, drow[None, :, None], dcol[:, None, :]].reshape(h, GRID_W, n_loc)
        q_r = lax.dynamic_slice_in_dim(q, r * GRID_W, GRID_W, axis=1)
        k_g = jnp.take(k, idx, axis=1)
        v_g = jnp.take(v, idx, axis=1)
        s_loc = jnp.einsum('bqhd,bqkhd->bhqk', q_r, k_g) * scale + bias
        s_ctx = jnp.einsum('bqhd,bkhd->bhqk', q_r, kc) * scale
        p = softmax_f32(jnp.concatenate([s_loc, s_ctx], -1), v)
        return (jnp.einsum('bhqk,bqkhd->bqhd', p[..., :n_loc], v_g)
                + jnp.einsum('bhqk,bkhd->bqhd', p[..., n_loc:], vc))

    out = lax.map(row_fn, jnp.arange(rows))
    return jnp.moveaxis(out, 0, 1).reshape(b, s, h, dh)


def dense_attention(q, k, v, scale):
    s = jnp.einsum('bqhd,bkhd->bhqk', q, k) * scale
    return jnp.einsum('bhqk,bkhd->bqhd', softmax_f32(s, v), v)


def mla_attention(qn, qr, kn, kr, v, scale):
    b, n, h, _ = qn.shape
    nb = n // Q_BLOCK

    def blk(args):
        qn_b, qr_b = args
        s = (jnp.einsum('bqhd,bkhd->bhqk', qn_b, kn) + jnp.einsum('bqhr,bkr->bhqk', qr_b, kr)) * scale
        return jnp.einsum('bhqk,bkhd->bqhd', softmax_f32(s, v), v)

    qn_blocks = qn.reshape(b, nb, Q_BLOCK, h, qn.shape[-1]).swapaxes(0, 1)
    qr_blocks = qr.reshape(b, nb, Q_BLOCK, h, qr.shape[-1]).swapaxes(0, 1)
    out = lax.map(blk, (qn_blocks, qr_blocks))
    return out.swapaxes(0, 1).reshape(b, n, h, v.shape[-1])


def _project(hh, w_in, q_norm, w_q_up, kv_norm, w_kv_up):
    bsz, n, _ = hh.shape
    qa, ka, va, q_c, kv_c, k_rope = jnp.split(hh @ w_in, SPLITS, axis=-1)
    heads = lambda t: t.reshape(bsz, n, NA_HEADS, NA_HEAD_DIM)
    q_mla = (rms_norm(q_c, q_norm) @ w_q_up).reshape(bsz, n, MLA_HEADS, MLA_NOPE + MLA_ROPE)
    kv_mla = (rms_norm(kv_c, kv_norm) @ w_kv_up).reshape(bsz, n, MLA_HEADS, MLA_NOPE + MLA_V)
    return (heads(qa), heads(ka), heads(va), q_mla[..., :MLA_NOPE], q_mla[..., MLA_NOPE:],
            kv_mla[..., :MLA_NOPE], k_rope, kv_mla[..., MLA_NOPE:])


def attention_mixer(h_lat, h_ctx, w_in, rel_bias, q_norm, w_q_up, kv_norm, w_kv_up, w_out, with_ctx_out):
    b, s, _ = h_lat.shape
    qa, ka, va, qn, qr, kn, kr, vb = _project(h_lat, w_in, q_norm, w_q_up, kv_norm, w_kv_up)
    qa_c, ka_c, va_c, qn_c, qr_c, kn_c, kr_c, vb_c = _project(h_ctx, w_in, q_norm, w_q_up, kv_norm, w_kv_up)
    ang_r, ang_c = grid_angles(s)
    qr = axial_rope(qr, ang_r, ang_c)
    kr = axial_rope(kr, ang_r, ang_c)
    mla_scale = (MLA_NOPE + MLA_ROPE) ** -0.5
    o_a = neighbourhood_attention(qa, ka, va, ka_c, va_c, rel_bias)
    o_b = mla_attention(qn, qr, jnp.concatenate([kn_c, kn], 1), jnp.concatenate([kr_c, kr], 1),
                        jnp.concatenate([vb_c, vb], 1), mla_scale)
    y_lat = jnp.concatenate([o_a.reshape(b, s, NA_W), o_b.reshape(b, s, MLA_OUT_W)], -1) @ w_out
    if not with_ctx_out:
        return y_lat, None
    n_ctx = h_ctx.shape[1]
    o_a_c = dense_attention(qa_c, ka_c, va_c, NA_HEAD_DIM ** -0.5)
    o_b_c = mla_attention(qn_c, qr_c, kn_c, kr_c, vb_c, mla_scale)
    y_ctx = jnp.concatenate([o_a_c.reshape(b, n_ctx, NA_W), o_b_c.reshape(b, n_ctx, MLA_OUT_W)], -1) @ w_out
    return y_lat, y_ctx


def pool_mixer(h, w, scale):
    b, n, d = h.shape
    t = jnp.arange(n)
    half = jnp.array(POOL_WINDOWS, dtype=jnp.int32) // 2
    lo = jnp.clip(t[:, None] - half, 0, n)
    hi = jnp.clip(t[:, None] + half, 0, n)
    cnt = (hi - lo).astype(jnp.float32)
    hf = h.astype(jnp.float32).reshape(b, n, POOL_GROUPS, POOL_GROUP)
    cs = jnp.concatenate([jnp.zeros((b, 1, POOL_GROUPS, POOL_GROUP), jnp.float32), jnp.cumsum(hf, axis=1)], 1)
    grp = jnp.arange(POOL_GROUPS)
    win_sum = cs[:, hi, grp] - cs[:, lo, grp]
    mixed = (win_sum / cnt[..., None] - hf).astype(h.dtype)
    return jnp.einsum('bngc,gcd->bngd', mixed, w).reshape(b, n, d) * scale


def swiglu(h, wg, wu, wd):
    return (jax.nn.silu(h @ wg) * (h @ wu)) @ wd


def moe_swiglu(h, router, wg, wu, wd):
    logits = (h @ router).astype(jnp.float32)
    top_val, top_idx = lax.top_k(logits, TOP_K)
    top_w = jax.nn.softmax(top_val, axis=-1)
    gates = jnp.sum(jax.nn.one_hot(top_idx, N_EXPERTS, dtype=jnp.float32) * top_w[..., None], axis=-2).astype(h.dtype)
    out = jnp.zeros_like(h)
    for e in range(N_EXPERTS):
        out = out + gates[..., e:e + 1] * swiglu(h, wg[e], wu[e], wd[e])
    return out


def setup_inputs(seed: int = 0) -> dict:
    key = jax.random.key(seed)
    ks = iter(jax.random.split(key, 40))

    def nrm(shape, scale):
        return jax.random.normal(next(ks), shape, jnp.float32) * scale

    D = D_MODEL
    beta = DEEPNORM_BETA
    return {
        'x': nrm((BATCH, SEQ, D), 1.0),
        'c': nrm((BATCH, D), 1.0),
        'ctx': nrm((BATCH, CTX_LEN, D), 1.0),
        'c_ctx': nrm((D,), 1.0),
        'mod_w': nrm((DEPTH, D, 6 * D), D ** -0.5),
        'mod_b': nrm((DEPTH, 6 * D), 0.02),
        'ln1_g': 1.0 + nrm((DEPTH, D), 0.02),
        'ln1_b': nrm((DEPTH, D), 0.02),
        'ln2_g': 1.0 + nrm((DEPTH, D), 0.02),
        'ln2_b': nrm((DEPTH, D), 0.02),
        'attn_w_in': nrm((N_ATTN_LAYERS, D, IN_COLS), D ** -0.5),
        'na_rel_bias': nrm((N_ATTN_LAYERS, NA_HEADS, 2 * NA_KH - 1, 2 * NA_KW - 1), 0.5),
        'mla_q_norm': 1.0 + nrm((N_ATTN_LAYERS, MLA_Q_LORA), 0.02),
        'mla_w_q_up': nrm((N_ATTN_LAYERS, MLA_Q_LORA, MLA_HEADS * (MLA_NOPE + MLA_ROPE)), MLA_Q_LORA ** -0.5),
        'mla_kv_norm': 1.0 + nrm((N_ATTN_LAYERS, MLA_KV_LORA), 0.02),
        'mla_w_kv_up': nrm((N_ATTN_LAYERS, MLA_KV_LORA, MLA_HEADS * (MLA_NOPE + MLA_V)), MLA_KV_LORA ** -0.5),
        'attn_w_out': nrm((N_ATTN_LAYERS, MIX_W, D), MIX_W ** -0.5 * beta),
        'ffn_w_gate': nrm((N_ATTN_LAYERS, D, FFN_DIM), D ** -0.5),
        'ffn_w_up': nrm((N_ATTN_LAYERS, D, FFN_DIM), D ** -0.5),
        'ffn_w_down': nrm((N_ATTN_LAYERS, FFN_DIM, D), FFN_DIM ** -0.5 * beta),
        'pool_w': nrm((N_POOL_LAYERS, POOL_GROUPS, POOL_GROUP, POOL_GROUP), POOL_GROUP ** -0.5 * beta),
        'pool_scale': 1.0 + nrm((N_POOL_LAYERS, D), 0.02),
        'moe_router': nrm((N_POOL_LAYERS, D, N_EXPERTS), D ** -0.5),
        'moe_w_gate': nrm((N_POOL_LAYERS, N_EXPERTS, D, EXPERT_DIM), D ** -0.5),
        'moe_w_up': nrm((N_POOL_LAYERS, N_EXPERTS, D, EXPERT_DIM), D ** -0.5),
        'moe_w_down': nrm((N_POOL_LAYERS, N_EXPERTS, EXPERT_DIM, D), EXPERT_DIM ** -0.5 * beta),
    }


def reference(x, c, ctx, c_ctx, mod_w, mod_b, ln1_g, ln1_b, ln2_g, ln2_b, attn_w_in, na_rel_bias,
              mla_q_norm, mla_w_q_up, mla_kv_norm, mla_w_kv_up, attn_w_out, ffn_w_gate, ffn_w_up,
              ffn_w_down, pool_w, pool_scale, moe_router, moe_w_gate, moe_w_up, moe_w_down):
    for i in range(DEPTH):
        j = i // 2
        even = i % 2 == 0
        ctx_live = any(l % 2 == 0 for l in range(i + 1, DEPTH))
        m_lat = (jax.nn.silu(c) @ mod_w[i] + mod_b[i])[:, None, :]
        sh1, sc1, g1, sh2, sc2, g2 = jnp.split(m_lat, 6, axis=-1)
        m_ctx = jax.nn.silu(c_ctx) @ mod_w[i] + mod_b[i]
        csh1, csc1, cg1, csh2, csc2, cg2 = jnp.split(m_ctx, 6, axis=-1)

        h_lat = modulate(x, sh1, sc1)
        if even:
            h_ctx = modulate(ctx, csh1, csc1)
            y_lat, y_ctx = attention_mixer(h_lat, h_ctx, attn_w_in[j], na_rel_bias[j], mla_q_norm[j],
                                           mla_w_q_up[j], mla_kv_norm[j], mla_w_kv_up[j], attn_w_out[j],
                                           ctx_live)
        else:
            y_lat = pool_mixer(h_lat, pool_w[j], pool_scale[j])
            y_ctx = pool_mixer(modulate(ctx, csh1, csc1), pool_w[j], pool_scale[j]) if ctx_live else None
        x = layer_norm(DEEPNORM_ALPHA * x + g1 * y_lat, ln1_g[i], ln1_b[i])
        if ctx_live:
            ctx = layer_norm(DEEPNORM_ALPHA * ctx + cg1 * y_ctx, ln1_g[i], ln1_b[i])

        h = modulate(x, sh2, sc2)
        n_ctx = ctx.shape[1] if ctx_live else 0
        if ctx_live:
            h = jnp.concatenate([modulate(ctx, csh2, csc2), h], axis=1)
        if even:
            y = swiglu(h, ffn_w_gate[j], ffn_w_up[j], ffn_w_down[j])
        else:
            y = moe_swiglu(h, moe_router[j], moe_w_gate[j], moe_w_up[j], moe_w_down[j])
        x = layer_norm(DEEPNORM_ALPHA * x + g2 * y[:, n_ctx:], ln2_g[i], ln2_b[i])
        if ctx_live:
            ctx = layer_norm(DEEPNORM_ALPHA * ctx + cg2 * y[:, :n_ctx], ln2_g[i], ln2_b[i])
    return x
```

```python
import numpy as np
import ml_dtypes
from contextlib import ExitStack
import concourse.bass as bass
import concourse.mybir as mybir
from concourse.bass_utils import run_bass_kernel_spmd

F32 = mybir.dt.float32
BF16 = mybir.dt.bfloat16
AF = mybir.ActivationFunctionType
ALU = mybir.AluOpType
AX = mybir.AxisListType

NCORE = 8
D = 1024
S = 16384
TPC = S // NCORE
NLT = TPC // 128
NCT = 2
NT = NLT + NCT
NTOK = NT * 128
DEPTH = 4
ALPHA = (2 * DEPTH) ** 0.25
FFN = 2816
EXPD = 3584
NEXP = 8
LN_EPS = 1e-5
RMS_EPS = 1e-6
NKEY = 256 + S
NKT = NKEY // 128
SM_MLA = 96 ** -0.5
BLOCKS = [(0, 512), (512, 512), (1024, 512), (1536, 512), (2048, 256)]


class Sched:
    def __init__(self):
        self.ops = []
        self.lastw = {}
        self.readers = {}
        self.bar = set()

    def add(self, st, fn, r=(), w=(), kind="c"):
        i = len(self.ops)
        deps = set(self.bar)
        for k in r:
            if k in self.lastw:
                deps.add(self.lastw[k])
        for k in w:
            if k in self.lastw:
                deps.add(self.lastw[k])
            for j in self.readers.get(k, ()):
                deps.add(j)
        self.ops.append(dict(st=st, fn=fn, kind=kind, deps=deps, inc=False, sv=None))
        for k in r:
            self.readers.setdefault(k, []).append(i)
        for k in w:
            self.lastw[k] = i
            self.readers[k] = []
        return i

    def barrier(self):
        last = {}
        dl = {}
        for i, o in enumerate(self.ops):
            if o["kind"] == "c":
                last[o["st"]] = i
            else:
                dl.setdefault(o["st"], []).append(i)
        self.bar = set(last.values())
        for lst in dl.values():
            self.bar.update(lst[-8:])

    def emit(self, nc, es):
        ops = self.ops
        NP = 8
        streams = ["pe", "act", "dve", "pool", "sp"]
        dma_idx = {s: [] for s in streams}
        for i, o in enumerate(ops):
            if o["kind"] == "d":
                lst = dma_idx[o["st"]]
                if len(lst) >= NP:
                    o["deps"].add(lst[-NP])
                lst.append(i)
            best = {}
            keep = []
            for j in o["deps"]:
                p = ops[j]
                if p["kind"] == "c":
                    if p["st"] == "pe" and o["st"] == "pe" and o["kind"] == "c":
                        continue
                    if p["st"] not in best or best[p["st"]] < j:
                        best[p["st"]] = j
                else:
                    keep.append(j)
            for j in best.values():
                ops[j]["inc"] = True
                keep.append(j)
            o["deps"] = keep
        csem = {s: es.enter_context(nc.semaphore("c_" + s)) for s in streams}
        dsem = {s: [es.enter_context(nc.semaphore("d_%s%d" % (s, k))) for k in range(NP)] for s in ("pool", "sp")}
        cnt = {s: 0 for s in streams}
        dcnt = {s: 0 for s in streams}
        for o in ops:
            if o["kind"] == "c":
                if o["inc"]:
                    cnt[o["st"]] += 1
                    o["sv"] = (csem[o["st"]], cnt[o["st"]], 1)
            else:
                k = dcnt[o["st"]]
                dcnt[o["st"]] += 1
                o["sv"] = (dsem[o["st"]][k % NP], 16 * (k // NP + 1), 16)
        bystream = {s: [o for o in ops if o["st"] == s] for s in streams}

        def run(eng, st):
            lst = bystream[st]
            waited = {}
            for o in lst:
                for j in o["deps"]:
                    sem, val, _ = ops[j]["sv"]
                    key = id(sem)
                    if waited.get(key, 0) < val:
                        eng.wait_ge(sem, val)
                        waited[key] = val
                ins = o["fn"](eng)
                if o["sv"] is not None:
                    ins.then_inc(o["sv"][0], o["sv"][2])
            if st in dsem:
                k = dcnt[st]
                for q in range(NP):
                    n = (k - q + NP - 1) // NP if k > q else 0
                    if n > 0:
                        eng.wait_ge(dsem[st][q], 16 * n)

        with nc.Block() as block:
            @block.tensor
            def _(e):
                run(e, "pe")

            @block.scalar
            def _(e):
                run(e, "act")

            @block.vector
            def _(e):
                run(e, "dve")

            @block.gpsimd
            def _(e):
                run(e, "pool")

            @block.sync
            def _(e):
                run(e, "sp")


class Prog:
    def __init__(self, arb_elems, arf_elems):
        self.nc = nc = bass.Bass("TRN2", target_bir_lowering=False)
        self.es = es = ExitStack()
        self.S = Sched()
        self.add = self.S.add
        self.x_in = self.ext("x", [NTOK, D])
        self.cvec_in = self.ext("cvec", [128, 2, 8])
        self.modw_in = self.ext("mod_w", [D, 6 * D])
        self.modb_in = self.ext("mod_b", [1, 6 * D])
        self.modbc_in = self.ext("mod_bc", [128, 48])
        self.ln_in = self.ext("ln", [5, D])
        self.ident_in = self.ext("ident", [128, 128])
        self.grow = nc.dram_tensor("grow", [2, 2, D], F32)
        self.X = self.sb("X", [128, NT, D], F32)
        self.ARB = self.sb("ARB", [128, arb_elems], BF16)
        self.ARF = self.sb("ARF", [128, arf_elems], F32)
        self.MCOL = self.sb("MCOL", [128, 2, 48], F32)
        self.MBCOL = self.sb("MBCOL", [128, 48], F32)
        self.SC = self.sb("SC", [128, 2, 8], F32)
        self.SCB = self.sb("SCB", [128, 8, 2], BF16)
        self.IDF = self.sb("IDF", [128, 128], F32)
        self.IDB = self.sb("IDB", [128, 128], BF16)
        self.ONB = self.sb("ONB", [128, 128], BF16)
        self.ONF = self.sb("ONF", [128, 64], F32)
        self.STAT = self.sb("STAT", [128, 8], F32)
        self.SCL = self.sb("SCL", [128, 2, 8], F32)
        self.SHF = self.sb("SHF", [128, 2, 8], F32)
        self.PS = es.enter_context(nc.psum_tensor("PS", [128, 7, 512], F32))
        self.PST = es.enter_context(nc.psum_tensor("PST", [128, 1024], BF16))
        self.bo = 0
        self.fo = 0
        add = self.add
        add("sp", lambda e: e.dma_start(out=self.IDF[:, :], in_=self.ident_in[:, :]), w=["IDF"], kind="d")
        add("dve", lambda e: e.tensor_copy(out=self.IDB[:, :], in_=self.IDF[:, :]), r=["IDF"], w=["IDB"])
        add("dve", lambda e: e.memset(self.ONB[:, :], 1.0), w=["ONB"])
        add("dve", lambda e: e.memset(self.ONF[:, :], 1.0), w=["ONF"])

    def ext(self, name, shape, dt=F32):
        return self.nc.dram_tensor(name, list(shape), dt, kind="ExternalInput")

    def outp(self, name, shape, dt=F32):
        return self.nc.dram_tensor(name, list(shape), dt, kind="ExternalOutput")

    def sb(self, name, shape, dt):
        return self.es.enter_context(self.nc.sbuf_tensor(name, list(shape), dt))

    def phase(self):
        self.S.barrier()
        self.bo = 0
        self.fo = 0

    def vb(self, pattern=None, n=None, **kw):
        v = self.ARB[:, self.bo:self.bo + n]
        self.bo += n
        assert self.bo <= self.ARB.shape[1], ("ARB overflow", self.bo)
        return v.rearrange(pattern, **kw) if pattern else v

    def vf(self, pattern=None, n=None, **kw):
        v = self.ARF[:, self.fo:self.fo + n]
        self.fo += n
        assert self.fo <= self.ARF.shape[1], ("ARF overflow", self.fo)
        return v.rearrange(pattern, **kw) if pattern else v

    def load_stream(self, first):
        X, add = self.X, self.add
        for t in range(NT):
            add("sp", lambda e, t=t: e.dma_start(out=X[:, t, :], in_=self.x_in[t * 128:(t + 1) * 128, :]), w=[("X", t)], kind="d")
            if first:
                add("dve", lambda e, t=t: e.tensor_scalar(out=X[:, t, :], in0=X[:, t, :], scalar1=ALPHA, scalar2=None, op0=ALU.mult),
                    r=[("X", t)], w=[("X", t)])

    def store_stream(self, y_out):
        for t in range(NT):
            self.add("sp", lambda e, t=t: e.dma_start(out=y_out[t * 128:(t + 1) * 128, :], in_=self.X[:, t, :]), r=[("X", t)], kind="d")

    def modulation(self):
        add, PS = self.add, self.PS
        SC, SCB, MCOL, MBCOL = self.SC, self.SCB, self.MCOL, self.MBCOL
        self.phase()
        MW = self.vb("p (k c) -> p k c", n=8 * 512, k=8)
        SCBC = self.vb("p (v k m) -> p v k m", n=2 * 8 * 128, v=2, k=8)
        GR = self.vf(n=512)
        MBR = self.vf(n=2 * D)
        mw = self.modw_in
        add("sp", lambda e: e.dma_start(out=SC[:, :, :], in_=self.cvec_in[:, :, :]), w=["SC"], kind="d")
        add("act", lambda e: e.activation(out=SC[:, :, :], in_=SC[:, :, :], func=AF.Silu), r=["SC"], w=["SC"])
        add("dve", lambda e: e.tensor_copy(out=SCB[:, :, :], in_=SC[:, :, :].rearrange("p v k -> p k v")), r=["SC"], w=["SCB"])
        for v in range(2):
            add("dve", lambda e, v=v: e.tensor_copy(out=SCBC[:, v, :, :], in_=SC[:, v, :].unsqueeze(2).to_broadcast([128, 8, 128])),
                r=["SC"], w=[("SCBC", v)])
        add("sp", lambda e: e.dma_start(out=MBCOL[:, :], in_=self.modbc_in[:, :]), w=["MBCOL"], kind="d")
        for cb in range(12):
            add("pool", lambda e, cb=cb: e.dma_start(out=MW[:, :, :], in_=mw.ap().rearrange("(k p) c -> p k c", p=128)[:, :, cb * 512:(cb + 1) * 512]),
                w=["MW"], kind="d")
            for jj in range(4):
                for kc in range(8):
                    add("pe", lambda e, jj=jj, kc=kc: e.matmul(PS[:, 6, jj * 2:jj * 2 + 2], lhsT=MW[:, kc, jj * 128:(jj + 1) * 128], rhs=SCB[:, kc, :],
                                                             start=(kc == 0), stop=(kc == 7)),
                        r=["MW", "SCB"], w=[("PS", 6)])
            add("dve", lambda e, cb=cb: e.tensor_tensor(out=MCOL[:, :, cb * 4:(cb + 1) * 4].rearrange("p v j -> p j v"),
                                                       in0=PS[:, 6, 0:8].rearrange("p (j v) -> p j v", v=2),
                                                       in1=MBCOL[:, cb * 4:(cb + 1) * 4].unsqueeze(2).to_broadcast([128, 4, 2]), op=ALU.add),
                r=[("PS", 6), "MBCOL"], w=["MCOL"])
        for gi, which in enumerate((2, 5)):
            add("sp", lambda e, which=which: e.dma_start(out=MBR[0:1, 0:D], in_=self.modb_in[0:1, which * D:(which + 1) * D]), w=["MBR"], kind="d")
            for hf in range(2):
                add("pool", lambda e, which=which, hf=hf: e.dma_start(out=MW[:, :, :], in_=mw.ap().rearrange("(k p) c -> p k c", p=128)[:, :, which * D + hf * 512: which * D + (hf + 1) * 512]),
                    w=["MW"], kind="d")
                for v in range(2):
                    for kc in range(8):
                        add("pe", lambda e, v=v, kc=kc: e.matmul(PS[:, 5, :], lhsT=SCBC[:, v, kc, :], rhs=MW[:, kc, :], start=(kc == 0), stop=(kc == 7)),
                            r=["MW", ("SCBC", v)], w=[("PS", 5)])
                    add("dve", lambda e, hf=hf: e.tensor_tensor(out=GR[0:1, :], in0=PS[0:1, 5, :], in1=MBR[0:1, hf * 512:(hf + 1) * 512], op=ALU.add),
                        r=[("PS", 5), "MBR"], w=["GR"])
                    add("sp", lambda e, gi=gi, v=v, hf=hf: e.dma_start(out=self.grow[gi, v:v + 1, hf * 512:(hf + 1) * 512], in_=GR[0:1, :]),
                        r=["GR"], w=[("grow", gi)], kind="d")

    def load_gate(self, GBC, gi):
        for v in range(2):
            self.add("sp", lambda e, v=v: e.dma_start(out=GBC[:, v, :], in_=self.grow[gi, v:v + 1, :].to_broadcast([128, D])),
                     r=[("grow", gi)], w=[("GBC", v)], kind="d")

    def mod_cols(self, shift_idx, scale_idx):
        self.add("dve", lambda e: e.tensor_scalar(out=self.SCL[:, :, :], in0=self.MCOL[:, :, scale_idx * 8:(scale_idx + 1) * 8], scalar1=1.0, scalar2=1.0 / ALPHA,
                                                  op0=ALU.add, op1=ALU.mult), r=["MCOL"], w=["SCL"])
        self.add("dve", lambda e: e.tensor_copy(out=self.SHF[:, :, :], in_=self.MCOL[:, :, shift_idx * 8:(shift_idx + 1) * 8]), r=["MCOL"], w=["SHF"])

    def make_HT(self, HT, tiles, with_shift=True):
        add, PS, X = self.add, self.PS, self.X
        for t in tiles:
            v = 0 if t < NLT else 1
            for half in range(2):
                b = half
                for q in range(4):
                    kc = half * 4 + q
                    add("pe", lambda e, t=t, kc=kc, b=b, q=q: e.transpose(PS[:, b, q * 128:(q + 1) * 128], X[:, t, kc * 128:(kc + 1) * 128], self.IDF[:, :]),
                        r=[("X", t), "IDF"], w=[("PS", b)])
                for q in range(4):
                    kc = half * 4 + q
                    if with_shift:
                        add("act", lambda e, t=t, kc=kc, b=b, q=q, v=v: e.activation(out=HT[:, kc, t * 128:(t + 1) * 128], in_=PS[:, b, q * 128:(q + 1) * 128],
                                                                                      func=AF.Identity, bias=self.SHF[:, v, kc:kc + 1], scale=self.SCL[:, v, kc:kc + 1]),
                            r=[("PS", b), "SCL", "SHF"], w=[("HT", t)])
                    else:
                        add("act", lambda e, t=t, kc=kc, b=b, q=q, v=v: e.activation(out=HT[:, kc, t * 128:(t + 1) * 128], in_=PS[:, b, q * 128:(q + 1) * 128],
                                                                                      func=AF.Copy, scale=self.SCL[:, v, kc:kc + 1]),
                            r=[("PS", b), "SCL"], w=[("HT", t)])

    def layer_norm(self, LNV, TMP, which, tiles, final=False, tkey="TMPA"):
        add, X, STAT = self.add, self.X, self.STAT
        a = 1.0 if final else ALPHA
        row = which * 2
        add("sp", lambda e: e.dma_start(out=LNV[:, 0, :], in_=self.ln_in[row:row + 1, :].to_broadcast([128, D])), w=[("LNV", 0)], kind="d")
        add("sp", lambda e: e.dma_start(out=LNV[:, 1, :], in_=self.ln_in[row + 1:row + 2, :].to_broadcast([128, D])), w=[("LNV", 1)], kind="d")
        if a != 1.0:
            add("dve", lambda e: e.tensor_scalar(out=LNV[:, :, :], in0=LNV[:, :, :], scalar1=a, scalar2=None, op0=ALU.mult),
                r=[("LNV", 0), ("LNV", 1)], w=[("LNV", 0), ("LNV", 1)])
        for t in tiles:
            k = ("X", t)
            add("dve", lambda e, t=t: e.tensor_reduce(out=STAT[:, 0:1], in_=X[:, t, :], axis=AX.X, op=ALU.add), r=[k], w=["STAT"])
            add("dve", lambda e: e.tensor_scalar(out=STAT[:, 1:2], in0=STAT[:, 0:1], scalar1=-1.0 / D, scalar2=None, op0=ALU.mult), r=["STAT"], w=["STAT"])
            add("dve", lambda e, t=t: e.tensor_scalar(out=X[:, t, :], in0=X[:, t, :], scalar1=STAT[:, 1:2], scalar2=None, op0=ALU.add), r=[k, "STAT"], w=[k])
            add("dve", lambda e, t=t: e.tensor_tensor(out=TMP[:, :], in0=X[:, t, :], in1=X[:, t, :], op=ALU.mult), r=[k], w=[tkey])
            add("dve", lambda e: e.tensor_reduce(out=STAT[:, 2:3], in_=TMP[:, :], axis=AX.X, op=ALU.add), r=[tkey], w=["STAT"])
            add("dve", lambda e: e.tensor_scalar(out=STAT[:, 3:4], in0=STAT[:, 2:3], scalar1=1.0 / D, scalar2=LN_EPS, op0=ALU.mult, op1=ALU.add), r=["STAT"], w=["STAT"])
            add("act", lambda e: e.activation(out=STAT[:, 5:6], in_=STAT[:, 3:4], func=AF.Sqrt), r=["STAT"], w=["STAT"])
            add("dve", lambda e: e.reciprocal(out=STAT[:, 4:5], in_=STAT[:, 5:6]), r=["STAT"], w=["STAT"])
            add("dve", lambda e, t=t: e.scalar_tensor_tensor(out=X[:, t, :], in0=X[:, t, :], scalar=STAT[:, 4:5], in1=LNV[:, 0, :], op0=ALU.mult, op1=ALU.mult),
                r=[k, "STAT", ("LNV", 0)], w=[k])
            add("dve", lambda e, t=t: e.tensor_tensor(out=X[:, t, :], in0=X[:, t, :], in1=LNV[:, 1, :], op=ALU.add), r=[k, ("LNV", 1)], w=[k])

    def accum(self, GBC, TMP, t, b0, gate=None, tkey="TMPA"):
        v = 0 if t < NLT else 1
        PS, X = self.PS, self.X
        src = PS[:, b0:b0 + 2, :].rearrange("p a b -> p (a b)")
        if gate is None:
            self.add("dve", lambda e: e.tensor_tensor(out=TMP[:, :], in0=src, in1=GBC[:, v, :], op=ALU.mult),
                     r=[("PS", b0), ("PS", b0 + 1), ("GBC", v)], w=[tkey])
        else:
            gap, gkey = gate
            self.add("dve", lambda e: e.scalar_tensor_tensor(out=TMP[:, :], in0=src, scalar=gap, in1=GBC[:, v, :], op0=ALU.mult, op1=ALU.mult),
                     r=[("PS", b0), ("PS", b0 + 1), ("GBC", v), gkey], w=[tkey])
        self.add("pool", lambda e: e.tensor_tensor(out=X[:, t, :], in0=X[:, t, :], in1=TMP[:, :], op=ALU.add), r=[("X", t), tkey], w=[("X", t)])

    def swiglu(self, bufs, HT, GBC, wg, wu, wd, F, tiles, e_off=0, gates=None, cnt0=0):
        add, PS = self.add, self.PS
        WA, WB, WD, AT, SG, TMP = bufs
        nbuf = len(WA)
        nfb = (F + 511) // 512
        ntok = len(tiles) * 128
        t0 = tiles[0] * 128
        blocks = [(b0, min(512, ntok - b0)) for b0 in range(0, ntok, 512)]
        cnt = cnt0
        hkeys = [("HT", tt) for tt in tiles]
        for fb in range(nfb):
            f0 = fb * 512
            fw = min(512, F - f0)
            nfc = fw // 128
            par = cnt % nbuf
            cnt += 1
            add("pool", lambda e, f0=f0, fw=fw, par=par: e.dma_start(out=WA[par][:, :, 0:fw], in_=wg.ap()[e_off * D:(e_off + 1) * D, :].rearrange("(k p) c -> p k c", p=128)[:, :, f0:f0 + fw]),
                w=[("WA", par)], kind="d")
            add("pool", lambda e, f0=f0, fw=fw, par=par: e.dma_start(out=WB[par][:, :, 0:fw], in_=wu.ap()[e_off * D:(e_off + 1) * D, :].rearrange("(k p) c -> p k c", p=128)[:, :, f0:f0 + fw]),
                w=[("WB", par)], kind="d")
            add("pool", lambda e, f0=f0, nfc=nfc, par=par: e.dma_start(out=WD[par][:, 0:nfc, :], in_=wd.ap()[e_off * F + f0:e_off * F + f0 + nfc * 128, :].rearrange("(k p) c -> p k c", p=128)),
                w=[("WD", par)], kind="d")
            for bi, (b0, bw) in enumerate(blocks):
                ap_ = bi % 2
                for fc in range(nfc):
                    for kc in range(8):
                        add("pe", lambda e, fc=fc, kc=kc, par=par, b0=b0, bw=bw: e.matmul(PS[:, 4, 0:bw], lhsT=WA[par][:, kc, fc * 128:(fc + 1) * 128], rhs=HT[:, kc, t0 + b0:t0 + b0 + bw],
                                                                                           start=(kc == 0), stop=(kc == 7)),
                            r=[("WA", par)] + hkeys, w=[("PS", 4)])
                    for kc in range(8):
                        add("pe", lambda e, fc=fc, kc=kc, par=par, b0=b0, bw=bw: e.matmul(PS[:, 5, 0:bw], lhsT=WB[par][:, kc, fc * 128:(fc + 1) * 128], rhs=HT[:, kc, t0 + b0:t0 + b0 + bw],
                                                                                           start=(kc == 0), stop=(kc == 7)),
                            r=[("WB", par)] + hkeys, w=[("PS", 5)])
                    sp_ = fc % 2
                    add("act", lambda e, sp_=sp_, bw=bw: e.activation(out=SG[sp_][:, 0:bw], in_=PS[:, 4, 0:bw], func=AF.Silu), r=[("PS", 4)], w=[("SG", sp_)])
                    add("dve", lambda e, sp_=sp_, ap_=ap_, fc=fc, bw=bw: e.tensor_tensor(out=AT[ap_][:, fc, 0:bw], in0=PS[:, 5, 0:bw], in1=SG[sp_][:, 0:bw], op=ALU.mult),
                        r=[("PS", 5), ("SG", sp_)], w=[("AT", ap_)])
                for ti in range(bw // 128):
                    t = tiles[0] + b0 // 128 + ti
                    yp = ti % 2
                    for hf in range(2):
                        for fc in range(nfc):
                            add("pe", lambda e, ap_=ap_, fc=fc, ti=ti, par=par, hf=hf, yp=yp: e.matmul(PS[:, 2 * yp + hf, :], lhsT=AT[ap_][:, fc, ti * 128:(ti + 1) * 128], rhs=WD[par][:, fc, hf * 512:(hf + 1) * 512],
                                                                                                       start=(fc == 0), stop=(fc == nfc - 1)),
                                r=[("AT", ap_), ("WD", par)], w=[("PS", 2 * yp + hf)])
                    self.accum(GBC, TMP[yp], t, 2 * yp, gate=None if gates is None else gates(t), tkey=("TMPA", yp))
        return cnt

    def ffn_bufs(self, nbuf):
        WA = [self.vb("p (k c) -> p k c", n=8 * 512, k=8) for _ in range(nbuf)]
        WB = [self.vb("p (k c) -> p k c", n=8 * 512, k=8) for _ in range(nbuf)]
        WD = [self.vb("p (k c) -> p k c", n=4 * D, k=4) for _ in range(nbuf)]
        AT = [self.vb("p (k c) -> p k c", n=4 * 512, k=4) for _ in range(2)]
        SG = [self.vf(n=512) for _ in range(2)]
        TMP = [self.vf(n=D) for _ in range(2)]
        return WA, WB, WD, AT, SG, TMP

    def finish(self):
        self.S.emit(self.nc, self.es)
        self.es.close()
        return self.nc


def build_PA(first):
    P = Prog(44 * 1024, 6 * 1024)
    add, PS = P.add, P.PS
    w_in = P.ext("w_in", [D, 1952])
    wkr_in = P.ext("wkr", [D, 96])
    wkrp_in = P.ext("wkrp", [D, 96])
    nrm_in = P.ext("nrm", [128, 3])
    rope_in = P.ext("rope", [32, 2, TPC])
    qa_o = P.outp("qa", [128, 4, NTOK], BF16)
    ka_o = P.outp("ka", [128, 4, NTOK], BF16)
    va_o = P.outp("va", [128, NT, 640], BF16)
    qcn_o = P.outp("qcn", [128, 2, NTOK], BF16)
    kvn_o = P.outp("kvn", [128, NTOK], BF16)
    kr_o = P.outp("kr", [32, NTOK], BF16)
    P.load_stream(first)
    P.modulation()
    P.mod_cols(0, 1)
    P.phase()
    HT = P.vb("p (k c) -> p k c", n=8 * NTOK, k=8)
    WQ = P.vb("p (k c) -> p k c", n=8 * 512, k=8)
    WS = P.vb("p (k c) -> p k c", n=8 * 256, k=8)
    WKV = P.vb("p (k c) -> p k c", n=8 * 128, k=8)
    WKR = P.vb("p (k c) -> p k c", n=8 * 96, k=8)
    WKRP = P.vb("p (k c) -> p k c", n=8 * 96, k=8)
    OUTB = P.vb("p (k c) -> p k c", n=2 * NTOK, k=2)
    VAO = P.vb("p (t h c) -> p t h c", n=2 * 640, t=2, h=8)
    QCN = P.vb("p (k c) -> p k c", n=2 * NTOK, k=2)
    KVN = P.vb(n=NTOK)
    KR = P.vb(n=NTOK)
    SQ = P.vb("p (k c) -> p k c", n=2 * 512, k=2)
    NRM = P.vf(n=3)
    RB = P.vf(n=512)
    T1 = P.vf(n=512)
    T2 = P.vf(n=512)
    ROPE = P.vf("p (a c) -> p a c", n=2 * 512, a=2)
    P.make_HT(HT, list(range(NT)))
    hk = [("HT", t) for t in range(NT)]
    w3 = w_in.ap().rearrange("(k p) c -> p k c", p=128)
    add("sp", lambda e: e.dma_start(out=NRM[:, :], in_=nrm_in[:, :]), w=["NRM"], kind="d")
    add("pool", lambda e: e.dma_start(out=WS[:, :, :], in_=w3[:, :, 1536:1792]), w=["WS"], kind="d")
    add("pool", lambda e: e.dma_start(out=WKV[:, :, :], in_=w3[:, :, 1792:1920]), w=["WKV"], kind="d")
    add("pool", lambda e: e.dma_start(out=WKR[:, :, :], in_=wkr_in.ap().rearrange("(k p) c -> p k c", p=128)), w=["WKR"], kind="d")
    add("pool", lambda e: e.dma_start(out=WKRP[:, :, :], in_=wkrp_in.ap().rearrange("(k p) c -> p k c", p=128)), w=["WKRP"], kind="d")
    add("dve", lambda e: e.memset(VAO[:, :, :, :], 1.0), w=[("VAO", 0), ("VAO", 1)])
    for wi, dst in ((0, qa_o), (1, ka_o)):
        add("pool", lambda e, wi=wi: e.dma_start(out=WQ[:, :, :], in_=w3[:, :, wi * 512:(wi + 1) * 512]), w=["WQ"], kind="d")
        n = 0
        for jc in range(4):
            for (b0, bw) in BLOCKS:
                b = n % 2
                n += 1
                for kc in range(8):
                    add("pe", lambda e, jc=jc, kc=kc, b=b, b0=b0, bw=bw: e.matmul(PS[:, b, 0:bw], lhsT=WQ[:, kc, jc * 128:(jc + 1) * 128], rhs=HT[:, kc, b0:b0 + bw],
                                                                                   start=(kc == 0), stop=(kc == 7)), r=["WQ"] + hk, w=[("PS", b)])
                add("act", lambda e, jc=jc, b=b, b0=b0, bw=bw: e.activation(out=OUTB[:, jc % 2, b0:b0 + bw], in_=PS[:, b, 0:bw], func=AF.Copy),
                    r=[("PS", b)], w=[("OUTB", jc % 2)])
            add("sp", lambda e, dst=dst, jc=jc: e.dma_start(out=dst[:, jc, :], in_=OUTB[:, jc % 2, :]), r=[("OUTB", jc % 2)], kind="d")
    add("pool", lambda e: e.dma_start(out=WQ[:, :, :], in_=w3[:, :, 1024:1536]), w=["WQ"], kind="d")
    for t in range(NT):
        b = t % 2
        for kc in range(8):
            add("pe", lambda e, t=t, kc=kc, b=b: e.matmul(PS[:, b, :], lhsT=HT[:, kc, t * 128:(t + 1) * 128], rhs=WQ[:, kc, :], start=(kc == 0), stop=(kc == 7)),
                r=["WQ", ("HT", t)], w=[("PS", b)])
        add("dve", lambda e, t=t, b=b: e.tensor_copy(out=VAO[:, b, :, 0:64], in_=PS[:, b, :].rearrange("p (h c) -> p h c", h=8)), r=[("PS", b)], w=[("VAO", b)])
        add("sp", lambda e, t=t, b=b: e.dma_start(out=va_o[:, t, :], in_=VAO[:, b, :, :].rearrange("p h c -> p (h c)")), r=[("VAO", b)], kind="d")
    for (b0, bw) in BLOCKS:
        isctx = b0 >= TPC
        for ch in range(2):
            for kc in range(8):
                add("pe", lambda e, ch=ch, kc=kc, b0=b0, bw=bw: e.matmul(PS[:, 2 + ch, 0:bw], lhsT=WS[:, kc, ch * 128:(ch + 1) * 128], rhs=HT[:, kc, b0:b0 + bw],
                                                                          start=(kc == 0), stop=(kc == 7)), r=["WS"] + hk, w=[("PS", 2 + ch)])
            add("act", lambda e, ch=ch, bw=bw: e.activation(out=SQ[:, ch, 0:bw], in_=PS[:, 2 + ch, 0:bw], func=AF.Square), r=[("PS", 2 + ch)], w=[("SQ", ch)])
        for ch in range(2):
            add("pe", lambda e, ch=ch, bw=bw: e.matmul(PS[:, 4, 0:bw], lhsT=P.ONB[:, :], rhs=SQ[:, ch, 0:bw], start=(ch == 0), stop=(ch == 1)),
                r=["ONB", ("SQ", ch)], w=[("PS", 4)])
        add("dve", lambda e, bw=bw: e.tensor_scalar(out=RB[:, 0:bw], in0=PS[:, 4, 0:bw], scalar1=1.0 / 256, scalar2=RMS_EPS, op0=ALU.mult, op1=ALU.add), r=[("PS", 4)], w=["RB"])
        add("act", lambda e, bw=bw: e.activation(out=RB[:, 0:bw], in_=RB[:, 0:bw], func=AF.Sqrt), r=["RB"], w=["RB"])
        add("dve", lambda e, bw=bw: e.reciprocal(out=RB[:, 0:bw], in_=RB[:, 0:bw]), r=["RB"], w=["RB"])
        for ch in range(2):
            add("dve", lambda e, ch=ch, b0=b0, bw=bw: e.scalar_tensor_tensor(out=QCN[:, ch, b0:b0 + bw], in0=PS[:, 2 + ch, 0:bw], scalar=NRM[:, ch:ch + 1], in1=RB[:, 0:bw],
                                                                              op0=ALU.mult, op1=ALU.mult), r=[("PS", 2 + ch), "NRM", "RB"], w=["QCN"])
        for kc in range(8):
            add("pe", lambda e, kc=kc, b0=b0, bw=bw: e.matmul(PS[:, 5, 0:bw], lhsT=WKV[:, kc, :], rhs=HT[:, kc, b0:b0 + bw], start=(kc == 0), stop=(kc == 7)),
                r=["WKV"] + hk, w=[("PS", 5)])
        add("act", lambda e, bw=bw: e.activation(out=SQ[:, 0, 0:bw], in_=PS[:, 5, 0:bw], func=AF.Square), r=[("PS", 5)], w=[("SQ", 0)])
        add("pe", lambda e, bw=bw: e.matmul(PS[:, 4, 0:bw], lhsT=P.ONB[:, :], rhs=SQ[:, 0, 0:bw], start=True, stop=True), r=["ONB", ("SQ", 0)], w=[("PS", 4)])
        add("dve", lambda e, bw=bw: e.tensor_scalar(out=RB[:, 0:bw], in0=PS[:, 4, 0:bw], scalar1=1.0 / 128, scalar2=RMS_EPS, op0=ALU.mult, op1=ALU.add), r=[("PS", 4)], w=["RB"])
        add("act", lambda e, bw=bw: e.activation(out=RB[:, 0:bw], in_=RB[:, 0:bw], func=AF.Sqrt), r=["RB"], w=["RB"])
        add("dve", lambda e, bw=bw: e.reciprocal(out=RB[:, 0:bw], in_=RB[:, 0:bw]), r=["RB"], w=["RB"])
        add("dve", lambda e, b0=b0, bw=bw: e.scalar_tensor_tensor(out=KVN[:, b0:b0 + bw], in0=PS[:, 5, 0:bw], scalar=NRM[:, 2:3], in1=RB[:, 0:bw], op0=ALU.mult, op1=ALU.mult),
            r=[("PS", 5), "NRM", "RB"], w=["KVN"])
        for kc in range(8):
            add("pe", lambda e, kc=kc, b0=b0, bw=bw: e.matmul(PS[0:96, 6, 0:bw], lhsT=WKR[:, kc, :], rhs=HT[:, kc, b0:b0 + bw], start=(kc == 0), stop=(kc == 7)),
                r=["WKR"] + hk, w=[("PS", 6)])
        if isctx:
            add("act", lambda e, b0=b0, bw=bw: e.activation(out=KR[64:96, b0:b0 + bw], in_=PS[64:96, 6, 0:bw], func=AF.Copy), r=[("PS", 6)], w=["KR"])
        else:
            for kc in range(8):
                add("pe", lambda e, kc=kc, b0=b0, bw=bw: e.matmul(PS[0:96, 0, 0:bw], lhsT=WKRP[:, kc, :], rhs=HT[:, kc, b0:b0 + bw], start=(kc == 0), stop=(kc == 7)),
                    r=["WKRP"] + hk, w=[("PS", 0)])
            add("sp", lambda e, b0=b0, bw=bw: e.dma_start(out=ROPE[64:96, :, 0:bw], in_=rope_in[:, :, b0:b0 + bw]), w=["ROPE"], kind="d")
            add("dve", lambda e, bw=bw: e.tensor_tensor(out=T1[64:96, 0:bw], in0=PS[64:96, 6, 0:bw], in1=ROPE[64:96, 0, 0:bw], op=ALU.mult), r=[("PS", 6), "ROPE"], w=["T1"])
            add("dve", lambda e, bw=bw: e.tensor_tensor(out=T2[64:96, 0:bw], in0=PS[64:96, 0, 0:bw], in1=ROPE[64:96, 1, 0:bw], op=ALU.mult), r=[("PS", 0), "ROPE"], w=["T2"])
            add("dve", lambda e, b0=b0, bw=bw: e.tensor_tensor(out=KR[64:96, b0:b0 + bw], in0=T1[64:96, 0:bw], in1=T2[64:96, 0:bw], op=ALU.add), r=["T1", "T2"], w=["KR"])
    add("sp", lambda e: e.dma_start(out=qcn_o[:, :, :], in_=QCN[:, :, :]), r=["QCN"], kind="d")
    add("sp", lambda e: e.dma_start(out=kvn_o[:, :], in_=KVN[:, :]), r=["KVN"], kind="d")
    add("sp", lambda e: e.dma_start(out=kr_o[:, :], in_=KR[64:96, :]), r=["KR"], kind="d")
    return P.finish()


def build_PB(first, ctx_out, stop=9):
    P = Prog(46 * 1024, 9 * 1024)
    add, PS, PST, X = P.add, P.PS, P.PST, P.X
    qa_in = P.ext("qa", [128, 4, NTOK], BF16)
    kext_in = P.ext("kext", [128, 4, 22 * 128 + 256], BF16)
    vext_in = P.ext("vext", [128, 24, 640], BF16)
    qcn_in = P.ext("qcn", [128, 2, NTOK], BF16)
    kvn_in = P.ext("kvn_all", [128, NKEY], BF16)
    kr_in = P.ext("kr_all", [32, NKEY], BF16)
    wq_in = P.ext("w_qup", [256, 768])
    wqp_in = P.ext("w_qupp", [256, 768])
    wkv_in = P.ext("w_kvup", [128, 1024])
    wo_in = P.ext("w_out", [D, D])
    ffg = P.ext("ffn_g", [D, FFN])
    ffu = P.ext("ffn_u", [D, FFN])
    ffd = P.ext("ffn_d", [FFN, D])
    nab_in = P.ext("nabias", [128, 7, 8, 128])
    nam_in = P.ext("namask", [NLT, 128, 7, 128])
    rope_in = P.ext("rope", [32, 2, TPC])
    y_out = P.outp("y", [NTOK, D])
    P.load_stream(first)
    P.modulation()
    qtiles = list(range(NT)) if ctx_out else list(range(NLT))

    P.phase()
    QA = P.vb("p (k c) -> p k c", n=4 * NTOK, k=4)
    KW = P.vb("p (k c) -> p k c", n=4 * 896, k=4)
    VW = P.vb("p (s c) -> p s c", n=7 * 640, s=7)
    KAC = P.vb("p (k c) -> p k c", n=4 * 256, k=4)
    VAC = P.vb("p (s c) -> p s c", n=2 * 640, s=2)
    EB = [P.vb("p (h q) -> p h q", n=512, h=4) for _ in range(2)]
    PB_ = [P.vb("p (h q) -> p h q", n=512, h=4) for _ in range(2)]
    MIX = P.vb("p (h c) -> p h c", n=512, h=8)
    MIXT = P.vb("p (k q) -> p k q", n=512, k=4)
    WON = P.vb("p (k c) -> p k c", n=4 * D, k=4)
    NB = P.vb("p (s h q) -> p s h q", n=7 * 8 * 128, s=7, h=8)
    MASK = P.vf("p (s q) -> p s q", n=7 * 128, s=7)
    TS = [P.vf("p (h q) -> p h q", n=512, h=4) for _ in range(2)]
    TMP = P.vf(n=D)
    GBC = P.vf("p (v c) -> p v c", n=2 * D, v=2)
    RC = P.vf(n=8)
    OACC = P.vf("p (h c) -> p h c", n=8 * 66, h=8)
    P.load_gate(GBC, 0)
    add("sp", lambda e: e.dma_start(out=QA[:, :, :], in_=qa_in[:, :, :]), w=["QA"], kind="d")
    add("sp", lambda e: e.dma_start(out=KAC[:, :, :], in_=kext_in[:, :, 22 * 128:22 * 128 + 256]), w=["KAC"], kind="d")
    add("sp", lambda e: e.dma_start(out=VAC[:, :, :], in_=vext_in[:, 22:24, :]), w=["VAC"], kind="d")
    add("pool", lambda e: e.dma_start(out=NB[:, :, :, :], in_=nab_in[:, :, :, :]), w=["NB"], kind="d")
    for pos in range(8):
        hd = 2 * (pos % 4) + pos // 4
        add("pool", lambda e, pos=pos, hd=hd: e.dma_start(out=WON[(pos % 2) * 64:(pos % 2) * 64 + 64, pos // 2, :], in_=wo_in[hd * 64:(hd + 1) * 64, :]), w=["WON"], kind="d")
    gcount = 0
    import os
    _budget = [int(os.environ.get("NABUDGET", "100000000"))]
    _radd = P.S.add

    def add(st, fn, r=(), w=(), kind="c"):
        if _budget[0] <= 0:
            return None
        _budget[0] -= 1
        return _radd(st, fn, r=r, w=w, kind=kind)
    P.add = add
    for T in (qtiles if stop >= 1 else []):
        islat = T < NLT
        keytiles = []
        if islat:
            add("sp", lambda e, T=T: e.dma_start(out=KW[:, :, :], in_=kext_in[:, :, T * 128:(T + 7) * 128]), w=["KW"], kind="d")
            add("sp", lambda e, T=T: e.dma_start(out=VW[:, :, :], in_=vext_in[:, T:T + 7, :]), w=["VW"], kind="d")
            add("sp", lambda e, T=T: e.dma_start(out=MASK[:, :, :], in_=nam_in[T, :, :, :]), w=["MASK"], kind="d")
            keytiles = [("w", s) for s in range(7)]
        keytiles += [("c", 0), ("c", 1)]
        grps = []
        for ki, (kind, s_) in enumerate(keytiles):
            for g in range(2):
                grps.append((ki, kind, s_, g, gcount % 2))
                gcount += 1

        def na_S(grp, T=T):
            ki, kind, s, g, sb_ = grp
            for hh in range(4):
                ch, hp = hh, g * 64
                src, key = (KW, "KW") if kind == "w" else (KAC, "KAC")
                add("pe", lambda e, sb_=sb_, hh=hh, ch=ch, hp=hp, s=s, T=T, src=src: e.matmul(PS[:, sb_, hh * 128:(hh + 1) * 128], lhsT=src[hp:hp + 64, ch, s * 128:(s + 1) * 128],
                                                                                             rhs=QA[hp:hp + 64, ch, T * 128:(T + 1) * 128], start=True, stop=True),
                    r=[key, "QA"], w=[("PS", sb_)])

        def na_E(grp):
            ki, kind, s, g, sb_ = grp
            psv = PS[:, sb_, :].rearrange("p (h q) -> p h q", h=4)
            if kind == "w":
                add("dve", lambda e, sb_=sb_, psv=psv, s=s, g=g: e.scalar_tensor_tensor(out=TS[sb_][:, :, :], in0=psv, scalar=0.125, in1=NB[:, s, 4 * g:4 * g + 4, :],
                                                                                        op0=ALU.mult, op1=ALU.add), r=[("PS", sb_), "NB"], w=[("TS", sb_)])
                add("act", lambda e, sb_=sb_: e.activation(out=EB[sb_][:, :, :], in_=TS[sb_][:, :, :], func=AF.Exp), r=[("TS", sb_)], w=[("EB", sb_)])
                add("dve", lambda e, sb_=sb_, s=s: e.tensor_tensor(out=PB_[sb_][:, :, :], in0=EB[sb_][:, :, :], in1=MASK[:, s:s + 1, :].to_broadcast([128, 4, 128]), op=ALU.mult),
                    r=[("EB", sb_), "MASK"], w=[("PB", sb_)])
            else:
                add("act", lambda e, sb_=sb_, psv=psv: e.activation(out=PB_[sb_][:, :, :], in_=psv, func=AF.Exp, scale=0.125), r=[("PS", sb_)], w=[("PB", sb_)])

        def na_PV(grp):
            ki, kind, s, g, sb_ = grp
            for hh in range(4):
                h = 2 * hh + g
                src, key = (VW, "VW") if kind == "w" else (VAC, "VAC")
                add("pe", lambda e, sb_=sb_, hh=hh, h=h, s=s, src=src: e.matmul(PS[:, 2 + sb_, hh * 66:(hh + 1) * 66], lhsT=PB_[sb_][:, hh, :], rhs=src[:, s, h * 80:h * 80 + 66],
                                                                                   start=True, stop=True), r=[("PB", sb_), key], w=[("PS", 2 + sb_)])
            pov = PS[:, 2 + sb_, 0:264].rearrange("p (h c) -> p h c", h=4)
            if ki == 0:
                add("dve", lambda e, g=g, pov=pov: e.tensor_copy(out=OACC[:, 4 * g:4 * g + 4, :], in_=pov), r=[("PS", 2 + sb_)], w=[("OACC", g)])
            else:
                add("dve", lambda e, g=g, pov=pov: e.tensor_tensor(out=OACC[:, 4 * g:4 * g + 4, :], in0=OACC[:, 4 * g:4 * g + 4, :], in1=pov, op=ALU.add),
                    r=[("PS", 2 + sb_), ("OACC", g)], w=[("OACC", g)])

        na_S(grps[0])
        for n_, grp in enumerate(grps):
            na_E(grp)
            if n_ + 1 < len(grps):
                na_S(grps[n_ + 1])
            na_PV(grp)
        for g in range(2):
            ov = OACC[:, 4 * g:4 * g + 4, :]
            add("dve", lambda e, g=g, ov=ov: e.reciprocal(out=RC[:, 4 * g:4 * g + 4], in_=ov[:, :, 64]), r=[("OACC", g)], w=["RC"])
            add("dve", lambda e, g=g, ov=ov: e.tensor_tensor(out=MIX[:, 4 * g:4 * g + 4, :], in0=ov[:, :, 0:64], in1=RC[:, 4 * g:4 * g + 4].unsqueeze(2).to_broadcast([128, 4, 64]), op=ALU.mult),
                r=[("OACC", g), "RC"], w=["MIX"])
        for c4 in range(4):
            add("pe", lambda e, c4=c4: e.transpose(PST[:, c4 * 128:(c4 + 1) * 128], MIX[:, 2 * c4:2 * c4 + 2, :].rearrange("p h c -> p (h c)"), P.IDB[:, :]),
                r=["MIX", "IDB"], w=["PST"])
        add("act", lambda e: e.activation(out=MIXT[:, :, :], in_=PST[:, 0:512].rearrange("p (k q) -> p k q", k=4), func=AF.Copy), r=["PST"], w=["MIXT"])
        for hf in range(2):
            for c4 in range(4):
                add("pe", lambda e, hf=hf, c4=c4: e.matmul(PS[:, 4 + hf, :], lhsT=MIXT[:, c4, :], rhs=WON[:, c4, hf * 512:(hf + 1) * 512], start=(c4 == 0), stop=(c4 == 3)),
                    r=["MIXT", "WON"], w=[("PS", 4 + hf)])
        P.accum(GBC, TMP, T, 4)

    add = _radd
    P.add = _radd
    P.phase()
    KT = P.vb(n=NKEY)
    VH = P.vb("p (t c) -> p t c", n=NKT * 80, t=NKT)
    QCN = P.vb("p (k c) -> p k c", n=2 * NTOK, k=2)
    QT = P.vb(n=NTOK)
    KVB = [P.vb(n=1024) for _ in range(2)]
    PT = [P.vb(n=512) for _ in range(2)]
    MXT = P.vb(n=512)
    WQ = P.vb("p (k c) -> p k c", n=2 * 768, k=2)
    WQP = P.vb("p (k c) -> p k c", n=2 * 768, k=2)
    WKV = P.vb(n=1024)
    WOH = P.vb(n=D)
    OSB = P.vf(n=512)
    RR = P.vf(n=512)
    T1 = P.vf(n=512)
    T2 = P.vf(n=512)
    TMP = P.vf(n=D)
    GBC = P.vf("p (v c) -> p v c", n=2 * D, v=2)
    ROPE = P.vf("p (a c) -> p a c", n=2 * TPC, a=2)
    P.load_gate(GBC, 0)
    add("sp", lambda e: e.dma_start(out=KT[64:96, :], in_=kr_in[:, :]), w=["KTr"], kind="d")
    add("sp", lambda e: e.dma_start(out=QCN[:, :, :], in_=qcn_in[:, :, :]), w=["QCN"], kind="d")
    add("sp", lambda e: e.dma_start(out=ROPE[64:96, :, :], in_=rope_in[:, :, :]), w=["ROPE"], kind="d")
    add("pool", lambda e: e.dma_start(out=WQ[:, :, :], in_=wq_in.ap().rearrange("(k p) c -> p k c", p=128)), w=["WQ"], kind="d")
    add("pool", lambda e: e.dma_start(out=WQP[:, :, :], in_=wqp_in.ap().rearrange("(k p) c -> p k c", p=128)), w=["WQP"], kind="d")
    add("pool", lambda e: e.dma_start(out=WKV[:, :], in_=wkv_in[:, :]), w=["WKV"], kind="d")
    add("dve", lambda e: e.memset(VH[:, :, :], 1.0), w=["VH"])
    kvblocks = [(k0, min(1024, NKEY - k0)) for k0 in range(0, NKEY, 1024)]
    qblocks = BLOCKS if ctx_out else BLOCKS[:4]
    nkv = 0
    for h in (range(8) if stop >= 2 else []):
        add("pool", lambda e, h=h: e.dma_start(out=WOH[0:64, :], in_=wo_in[512 + h * 64:512 + (h + 1) * 64, :]), w=["WOH"], kind="d")
        for (k0, kw) in kvblocks:
            kb = nkv % 2
            nkv += 1
            add("sp", lambda e, kb=kb, k0=k0, kw=kw: e.dma_start(out=KVB[kb][:, 0:kw], in_=kvn_in[:, k0:k0 + kw]), w=[("KVB", kb)], kind="d")
            for hf in range(0, kw, 512):
                w_ = min(512, kw - hf)
                b = 4 + (hf // 512)
                add("pe", lambda e, kb=kb, hf=hf, w_=w_, b=b, h=h: e.matmul(PS[0:64, b, 0:w_], lhsT=WKV[:, h * 128:h * 128 + 64], rhs=KVB[kb][:, hf:hf + w_], start=True, stop=True),
                    r=["WKV", ("KVB", kb)], w=[("PS", b)])
                add("act", lambda e, k0=k0, hf=hf, w_=w_, b=b: e.activation(out=KT[0:64, k0 + hf:k0 + hf + w_], in_=PS[0:64, b, 0:w_], func=AF.Copy),
                    r=[("PS", b)], w=["KTn"])
            nt_ = kw // 128
            for ti in range(nt_):
                add("pe", lambda e, kb=kb, ti=ti, h=h: e.matmul(PS[:, 6, ti * 64:(ti + 1) * 64], lhsT=KVB[kb][:, ti * 128:(ti + 1) * 128], rhs=WKV[:, h * 128 + 64:h * 128 + 128],
                                                                start=True, stop=True), r=["WKV", ("KVB", kb)], w=[("PS", 6)])
            add("dve", lambda e, k0=k0, nt_=nt_: e.tensor_copy(out=VH[:, k0 // 128:k0 // 128 + nt_, 0:64], in_=PS[:, 6, 0:nt_ * 64].rearrange("p (t c) -> p t c", t=nt_)),
                r=[("PS", 6)], w=["VH"])
        for (b0, bw) in qblocks:
            isctx = b0 >= TPC
            for kc in range(2):
                add("pe", lambda e, kc=kc, b0=b0, bw=bw, h=h: e.matmul(PS[0:96, 0, 0:bw], lhsT=WQ[:, kc, h * 96:(h + 1) * 96], rhs=QCN[:, kc, b0:b0 + bw], start=(kc == 0), stop=(kc == 1)),
                    r=["WQ", "QCN"], w=[("PS", 0)])
            add("act", lambda e, b0=b0, bw=bw: e.activation(out=QT[0:64, b0:b0 + bw], in_=PS[0:64, 0, 0:bw], func=AF.Copy), r=[("PS", 0)], w=["QT"])
            if isctx:
                add("act", lambda e, b0=b0, bw=bw: e.activation(out=QT[64:96, b0:b0 + bw], in_=PS[64:96, 0, 0:bw], func=AF.Copy), r=[("PS", 0)], w=["QT"])
            else:
                for kc in range(2):
                    add("pe", lambda e, kc=kc, b0=b0, bw=bw, h=h: e.matmul(PS[0:96, 1, 0:bw], lhsT=WQP[:, kc, h * 96:(h + 1) * 96], rhs=QCN[:, kc, b0:b0 + bw], start=(kc == 0), stop=(kc == 1)),
                        r=["WQP", "QCN"], w=[("PS", 1)])
                add("dve", lambda e, b0=b0, bw=bw: e.tensor_tensor(out=T1[64:96, 0:bw], in0=PS[64:96, 0, 0:bw], in1=ROPE[64:96, 0, b0:b0 + bw], op=ALU.mult), r=[("PS", 0), "ROPE"], w=["T1"])
                add("dve", lambda e, b0=b0, bw=bw: e.tensor_tensor(out=T2[64:96, 0:bw], in0=PS[64:96, 1, 0:bw], in1=ROPE[64:96, 1, b0:b0 + bw], op=ALU.mult), r=[("PS", 1), "ROPE"], w=["T2"])
                add("dve", lambda e, b0=b0, bw=bw: e.tensor_tensor(out=QT[64:96, b0:b0 + bw], in0=T1[64:96, 0:bw], in1=T2[64:96, 0:bw], op=ALU.add), r=["T1", "T2"], w=["QT"])
        its = [(kt, qb) for kt in range(NKT) for qb in range(4)]

        def emit_S(n):
            kt, qb = its[n]
            sb_ = n % 2
            add("pe", lambda e, sb_=sb_, kt=kt, qb=qb: e.matmul(PS[:, sb_, :], lhsT=KT[0:96, kt * 128:(kt + 1) * 128], rhs=QT[0:96, qb * 512:(qb + 1) * 512], start=True, stop=True),
                r=["KTn", "KTr", "QT"], w=[("PS", sb_)])
        emit_S(0)
        for n, (kt, qb) in enumerate(its):
            sb_ = n % 2
            add("act", lambda e, sb_=sb_: e.activation(out=PT[sb_][:, :], in_=PS[:, sb_, :], func=AF.Exp, scale=SM_MLA), r=[("PS", sb_)], w=[("PT", sb_)])
            if n + 1 < len(its):
                emit_S(n + 1)
            add("pe", lambda e, sb_=sb_, kt=kt, qb=qb: e.matmul(PS[0:66, 2 + qb, :], lhsT=VH[:, kt, 0:66], rhs=PT[sb_][:, :], start=(kt == 0), stop=(kt == NKT - 1)),
                r=["VH", ("PT", sb_)], w=[("PS", 2 + qb)])
        obanks = [(2 + qb, qb * 512, 512) for qb in range(4)]
        for (ob, q0, qw) in obanks + ([(6, TPC, 256)] if ctx_out else []):
            if ob == 6:
                for kt in range(2):
                    sb_ = kt
                    add("pe", lambda e, sb_=sb_, kt=kt: e.matmul(PS[:, sb_, 0:256], lhsT=KT[0:96, kt * 128:(kt + 1) * 128], rhs=QT[0:96, TPC:TPC + 256], start=True, stop=True),
                        r=["KTn", "KTr", "QT"], w=[("PS", sb_)])
                    add("act", lambda e, sb_=sb_: e.activation(out=PT[sb_][:, 0:256], in_=PS[:, sb_, 0:256], func=AF.Exp, scale=SM_MLA), r=[("PS", sb_)], w=[("PT", sb_)])
                    add("pe", lambda e, sb_=sb_, kt=kt: e.matmul(PS[0:66, 6, 0:256], lhsT=VH[:, kt, 0:66], rhs=PT[sb_][:, 0:256], start=(kt == 0), stop=(kt == 1)),
                        r=["VH", ("PT", sb_)], w=[("PS", 6)])
            add("act", lambda e, ob=ob, qw=qw: e.activation(out=OSB[0:64, 0:qw], in_=PS[0:64, ob, 0:qw], func=AF.Copy), r=[("PS", ob)], w=["OSB"])
            add("dve", lambda e, ob=ob, qw=qw: e.reciprocal(out=RR[64:65, 0:qw], in_=PS[64:65, ob, 0:qw]), r=[("PS", ob)], w=["RR"])
            add("pe", lambda e, ob=ob, qw=qw: e.matmul(PS[0:64, ob, 0:qw], lhsT=P.ONF[64:65, 0:64], rhs=RR[64:65, 0:qw], start=True, stop=True),
                r=["RR", "ONF", "OSB"], w=[("PS", ob)])
            add("dve", lambda e, ob=ob, qw=qw: e.tensor_tensor(out=MXT[0:64, 0:qw], in0=OSB[0:64, 0:qw], in1=PS[0:64, ob, 0:qw], op=ALU.mult), r=["OSB", ("PS", ob)], w=["MXT"])
            for ti in range(qw // 128):
                t = q0 // 128 + ti
                for hf in range(2):
                    add("pe", lambda e, ti=ti, hf=hf: e.matmul(PS[:, hf, :], lhsT=MXT[0:64, ti * 128:(ti + 1) * 128], rhs=WOH[0:64, hf * 512:(hf + 1) * 512], start=True, stop=True),
                        r=["MXT", "WOH"], w=[("PS", hf)])
                P.accum(GBC, TMP, t, 0)

    P.phase()
    HT = P.vb("p (k c) -> p k c", n=8 * NTOK, k=8)
    bufs = P.ffn_bufs(2)
    GBC = P.vf("p (v c) -> p v c", n=2 * D, v=2)
    LNV = P.vf("p (v c) -> p v c", n=2 * D, v=2)
    tiles = list(range(NT)) if ctx_out else list(range(NLT))
    if stop >= 3:
        P.layer_norm(LNV, bufs[5][0], 0, tiles, tkey=("TMPA", 0))
    if stop >= 4:
        P.mod_cols(3, 4)
        P.load_gate(GBC, 1)
        P.make_HT(HT, tiles)
        P.swiglu(bufs, HT, GBC, ffg, ffu, ffd, FFN, tiles)
        P.layer_norm(LNV, bufs[5][0], 1, tiles, tkey=("TMPA", 0))
    P.store_stream(y_out)
    return P.finish()


def build_PC(final, ctx_live, stop=9):
    P = Prog(48 * 1024, 9 * 1024)
    add, PS, X = P.add, P.PS, P.X
    halo_in = P.ext("halo", [8, 2, D])
    asame_in = P.ext("a_same", [128, 5, 4, 128])
    aprev_in = P.ext("a_prev", [128, 4, 128])
    anext_in = P.ext("a_next", [128, 4, 128])
    ahp_in = P.ext("a_hp", [8, 4, 128])
    ahn_in = P.ext("a_hn", [8, 4, 128])
    pw_in = P.ext("pool_w", [4, 256, 256])
    rt_in = P.ext("router", [D, NEXP])
    mg = P.ext("moe_g", [NEXP * D, EXPD])
    mu = P.ext("moe_u", [NEXP * D, EXPD])
    md = P.ext("moe_d", [NEXP * EXPD, D])
    y_out = P.outp("y", [NTOK, D])
    P.load_stream(False)
    P.modulation()
    tiles = list(range(NT)) if ctx_live else list(range(NLT))
    P.phase()
    XB = P.vb("p (t c) -> p t c", n=NT * D, t=NT)
    MT = P.vb("p (k c) -> p k c", n=8 * NTOK, k=8)
    WP = P.vb("p (g k d) -> p g k d", n=4 * 2 * 256, g=4, k=2)
    ASAME = P.vb("p (a g q) -> p a g q", n=5 * 4 * 128, a=5, g=4)
    APREV = P.vb("p (g q) -> p g q", n=512, g=4)
    ANEXT = P.vb("p (g q) -> p g q", n=512, g=4)
    AHP = P.vb("p (g q) -> p g q", n=512, g=4)
    AHN = P.vb("p (g q) -> p g q", n=512, g=4)
    HALO = P.vb("p (a c) -> p a c", n=2 * D, a=2)
    GBC = P.vf("p (v c) -> p v c", n=2 * D, v=2)
    PSC = P.vf(n=D)
    TMP = P.vf(n=D)
    LNV = P.vf("p (v c) -> p v c", n=2 * D, v=2)
    P.mod_cols(0, 1)
    P.load_gate(GBC, 0)
    add("sp", lambda e: e.dma_start(out=PSC[:, :], in_=P.ln_in[4:5, :].to_broadcast([128, D])), w=["PSC"], kind="d")
    add("pool", lambda e: e.dma_start(out=ASAME[:, :, :, :], in_=asame_in[:, :, :, :]), w=["ATAB"], kind="d")
    add("pool", lambda e: e.dma_start(out=APREV[:, :, :], in_=aprev_in[:, :, :]), w=["ATAB"], kind="d")
    add("pool", lambda e: e.dma_start(out=ANEXT[:, :, :], in_=anext_in[:, :, :]), w=["ATAB"], kind="d")
    add("pool", lambda e: e.dma_start(out=AHP[0:8, :, :], in_=ahp_in[:, :, :]), w=["ATAB"], kind="d")
    add("pool", lambda e: e.dma_start(out=AHN[0:8, :, :], in_=ahn_in[:, :, :]), w=["ATAB"], kind="d")
    add("pool", lambda e: e.dma_start(out=HALO[0:8, :, :], in_=halo_in[:, :, :]), w=["ATAB"], kind="d")
    add("pool", lambda e: e.dma_start(out=WP[:, :, :, :], in_=pw_in.ap().rearrange("g (k p) d -> p g k d", p=128)), w=["WP"], kind="d")
    for t in tiles:
        add("act", lambda e, t=t: e.activation(out=XB[:, t, :], in_=X[:, t, :], func=AF.Copy), r=[("X", t)], w=[("XB", t)])
    nb = 0
    for t in tiles:
        for half in range(2):
            b = nb % 2
            nb += 1
            for q in range(4):
                kc = half * 4 + q
                g = kc // 2
                cs = slice(kc * 128, (kc + 1) * 128)
                if t < NLT:
                    cls = 1 if t == 0 else (2 if t == NLT - 1 else 0)
                    srcs = [(XB[:, t, cs], ASAME[:, cls, g, :], ("XB", t))]
                    srcs.append((XB[:, t - 1, cs], APREV[:, g, :], ("XB", t - 1)) if t > 0 else (HALO[0:8, 0, cs], AHP[0:8, g, :], "ATAB"))
                    srcs.append((XB[:, t + 1, cs], ANEXT[:, g, :], ("XB", t + 1)) if t < NLT - 1 else (HALO[0:8, 1, cs], AHN[0:8, g, :], "ATAB"))
                elif t == NLT:
                    srcs = [(XB[:, t, cs], ASAME[:, 3, g, :], ("XB", t)), (XB[:, t + 1, cs], ANEXT[:, g, :], ("XB", t + 1))]
                else:
                    srcs = [(XB[:, t, cs], ASAME[:, 4, g, :], ("XB", t)), (XB[:, t - 1, cs], APREV[:, g, :], ("XB", t - 1))]
                for si, (l_, r_, key) in enumerate(srcs):
                    add("pe", lambda e, b=b, q=q, l_=l_, r_=r_, si=si, ns=len(srcs): e.matmul(PS[:, b, q * 128:(q + 1) * 128], lhsT=l_, rhs=r_, start=(si == 0), stop=(si == ns - 1)),
                        r=[key, "ATAB"], w=[("PS", b)])
            v = 0 if t < NLT else 1
            for q in range(4):
                kc = half * 4 + q
                add("act", lambda e, t=t, kc=kc, b=b, q=q, v=v: e.activation(out=MT[:, kc, t * 128:(t + 1) * 128], in_=PS[:, b, q * 128:(q + 1) * 128],
                                                                              func=AF.Copy, scale=P.SCL[:, v, kc:kc + 1]), r=[("PS", b), "SCL"], w=[("MT", t)])
    for t in tiles:
        for g in range(4):
            for k in range(2):
                add("pe", lambda e, t=t, g=g, k=k: e.matmul(PS[:, 2 + g // 2, (g % 2) * 256:(g % 2 + 1) * 256], lhsT=MT[:, 2 * g + k, t * 128:(t + 1) * 128], rhs=WP[:, g, k, :],
                                                            start=(k == 0), stop=(k == 1)), r=[("MT", t), "WP"], w=[("PS", 2 + g // 2)])
        v = 0 if t < NLT else 1
        src = PS[:, 2:4, :].rearrange("p a b -> p (a b)")
        add("dve", lambda e, v=v, src=src, G=GBC, T_=TMP: e.tensor_tensor(out=T_[:, :], in0=src, in1=G[:, v, :], op=ALU.mult), r=[("PS", 2), ("PS", 3), ("GBC", v)], w=["TMPA"])
        add("dve", lambda e, T_=TMP, P_=PSC: e.tensor_tensor(out=T_[:, :], in0=T_[:, :], in1=P_[:, :], op=ALU.mult), r=["TMPA", "PSC"], w=["TMPA"])
        add("pool", lambda e, t=t, T_=TMP: e.tensor_tensor(out=X[:, t, :], in0=X[:, t, :], in1=T_[:, :], op=ALU.add), r=[("X", t), "TMPA"], w=[("X", t)])
    if stop >= 2:
        P.layer_norm(LNV, TMP, 0, tiles)
    P.phase()
    HT = P.vb("p (k c) -> p k c", n=8 * NTOK, k=8)
    bufs = P.ffn_bufs(2)
    GBC = P.vf("p (v c) -> p v c", n=2 * D, v=2)
    LNV = P.vf("p (v c) -> p v c", n=2 * D, v=2)
    GATES = P.vf("p (t e) -> p t e", n=NT * 8, t=NT)
    H32 = P.vf("p (k c) -> p k c", n=8 * 128, k=8)
    RT = P.vf("p (k e) -> p k e", n=64, k=8)
    LG = P.vf(n=8)
    L2 = P.vf(n=8)
    E1 = P.vf(n=8)
    E2 = P.vf(n=8)
    MS = P.vf(n=8)
    if stop >= 3:
        P.mod_cols(3, 4)
        P.load_gate(GBC, 1)
        add("sp", lambda e: e.dma_start(out=RT[:, :, :], in_=rt_in.ap().rearrange("(k p) e -> p k e", p=128)), w=["RT"], kind="d")
        for t in tiles:
            v = 0 if t < NLT else 1
            for half in range(2):
                b = half
                for q in range(4):
                    kc = half * 4 + q
                    add("pe", lambda e, t=t, kc=kc, b=b, q=q: e.transpose(PS[:, b, q * 128:(q + 1) * 128], X[:, t, kc * 128:(kc + 1) * 128], P.IDF[:, :]),
                        r=[("X", t), "IDF"], w=[("PS", b)])
                for q in range(4):
                    kc = half * 4 + q
                    add("act", lambda e, t=t, kc=kc, b=b, q=q, v=v: e.activation(out=HT[:, kc, t * 128:(t + 1) * 128], in_=PS[:, b, q * 128:(q + 1) * 128],
                                                                                  func=AF.Identity, bias=P.SHF[:, v, kc:kc + 1], scale=P.SCL[:, v, kc:kc + 1]),
                        r=[("PS", b), "SCL", "SHF"], w=[("HT", t)])
                    add("act", lambda e, kc=kc, b=b, q=q, v=v: e.activation(out=H32[:, kc, :], in_=PS[:, b, q * 128:(q + 1) * 128],
                                                                            func=AF.Identity, bias=P.SHF[:, v, kc:kc + 1], scale=P.SCL[:, v, kc:kc + 1]),
                        r=[("PS", b), "SCL", "SHF"], w=["H32"])
            for kc in range(8):
                add("pe", lambda e, kc=kc: e.matmul(PS[:, 6, 0:8], lhsT=H32[:, kc, :], rhs=RT[:, kc, :], start=(kc == 0), stop=(kc == 7)), r=["H32", "RT"], w=[("PS", 6)])
            add("dve", lambda e: e.tensor_copy(out=LG[:, :], in_=PS[:, 6, 0:8]), r=[("PS", 6)], w=["LG"])
            add("dve", lambda e: e.tensor_reduce(out=MS[:, 0:1], in_=LG[:, :], axis=AX.X, op=ALU.max), r=["LG"], w=["MS"])
            add("dve", lambda e: e.tensor_scalar(out=E1[:, :], in0=LG[:, :], scalar1=MS[:, 0:1], scalar2=None, op0=ALU.is_equal), r=["LG", "MS"], w=["E1"])
            add("dve", lambda e: e.scalar_tensor_tensor(out=L2[:, :], in0=E1[:, :], scalar=-1e30, in1=LG[:, :], op0=ALU.mult, op1=ALU.add), r=["E1", "LG"], w=["L2"])
            add("dve", lambda e: e.tensor_reduce(out=MS[:, 1:2], in_=L2[:, :], axis=AX.X, op=ALU.max), r=["L2"], w=["MS"])
            add("dve", lambda e: e.tensor_scalar(out=E2[:, :], in0=L2[:, :], scalar1=MS[:, 1:2], scalar2=None, op0=ALU.is_equal), r=["L2", "MS"], w=["E2"])
            add("dve", lambda e: e.tensor_tensor(out=MS[:, 2:3], in0=MS[:, 1:2], in1=MS[:, 0:1], op=ALU.subtract), r=["MS"], w=["MS"])
            add("act", lambda e: e.activation(out=MS[:, 3:4], in_=MS[:, 2:3], func=AF.Exp), r=["MS"], w=["MS"])
            add("dve", lambda e: e.tensor_scalar(out=MS[:, 4:5], in0=MS[:, 3:4], scalar1=1.0, scalar2=None, op0=ALU.add), r=["MS"], w=["MS"])
            add("dve", lambda e: e.reciprocal(out=MS[:, 5:6], in_=MS[:, 4:5]), r=["MS"], w=["MS"])
            add("dve", lambda e: e.tensor_tensor(out=MS[:, 6:7], in0=MS[:, 3:4], in1=MS[:, 5:6], op=ALU.mult), r=["MS"], w=["MS"])
            add("dve", lambda e: e.tensor_scalar(out=E1[:, :], in0=E1[:, :], scalar1=MS[:, 5:6], scalar2=None, op0=ALU.mult), r=["E1", "MS"], w=["E1"])
            add("dve", lambda e, t=t: e.scalar_tensor_tensor(out=GATES[:, t, :], in0=E2[:, :], scalar=MS[:, 6:7], in1=E1[:, :], op0=ALU.mult, op1=ALU.add),
                r=["E2", "E1", "MS"], w=["GATES"])
        cnt = 0
        for ex in (range(NEXP) if stop >= 4 else []):
            cnt = P.swiglu(bufs, HT, GBC, mg, mu, md, EXPD, tiles, e_off=ex, gates=lambda t, ex=ex: (GATES[:, t, ex:ex + 1], "GATES"), cnt0=cnt)
        P.layer_norm(LNV, bufs[5][0], 1, tiles, final=final, tkey=("TMPA", 0))
    P.store_stream(y_out)
    return P.finish()


_PROGS = {}


def _prog(key, fn):
    if key not in _PROGS:
        _PROGS[key] = fn()
    return _PROGS[key]


def _rope_perm():
    d = np.arange(32)
    dd = d % 16
    return np.where(dd < 8, d + 8, d - 8)


def _rope_tables(c):
    t = c * TPC + np.arange(TPC)
    row = (t // 64).astype(np.float32)
    col = (t % 64).astype(np.float32)
    inv = (1.0 / (np.float32(10000.0) ** (np.arange(8, dtype=np.float32) / np.float32(8)))).astype(np.float32)
    tab = np.zeros((32, 2, TPC), np.float32)
    for d in range(32):
        pos = row if d < 16 else col
        dd = d % 16
        ang = (pos * inv[dd % 8]).astype(np.float32)
        tab[d, 0] = np.cos(ang)
        tab[d, 1] = -np.sin(ang) if dd < 8 else np.sin(ang)
    return tab


def _na_bias_table(rel_bias):
    kr, kc = np.divmod(np.arange(128), 64)
    out = np.zeros((128, 7, 8, 128), np.float32)
    for s in range(7):
        drow = 2 * (s - 3) + kr[:, None] - kr[None, :] + 7
        dcol = kc[:, None] - kc[None, :] + 15
        ok = (drow >= 0) & (drow < 15) & (dcol >= 0) & (dcol < 31)
        g = rel_bias[:, np.clip(drow, 0, 14), np.clip(dcol, 0, 30)]
        out[:, s] = np.where(ok[None], g, 0.0).transpose(1, 0, 2)
    return out


def _na_mask(c):
    kr, kc = np.divmod(np.arange(128), 64)
    m = np.zeros((NLT, 128, 7, 128), np.float32)
    for T in range(NLT):
        qrow = 32 * c + 2 * T + kr
        rs = np.clip(qrow - 4, 0, 256 - 8)
        cs = np.clip(kc - 8, 0, 64 - 16)
        for s in range(7):
            ktg = 16 * c + T + s - 3
            if ktg < 0 or ktg >= S // 128:
                continue
            krow = 2 * ktg + kr
            ok = (krow[:, None] >= rs[None, :]) & (krow[:, None] < rs[None, :] + 8) & (kc[:, None] >= cs[None, :]) & (kc[:, None] < cs[None, :] + 16)
            m[T, :, s, :] = ok
    return m


def _common(inp, i, xs):
    maps = []
    for c in range(NCORE):
        m = {}
        m["x"] = xs[c]
        m["cvec"] = np.ascontiguousarray(np.stack([inp["c"][0], inp["c_ctx"]]).reshape(2, 8, 128).transpose(2, 0, 1))
        m["mod_w"] = np.ascontiguousarray(inp["mod_w"][i])
        m["mod_b"] = np.ascontiguousarray(inp["mod_b"][i][None])
        m["mod_bc"] = np.ascontiguousarray(inp["mod_b"][i].reshape(48, 128).T)
        m["ln"] = np.ascontiguousarray(np.stack([inp["ln1_g"][i], inp["ln1_b"][i], inp["ln2_g"][i], inp["ln2_b"][i], inp["pool_scale"][i // 2]]))
        m["ident"] = np.eye(128, dtype=np.float32)
        maps.append(m)
    return maps


def run_PA(inp, i, xs, first):
    j = i // 2
    nc = _prog(("PA", first), lambda: build_PA(first))
    maps = _common(inp, i, xs)
    perm = _rope_perm()
    w_in = np.ascontiguousarray(inp["attn_w_in"][j])
    wkr = np.zeros((D, 96), np.float32)
    wkr[:, 64:] = w_in[:, 1920:1952]
    wkrp = np.zeros((D, 96), np.float32)
    wkrp[:, 64:] = w_in[:, 1920:1952][:, perm]
    nrm = np.ascontiguousarray(np.stack([inp["mla_q_norm"][j][:128], inp["mla_q_norm"][j][128:], inp["mla_kv_norm"][j]], axis=1))
    for c, m in enumerate(maps):
        m.update(w_in=w_in, wkr=wkr, wkrp=wkrp, nrm=nrm, rope=_rope_tables(c))
    return run_bass_kernel_spmd(nc, maps, core_ids=list(range(NCORE))).results


def pb_maps(inp, i, xs, pa):
    j = i // 2
    maps = _common(inp, i, xs)
    perm = _rope_perm()
    wq = np.ascontiguousarray(inp["mla_w_q_up"][j])
    wq3 = wq.reshape(256, 8, 96)
    wqp = np.zeros((256, 8, 96), np.float32)
    wqp[:, :, 64:] = wq3[:, :, 64:][:, :, perm]
    nab = np.ascontiguousarray(_na_bias_table(inp["na_rel_bias"][j])[:, :, [0, 2, 4, 6, 1, 3, 5, 7], :])
    bf = ml_dtypes.bfloat16
    kvn_all = np.concatenate([pa[0]["kvn"][:, TPC:]] + [pa[c]["kvn"][:, :TPC] for c in range(NCORE)], axis=1)
    kr_all = np.concatenate([pa[0]["kr"][:, TPC:]] + [pa[c]["kr"][:, :TPC] for c in range(NCORE)], axis=1)
    for c, m in enumerate(maps):
        ka, va = pa[c]["ka"], pa[c]["va"]
        zk = np.zeros((128, 4, 384), bf)
        zv = np.zeros((128, 3, 640), bf)
        kprev = pa[c - 1]["ka"][:, :, TPC - 384:TPC] if c > 0 else zk
        knext = pa[c + 1]["ka"][:, :, 0:384] if c < NCORE - 1 else zk
        vprev = pa[c - 1]["va"][:, NLT - 3:NLT] if c > 0 else zv
        vnext = pa[c + 1]["va"][:, 0:3] if c < NCORE - 1 else zv
        m["kext"] = np.ascontiguousarray(np.concatenate([kprev, ka[:, :, :TPC], knext, ka[:, :, TPC:]], axis=2))
        m["vext"] = np.ascontiguousarray(np.concatenate([vprev, va[:, :NLT], vnext, va[:, NLT:]], axis=1))
        m["qa"] = pa[c]["qa"]
        m["qcn"] = pa[c]["qcn"]
        m["kvn_all"] = np.ascontiguousarray(kvn_all)
        m["kr_all"] = np.ascontiguousarray(kr_all)
        m["w_qup"] = wq
        m["w_qupp"] = np.ascontiguousarray(wqp.reshape(256, 768))
        m["w_kvup"] = np.ascontiguousarray(inp["mla_w_kv_up"][j])
        m["w_out"] = np.ascontiguousarray(inp["attn_w_out"][j])
        m["ffn_g"] = np.ascontiguousarray(inp["ffn_w_gate"][j])
        m["ffn_u"] = np.ascontiguousarray(inp["ffn_w_up"][j])
        m["ffn_d"] = np.ascontiguousarray(inp["ffn_w_down"][j])
        m["nabias"] = nab
        m["namask"] = _na_mask(c)
        m["rope"] = _rope_tables(c)
    return maps


def run_PB(inp, i, xs, pa, first, ctx_out):
    nc = _prog(("PB", first, ctx_out), lambda: build_PB(first, ctx_out))
    return run_bass_kernel_spmd(nc, pb_maps(inp, i, xs, pa), core_ids=list(range(NCORE))).results


def _pool_tables(c):
    halves = (1, 2, 4, 8)
    p = np.arange(128)
    same = np.zeros((128, 5, 4, 128), np.float32)
    prev = np.zeros((128, 4, 128), np.float32)
    nxt = np.zeros((128, 4, 128), np.float32)
    hp = np.zeros((8, 4, 128), np.float32)
    hn = np.zeros((8, 4, 128), np.float32)
    eye = np.eye(128, dtype=np.float32)
    for g, h in enumerate(halves):
        src, dst = p[:, None], p[None, :]
        inwin = (src >= dst - h) & (src < dst + h)
        gen = inwin / np.float32(2 * h) - eye
        cnt_first = (np.minimum(p + h, 128 + h) - np.maximum(p - h, 0)).astype(np.float32)
        first = inwin / cnt_first[None, :] - eye
        cnt_last = (np.minimum(p + h, 128) - (p - h)).astype(np.float32)
        last = inwin / cnt_last[None, :] - eye
        same[:, 0, g] = gen
        same[:, 1, g] = first if c == 0 else gen
        same[:, 2, g] = last if c == NCORE - 1 else gen
        same[:, 3, g] = first
        same[:, 4, g] = last
        prev[:, g] = ((src - 128) >= dst - h) / np.float32(2 * h)
        nxt[:, g] = ((src + 128) < dst + h) / np.float32(2 * h)
        j = np.arange(8)[:, None]
        hp[:, g] = ((j - 8) >= dst - h) / np.float32(2 * h)
        hn[:, g] = ((j + 128) < dst + h) / np.float32(2 * h)
    return dict(a_same=same, a_prev=prev, a_next=nxt, a_hp=hp, a_hn=hn)


def pc_maps(inp, i, xs):
    j = i // 2
    maps = _common(inp, i, xs)
    for c, m in enumerate(maps):
        halo = np.zeros((8, 2, D), np.float32)
        if c > 0:
            halo[:, 0] = xs[c - 1][TPC - 8:TPC]
        if c < NCORE - 1:
            halo[:, 1] = xs[c + 1][0:8]
        m["halo"] = halo
        m.update(_pool_tables(c))
        m["pool_w"] = np.ascontiguousarray(inp["pool_w"][j])
        m["router"] = np.ascontiguousarray(inp["moe_router"][j])
        m["moe_g"] = inp["moe_w_gate"][j].reshape(NEXP * D, EXPD)
        m["moe_u"] = inp["moe_w_up"][j].reshape(NEXP * D, EXPD)
        m["moe_d"] = inp["moe_w_down"][j].reshape(NEXP * EXPD, D)
    return maps


def run_PC(inp, i, xs, final, ctx_live):
    nc = _prog(("PC", final, ctx_live), lambda: build_PC(final, ctx_live))
    return run_bass_kernel_spmd(nc, pc_maps(inp, i, xs), core_ids=list(range(NCORE))).results


def kernel(**inp):
    inp = {k: np.asarray(v) for k, v in inp.items()}
    xs = [np.ascontiguousarray(np.concatenate([inp["x"][0, c * TPC:(c + 1) * TPC], inp["ctx"][0]], axis=0)) for c in range(NCORE)]
    pa = run_PA(inp, 0, xs, True)
    res = run_PB(inp, 0, xs, pa, True, True)
    xs = [res[c]["y"] for c in range(NCORE)]
    res = run_PC(inp, 1, xs, False, True)
    xs = [res[c]["y"] for c in range(NCORE)]
    pa = run_PA(inp, 2, xs, False)
    res = run_PB(inp, 2, xs, pa, False, False)
    xs = [res[c]["y"] for c in range(NCORE)]
    res = run_PC(inp, 3, xs, True, False)
    out = np.concatenate([res[c]["y"][:TPC] for c in range(NCORE)], axis=0)
    return out[None].astype(np.float32)
```

```python
import numpy as np
import ml_dtypes
from contextlib import ExitStack
import concourse.bass as bass
import concourse.mybir as mybir
from concourse.bass_utils import run_bass_kernel_spmd

F32 = mybir.dt.float32
BF16 = mybir.dt.bfloat16
AF = mybir.ActivationFunctionType
ALU = mybir.AluOpType
AX = mybir.AxisListType

NCORE = 8
D = 1024
S = 16384
TPC = S // NCORE
NLT = TPC // 128
NCT = 2
NT = NLT + NCT
NTOK = NT * 128
DEPTH = 4
ALPHA = (2 * DEPTH) ** 0.25
FFN = 2816
EXPD = 3584
NEXP = 8
LN_EPS = 1e-5
RMS_EPS = 1e-6
NKEY = 256 + S
NKT = NKEY // 128
SM_MLA = 96 ** -0.5
BLOCKS = [(0, 512), (512, 512), (1024, 512), (1536, 512), (2048, 256)]


class Sched:
    def __init__(self):
        self.ops = []
        self.lastw = {}
        self.readers = {}
        self.bar = set()

    def add(self, st, fn, r=(), w=(), kind="c"):
        i = len(self.ops)
        deps = set(self.bar)
        for k in r:
            if k in self.lastw:
                deps.add(self.lastw[k])
        for k in w:
            if k in self.lastw:
                deps.add(self.lastw[k])
            for j in self.readers.get(k, ()):
                deps.add(j)
        self.ops.append(dict(st=st, fn=fn, kind=kind, deps=deps, inc=False, sv=None))
        for k in r:
            self.readers.setdefault(k, []).append(i)
        for k in w:
            self.lastw[k] = i
            self.readers[k] = []
        return i

    def barrier(self):
        last = {}
        dl = {}
        for i, o in enumerate(self.ops):
            if o["kind"] == "c":
                last[o["st"]] = i
            else:
                dl.setdefault(o["st"], []).append(i)
        self.bar = set(last.values())
        for lst in dl.values():
            self.bar.update(lst[-8:])

    def emit(self, nc, es):
        ops = self.ops
        NP = 8
        streams = ["pe", "act", "dve", "pool", "sp"]
        dma_idx = {s: [] for s in streams}
        for i, o in enumerate(ops):
            if o["kind"] == "d":
                lst = dma_idx[o["st"]]
                if len(lst) >= NP:
                    o["deps"].add(lst[-NP])
                lst.append(i)
            best = {}
            keep = []
            for j in o["deps"]:
                p = ops[j]
                if p["kind"] == "c":
                    if p["st"] == "pe" and o["st"] == "pe" and o["kind"] == "c":
                        continue
                    if p["st"] not in best or best[p["st"]] < j:
                        best[p["st"]] = j
                else:
                    keep.append(j)
            for j in best.values():
                ops[j]["inc"] = True
                keep.append(j)
            o["deps"] = keep
        csem = {s: es.enter_context(nc.semaphore("c_" + s)) for s in streams}
        dsem = {s: [es.enter_context(nc.semaphore("d_%s%d" % (s, k))) for k in range(NP)] for s in ("pool", "sp")}
        cnt = {s: 0 for s in streams}
        dcnt = {s: 0 for s in streams}
        for o in ops:
            if o["kind"] == "c":
                if o["inc"]:
                    cnt[o["st"]] += 1
                    o["sv"] = (csem[o["st"]], cnt[o["st"]], 1)
            else:
                k = dcnt[o["st"]]
                dcnt[o["st"]] += 1
                o["sv"] = (dsem[o["st"]][k % NP], 16 * (k // NP + 1), 16)
        bystream = {s: [o for o in ops if o["st"] == s] for s in streams}

        def run(eng, st):
            lst = bystream[st]
            waited = {}
            for o in lst:
                for j in o["deps"]:
                    sem, val, _ = ops[j]["sv"]
                    key = id(sem)
                    if waited.get(key, 0) < val:
                        eng.wait_ge(sem, val)
                        waited[key] = val
                ins = o["fn"](eng)
                if o["sv"] is not None:
                    ins.then_inc(o["sv"][0], o["sv"][2])
            if st in dsem:
                k = dcnt[st]
                for q in range(NP):
                    n = (k - q + NP - 1) // NP if k > q else 0
                    if n > 0:
                        eng.wait_ge(dsem[st][q], 16 * n)

        with nc.Block() as block:
            @block.tensor
            def _(e):
                run(e, "pe")

            @block.scalar
            def _(e):
                run(e, "act")

            @block.vector
            def _(e):
                run(e, "dve")

            @block.gpsimd
            def _(e):
                run(e, "pool")

            @block.sync
            def _(e):
                run(e, "sp")


class Prog:
    def __init__(self, arb_elems, arf_elems):
        self.nc = nc = bass.Bass("TRN2", target_bir_lowering=False)
        self.es = es = ExitStack()
        self.S = Sched()
        self.add = self.S.add
        self.x_in = self.ext("x", [NTOK, D])
        self.cvec_in = self.ext("cvec", [128, 2, 8])
        self.modw_in = self.ext("mod_w", [D, 6 * D])
        self.modb_in = self.ext("mod_b", [1, 6 * D])
        self.modbc_in = self.ext("mod_bc", [128, 48])
        self.ln_in = self.ext("ln", [5, D])
        self.ident_in = self.ext("ident", [128, 128])
        self.grow = nc.dram_tensor("grow", [2, 2, D], F32)
        self.X = self.sb("X", [128, NT, D], F32)
        self.ARB = self.sb("ARB", [128, arb_elems], BF16)
        self.ARF = self.sb("ARF", [128, arf_elems], F32)
        self.MCOL = self.sb("MCOL", [128, 2, 48], F32)
        self.MBCOL = self.sb("MBCOL", [128, 48], F32)
        self.SC = self.sb("SC", [128, 2, 8], F32)
        self.SCB = self.sb("SCB", [128, 8, 2], BF16)
        self.IDF = self.sb("IDF", [128, 128], F32)
        self.IDB = self.sb("IDB", [128, 128], BF16)
        self.ONB = self.sb("ONB", [128, 128], BF16)
        self.ONF = self.sb("ONF", [128, 64], F32)
        self.STAT = self.sb("STAT", [128, 8], F32)
        self.SCL = self.sb("SCL", [128, 2, 8], F32)
        self.SHF = self.sb("SHF", [128, 2, 8], F32)
        self.PS = es.enter_context(nc.psum_tensor("PS", [128, 8, 512], F32))
        self.bo = 0
        self.fo = 0
        add = self.add
        add("sp", lambda e: e.dma_start(out=self.IDF[:, :], in_=self.ident_in[:, :]), w=["IDF"], kind="d")
        add("dve", lambda e: e.tensor_copy(out=self.IDB[:, :], in_=self.IDF[:, :]), r=["IDF"], w=["IDB"])
        add("dve", lambda e: e.memset(self.ONB[:, :], 1.0), w=["ONB"])
        add("dve", lambda e: e.memset(self.ONF[:, :], 1.0), w=["ONF"])

    def ext(self, name, shape, dt=F32):
        return self.nc.dram_tensor(name, list(shape), dt, kind="ExternalInput")

    def outp(self, name, shape, dt=F32):
        return self.nc.dram_tensor(name, list(shape), dt, kind="ExternalOutput")

    def sb(self, name, shape, dt):
        return self.es.enter_context(self.nc.sbuf_tensor(name, list(shape), dt))

    def phase(self):
        self.S.barrier()
        self.bo = 0
        self.fo = 0

    def vb(self, pattern=None, n=None, **kw):
        v = self.ARB[:, self.bo:self.bo + n]
        self.bo += n
        assert self.bo <= self.ARB.shape[1], ("ARB overflow", self.bo)
        return v.rearrange(pattern, **kw) if pattern else v

    def vf(self, pattern=None, n=None, **kw):
        v = self.ARF[:, self.fo:self.fo + n]
        self.fo += n
        assert self.fo <= self.ARF.shape[1], ("ARF overflow", self.fo)
        return v.rearrange(pattern, **kw) if pattern else v

    def load_stream(self, first):
        X, add = self.X, self.add
        for t in range(NT):
            add("sp", lambda e, t=t: e.dma_start(out=X[:, t, :], in_=self.x_in[t * 128:(t + 1) * 128, :]), w=[("X", t)], kind="d")
            if first:
                add("dve", lambda e, t=t: e.tensor_scalar(out=X[:, t, :], in0=X[:, t, :], scalar1=ALPHA, scalar2=None, op0=ALU.mult),
                    r=[("X", t)], w=[("X", t)])

    def store_stream(self, y_out):
        for t in range(NT):
            self.add("sp", lambda e, t=t: e.dma_start(out=y_out[t * 128:(t + 1) * 128, :], in_=self.X[:, t, :]), r=[("X", t)], kind="d")

    def modulation(self):
        add, PS = self.add, self.PS
        SC, SCB, MCOL, MBCOL = self.SC, self.SCB, self.MCOL, self.MBCOL
        self.phase()
        MW = self.vb("p (k c) -> p k c", n=8 * 512, k=8)
        SCBC = self.vb("p (v k m) -> p v k m", n=2 * 8 * 128, v=2, k=8)
        GR = self.vf(n=512)
        MBR = self.vf(n=2 * D)
        mw = self.modw_in
        add("sp", lambda e: e.dma_start(out=SC[:, :, :], in_=self.cvec_in[:, :, :]), w=["SC"], kind="d")
        add("act", lambda e: e.activation(out=SC[:, :, :], in_=SC[:, :, :], func=AF.Silu), r=["SC"], w=["SC"])
        add("dve", lambda e: e.tensor_copy(out=SCB[:, :, :], in_=SC[:, :, :].rearrange("p v k -> p k v")), r=["SC"], w=["SCB"])
        for v in range(2):
            add("dve", lambda e, v=v: e.tensor_copy(out=SCBC[:, v, :, :], in_=SC[:, v, :].unsqueeze(2).to_broadcast([128, 8, 128])),
                r=["SC"], w=[("SCBC", v)])
        add("sp", lambda e: e.dma_start(out=MBCOL[:, :], in_=self.modbc_in[:, :]), w=["MBCOL"], kind="d")
        for cb in range(12):
            add("pool", lambda e, cb=cb: e.dma_start(out=MW[:, :, :], in_=mw.ap().rearrange("(k p) c -> p k c", p=128)[:, :, cb * 512:(cb + 1) * 512]),
                w=["MW"], kind="d")
            for jj in range(4):
                for kc in range(8):
                    add("pe", lambda e, jj=jj, kc=kc: e.matmul(PS[:, 6, jj * 2:jj * 2 + 2], lhsT=MW[:, kc, jj * 128:(jj + 1) * 128], rhs=SCB[:, kc, :],
                                                             start=(kc == 0), stop=(kc == 7)),
                        r=["MW", "SCB"], w=[("PS", 6)])
            add("dve", lambda e, cb=cb: e.tensor_tensor(out=MCOL[:, :, cb * 4:(cb + 1) * 4].rearrange("p v j -> p j v"),
                                                       in0=PS[:, 6, 0:8].rearrange("p (j v) -> p j v", v=2),
                                                       in1=MBCOL[:, cb * 4:(cb + 1) * 4].unsqueeze(2).to_broadcast([128, 4, 2]), op=ALU.add),
                r=[("PS", 6), "MBCOL"], w=["MCOL"])
        for gi, which in enumerate((2, 5)):
            add("sp", lambda e, which=which: e.dma_start(out=MBR[0:1, 0:D], in_=self.modb_in[0:1, which * D:(which + 1) * D]), w=["MBR"], kind="d")
            for hf in range(2):
                add("pool", lambda e, which=which, hf=hf: e.dma_start(out=MW[:, :, :], in_=mw.ap().rearrange("(k p) c -> p k c", p=128)[:, :, which * D + hf * 512: which * D + (hf + 1) * 512]),
                    w=["MW"], kind="d")
                for v in range(2):
                    for kc in range(8):
                        add("pe", lambda e, v=v, kc=kc: e.matmul(PS[:, 5, :], lhsT=SCBC[:, v, kc, :], rhs=MW[:, kc, :], start=(kc == 0), stop=(kc == 7)),
                            r=["MW", ("SCBC", v)], w=[("PS", 5)])
                    add("dve", lambda e, hf=hf: e.tensor_tensor(out=GR[0:1, :], in0=PS[0:1, 5, :], in1=MBR[0:1, hf * 512:(hf + 1) * 512], op=ALU.add),
                        r=[("PS", 5), "MBR"], w=["GR"])
                    add("sp", lambda e, gi=gi, v=v, hf=hf: e.dma_start(out=self.grow[gi, v:v + 1, hf * 512:(hf + 1) * 512], in_=GR[0:1, :]),
                        r=["GR"], w=[("grow", gi)], kind="d")

    def load_gate(self, GBC, gi):
        for v in range(2):
            self.add("sp", lambda e, v=v: e.dma_start(out=GBC[:, v, :], in_=self.grow[gi, v:v + 1, :].to_broadcast([128, D])),
                     r=[("grow", gi)], w=[("GBC", v)], kind="d")

    def mod_cols(self, shift_idx, scale_idx):
        self.add("dve", lambda e: e.tensor_scalar(out=self.SCL[:, :, :], in0=self.MCOL[:, :, scale_idx * 8:(scale_idx + 1) * 8], scalar1=1.0, scalar2=1.0 / ALPHA,
                                                  op0=ALU.add, op1=ALU.mult), r=["MCOL"], w=["SCL"])
        self.add("dve", lambda e: e.tensor_copy(out=self.SHF[:, :, :], in_=self.MCOL[:, :, shift_idx * 8:(shift_idx + 1) * 8]), r=["MCOL"], w=["SHF"])

    def make_HT(self, HT, tiles, with_shift=True):
        add, PS, X = self.add, self.PS, self.X
        for t in tiles:
            v = 0 if t < NLT else 1
            for half in range(2):
                b = half
                for q in range(4):
                    kc = half * 4 + q
                    add("pe", lambda e, t=t, kc=kc, b=b, q=q: e.transpose(PS[:, b, q * 128:(q + 1) * 128], X[:, t, kc * 128:(kc + 1) * 128], self.IDF[:, :]),
                        r=[("X", t), "IDF"], w=[("PS", b)])
                for q in range(4):
                    kc = half * 4 + q
                    if with_shift:
                        add("act", lambda e, t=t, kc=kc, b=b, q=q, v=v: e.activation(out=HT[:, kc, t * 128:(t + 1) * 128], in_=PS[:, b, q * 128:(q + 1) * 128],
                                                                                      func=AF.Identity, bias=self.SHF[:, v, kc:kc + 1], scale=self.SCL[:, v, kc:kc + 1]),
                            r=[("PS", b), "SCL", "SHF"], w=[("HT", t)])
                    else:
                        add("act", lambda e, t=t, kc=kc, b=b, q=q, v=v: e.activation(out=HT[:, kc, t * 128:(t + 1) * 128], in_=PS[:, b, q * 128:(q + 1) * 128],
                                                                                      func=AF.Copy, scale=self.SCL[:, v, kc:kc + 1]),
                            r=[("PS", b), "SCL"], w=[("HT", t)])

    def layer_norm(self, LNV, TMP, which, tiles, final=False, tkey="TMPA"):
        add, X, STAT = self.add, self.X, self.STAT
        a = 1.0 if final else ALPHA
        row = which * 2
        add("sp", lambda e: e.dma_start(out=LNV[:, 0, :], in_=self.ln_in[row:row + 1, :].to_broadcast([128, D])), w=[("LNV", 0)], kind="d")
        add("sp", lambda e: e.dma_start(out=LNV[:, 1, :], in_=self.ln_in[row + 1:row + 2, :].to_broadcast([128, D])), w=[("LNV", 1)], kind="d")
        if a != 1.0:
            add("dve", lambda e: e.tensor_scalar(out=LNV[:, :, :], in0=LNV[:, :, :], scalar1=a, scalar2=None, op0=ALU.mult),
                r=[("LNV", 0), ("LNV", 1)], w=[("LNV", 0), ("LNV", 1)])
        for t in tiles:
            k = ("X", t)
            add("dve", lambda e, t=t: e.tensor_reduce(out=STAT[:, 0:1], in_=X[:, t, :], axis=AX.X, op=ALU.add), r=[k], w=["STAT"])
            add("dve", lambda e: e.tensor_scalar(out=STAT[:, 1:2], in0=STAT[:, 0:1], scalar1=-1.0 / D, scalar2=None, op0=ALU.mult), r=["STAT"], w=["STAT"])
            add("dve", lambda e, t=t: e.tensor_scalar(out=X[:, t, :], in0=X[:, t, :], scalar1=STAT[:, 1:2], scalar2=None, op0=ALU.add), r=[k, "STAT"], w=[k])
            add("dve", lambda e, t=t: e.tensor_tensor(out=TMP[:, :], in0=X[:, t, :], in1=X[:, t, :], op=ALU.mult), r=[k], w=[tkey])
            add("dve", lambda e: e.tensor_reduce(out=STAT[:, 2:3], in_=TMP[:, :], axis=AX.X, op=ALU.add), r=[tkey], w=["STAT"])
            add("dve", lambda e: e.tensor_scalar(out=STAT[:, 3:4], in0=STAT[:, 2:3], scalar1=1.0 / D, scalar2=LN_EPS, op0=ALU.mult, op1=ALU.add), r=["STAT"], w=["STAT"])
            add("act", lambda e: e.activation(out=STAT[:, 5:6], in_=STAT[:, 3:4], func=AF.Sqrt), r=["STAT"], w=["STAT"])
            add("dve", lambda e: e.reciprocal(out=STAT[:, 4:5], in_=STAT[:, 5:6]), r=["STAT"], w=["STAT"])
            add("dve", lambda e, t=t: e.scalar_tensor_tensor(out=X[:, t, :], in0=X[:, t, :], scalar=STAT[:, 4:5], in1=LNV[:, 0, :], op0=ALU.mult, op1=ALU.mult),
                r=[k, "STAT", ("LNV", 0)], w=[k])
            add("dve", lambda e, t=t: e.tensor_tensor(out=X[:, t, :], in0=X[:, t, :], in1=LNV[:, 1, :], op=ALU.add), r=[k, ("LNV", 1)], w=[k])

    def accum(self, GBC, TMP, t, b0, gate=None, tkey="TMPA"):
        v = 0 if t < NLT else 1
        PS, X = self.PS, self.X
        src = PS[:, b0:b0 + 2, :].rearrange("p a b -> p (a b)")
        if gate is None:
            self.add("dve", lambda e: e.tensor_tensor(out=TMP[:, :], in0=src, in1=GBC[:, v, :], op=ALU.mult),
                     r=[("PS", b0), ("PS", b0 + 1), ("GBC", v)], w=[tkey])
        else:
            gap, gkey = gate
            self.add("dve", lambda e: e.scalar_tensor_tensor(out=TMP[:, :], in0=src, scalar=gap, in1=GBC[:, v, :], op0=ALU.mult, op1=ALU.mult),
                     r=[("PS", b0), ("PS", b0 + 1), ("GBC", v), gkey], w=[tkey])
        self.add("pool", lambda e: e.tensor_tensor(out=X[:, t, :], in0=X[:, t, :], in1=TMP[:, :], op=ALU.add), r=[("X", t), tkey], w=[("X", t)])

    def swiglu(self, bufs, HT, GBC, wg, wu, wd, F, tiles, e_off=0, gates=None, cnt0=0):
        add, PS = self.add, self.PS
        WA, WB, WD, AT, SG, TMP = bufs
        nbuf = len(WA)
        nfb = (F + 511) // 512
        ntok = len(tiles) * 128
        t0 = tiles[0] * 128
        blocks = [(b0, min(512, ntok - b0)) for b0 in range(0, ntok, 512)]
        cnt = cnt0
        hkeys = [("HT", tt) for tt in tiles]
        for fb in range(nfb):
            f0 = fb * 512
            fw = min(512, F - f0)
            nfc = fw // 128
            par = cnt % nbuf
            cnt += 1
            add("pool", lambda e, f0=f0, fw=fw, par=par: e.dma_start(out=WA[par][:, :, 0:fw], in_=wg.ap()[e_off * D:(e_off + 1) * D, :].rearrange("(k p) c -> p k c", p=128)[:, :, f0:f0 + fw]),
                w=[("WA", par)], kind="d")
            add("pool", lambda e, f0=f0, fw=fw, par=par: e.dma_start(out=WB[par][:, :, 0:fw], in_=wu.ap()[e_off * D:(e_off + 1) * D, :].rearrange("(k p) c -> p k c", p=128)[:, :, f0:f0 + fw]),
                w=[("WB", par)], kind="d")
            add("pool", lambda e, f0=f0, nfc=nfc, par=par: e.dma_start(out=WD[par][:, 0:nfc, :], in_=wd.ap()[e_off * F + f0:e_off * F + f0 + nfc * 128, :].rearrange("(k p) c -> p k c", p=128)),
                w=[("WD", par)], kind="d")
            for bi, (b0, bw) in enumerate(blocks):
                ap_ = bi % 2
                for fc in range(nfc):
                    for kc in range(8):
                        add("pe", lambda e, fc=fc, kc=kc, par=par, b0=b0, bw=bw: e.matmul(PS[:, 4, 0:bw], lhsT=WA[par][:, kc, fc * 128:(fc + 1) * 128], rhs=HT[:, kc, t0 + b0:t0 + b0 + bw],
                                                                                           start=(kc == 0), stop=(kc == 7)),
                            r=[("WA", par)] + hkeys, w=[("PS", 4)])
                    for kc in range(8):
                        add("pe", lambda e, fc=fc, kc=kc, par=par, b0=b0, bw=bw: e.matmul(PS[:, 5, 0:bw], lhsT=WB[par][:, kc, fc * 128:(fc + 1) * 128], rhs=HT[:, kc, t0 + b0:t0 + b0 + bw],
                                                                                           start=(kc == 0), stop=(kc == 7)),
                            r=[("WB", par)] + hkeys, w=[("PS", 5)])
                    sp_ = fc % 2
                    add("act", lambda e, sp_=sp_, bw=bw: e.activation(out=SG[sp_][:, 0:bw], in_=PS[:, 4, 0:bw], func=AF.Silu), r=[("PS", 4)], w=[("SG", sp_)])
                    add("dve", lambda e, sp_=sp_, ap_=ap_, fc=fc, bw=bw: e.tensor_tensor(out=AT[ap_][:, fc, 0:bw], in0=PS[:, 5, 0:bw], in1=SG[sp_][:, 0:bw], op=ALU.mult),
                        r=[("PS", 5), ("SG", sp_)], w=[("AT", ap_)])
                for ti in range(bw // 128):
                    t = tiles[0] + b0 // 128 + ti
                    yp = ti % 2
                    for hf in range(2):
                        for fc in range(nfc):
                            add("pe", lambda e, ap_=ap_, fc=fc, ti=ti, par=par, hf=hf, yp=yp: e.matmul(PS[:, 2 * yp + hf, :], lhsT=AT[ap_][:, fc, ti * 128:(ti + 1) * 128], rhs=WD[par][:, fc, hf * 512:(hf + 1) * 512],
                                                                                                       start=(fc == 0), stop=(fc == nfc - 1)),
                                r=[("AT", ap_), ("WD", par)], w=[("PS", 2 * yp + hf)])
                    self.accum(GBC, TMP[yp], t, 2 * yp, gate=None if gates is None else gates(t), tkey=("TMPA", yp))
        return cnt

    def ffn_bufs(self, nbuf):
        WA = [self.vb("p (k c) -> p k c", n=8 * 512, k=8) for _ in range(nbuf)]
        WB = [self.vb("p (k c) -> p k c", n=8 * 512, k=8) for _ in range(nbuf)]
        WD = [self.vb("p (k c) -> p k c", n=4 * D, k=4) for _ in range(nbuf)]
        AT = [self.vb("p (k c) -> p k c", n=4 * 512, k=4) for _ in range(2)]
        SG = [self.vf(n=512) for _ in range(2)]
        TMP = [self.vf(n=D) for _ in range(2)]
        return WA, WB, WD, AT, SG, TMP

    def finish(self):
        self.S.emit(self.nc, self.es)
        self.es.close()
        return self.nc


def build_PA(first):
    P = Prog(44 * 1024, 6 * 1024)
    add, PS = P.add, P.PS
    w_in = P.ext("w_in", [D, 1952])
    wkr_in = P.ext("wkr", [D, 96])
    wkrp_in = P.ext("wkrp", [D, 96])
    nrm_in = P.ext("nrm", [128, 3])
    rope_in = P.ext("rope", [32, 2, TPC])
    qa_o = P.outp("qa", [128, 4, NTOK], BF16)
    ka_o = P.outp("ka", [128, 4, NTOK], BF16)
    va_o = P.outp("va", [128, NT, 640], BF16)
    qcn_o = P.outp("qcn", [128, 2, NTOK], BF16)
    kvn_o = P.outp("kvn", [128, NTOK], BF16)
    kr_o = P.outp("kr", [32, NTOK], BF16)
    P.load_stream(first)
    P.modulation()
    P.mod_cols(0, 1)
    P.phase()
    HT = P.vb("p (k c) -> p k c", n=8 * NTOK, k=8)
    WQ = P.vb("p (k c) -> p k c", n=8 * 512, k=8)
    WS = P.vb("p (k c) -> p k c", n=8 * 256, k=8)
    WKV = P.vb("p (k c) -> p k c", n=8 * 128, k=8)
    WKR = P.vb("p (k c) -> p k c", n=8 * 96, k=8)
    WKRP = P.vb("p (k c) -> p k c", n=8 * 96, k=8)
    OUTB = P.vb("p (k c) -> p k c", n=2 * NTOK, k=2)
    VAO = P.vb("p (t h c) -> p t h c", n=2 * 640, t=2, h=8)
    QCN = P.vb("p (k c) -> p k c", n=2 * NTOK, k=2)
    KVN = P.vb(n=NTOK)
    KR = P.vb(n=NTOK)
    SQ = P.vb("p (k c) -> p k c", n=2 * 512, k=2)
    NRM = P.vf(n=3)
    RB = P.vf(n=512)
    T1 = P.vf(n=512)
    T2 = P.vf(n=512)
    ROPE = P.vf("p (a c) -> p a c", n=2 * 512, a=2)
    P.make_HT(HT, list(range(NT)))
    hk = [("HT", t) for t in range(NT)]
    w3 = w_in.ap().rearrange("(k p) c -> p k c", p=128)
    add("sp", lambda e: e.dma_start(out=NRM[:, :], in_=nrm_in[:, :]), w=["NRM"], kind="d")
    add("pool", lambda e: e.dma_start(out=WS[:, :, :], in_=w3[:, :, 1536:1792]), w=["WS"], kind="d")
    add("pool", lambda e: e.dma_start(out=WKV[:, :, :], in_=w3[:, :, 1792:1920]), w=["WKV"], kind="d")
    add("pool", lambda e: e.dma_start(out=WKR[:, :, :], in_=wkr_in.ap().rearrange("(k p) c -> p k c", p=128)), w=["WKR"], kind="d")
    add("pool", lambda e: e.dma_start(out=WKRP[:, :, :], in_=wkrp_in.ap().rearrange("(k p) c -> p k c", p=128)), w=["WKRP"], kind="d")
    add("dve", lambda e: e.memset(VAO[:, :, :, :], 1.0), w=[("VAO", 0), ("VAO", 1)])
    for wi, dst in ((0, qa_o), (1, ka_o)):
        add("pool", lambda e, wi=wi: e.dma_start(out=WQ[:, :, :], in_=w3[:, :, wi * 512:(wi + 1) * 512]), w=["WQ"], kind="d")
        n = 0
        for jc in range(4):
            for (b0, bw) in BLOCKS:
                b = n % 2
                n += 1
                for kc in range(8):
                    add("pe", lambda e, jc=jc, kc=kc, b=b, b0=b0, bw=bw: e.matmul(PS[:, b, 0:bw], lhsT=WQ[:, kc, jc * 128:(jc + 1) * 128], rhs=HT[:, kc, b0:b0 + bw],
                                                                                   start=(kc == 0), stop=(kc == 7)), r=["WQ"] + hk, w=[("PS", b)])
                add("act", lambda e, jc=jc, b=b, b0=b0, bw=bw: e.activation(out=OUTB[:, jc % 2, b0:b0 + bw], in_=PS[:, b, 0:bw], func=AF.Copy),
                    r=[("PS", b)], w=[("OUTB", jc % 2)])
            add("sp", lambda e, dst=dst, jc=jc: e.dma_start(out=dst[:, jc, :], in_=OUTB[:, jc % 2, :]), r=[("OUTB", jc % 2)], kind="d")
    add("pool", lambda e: e.dma_start(out=WQ[:, :, :], in_=w3[:, :, 1024:1536]), w=["WQ"], kind="d")
    for t in range(NT):
        b = t % 2
        for kc in range(8):
            add("pe", lambda e, t=t, kc=kc, b=b: e.matmul(PS[:, b, :], lhsT=HT[:, kc, t * 128:(t + 1) * 128], rhs=WQ[:, kc, :], start=(kc == 0), stop=(kc == 7)),
                r=["WQ", ("HT", t)], w=[("PS", b)])
        add("dve", lambda e, t=t, b=b: e.tensor_copy(out=VAO[:, b, :, 0:64], in_=PS[:, b, :].rearrange("p (h c) -> p h c", h=8)), r=[("PS", b)], w=[("VAO", b)])
        add("sp", lambda e, t=t, b=b: e.dma_start(out=va_o[:, t, :], in_=VAO[:, b, :, :].rearrange("p h c -> p (h c)")), r=[("VAO", b)], kind="d")
    for (b0, bw) in BLOCKS:
        isctx = b0 >= TPC
        for ch in range(2):
            for kc in range(8):
                add("pe", lambda e, ch=ch, kc=kc, b0=b0, bw=bw: e.matmul(PS[:, 2 + ch, 0:bw], lhsT=WS[:, kc, ch * 128:(ch + 1) * 128], rhs=HT[:, kc, b0:b0 + bw],
                                                                          start=(kc == 0), stop=(kc == 7)), r=["WS"] + hk, w=[("PS", 2 + ch)])
            add("act", lambda e, ch=ch, bw=bw: e.activation(out=SQ[:, ch, 0:bw], in_=PS[:, 2 + ch, 0:bw], func=AF.Square), r=[("PS", 2 + ch)], w=[("SQ", ch)])
        for ch in range(2):
            add("pe", lambda e, ch=ch, bw=bw: e.matmul(PS[:, 4, 0:bw], lhsT=P.ONB[:, :], rhs=SQ[:, ch, 0:bw], start=(ch == 0), stop=(ch == 1)),
                r=["ONB", ("SQ", ch)], w=[("PS", 4)])
        add("dve", lambda e, bw=bw: e.tensor_scalar(out=RB[:, 0:bw], in0=PS[:, 4, 0:bw], scalar1=1.0 / 256, scalar2=RMS_EPS, op0=ALU.mult, op1=ALU.add), r=[("PS", 4)], w=["RB"])
        add("act", lambda e, bw=bw: e.activation(out=RB[:, 0:bw], in_=RB[:, 0:bw], func=AF.Sqrt), r=["RB"], w=["RB"])
        add("dve", lambda e, bw=bw: e.reciprocal(out=RB[:, 0:bw], in_=RB[:, 0:bw]), r=["RB"], w=["RB"])
        for ch in range(2):
            add("dve", lambda e, ch=ch, b0=b0, bw=bw: e.scalar_tensor_tensor(out=QCN[:, ch, b0:b0 + bw], in0=PS[:, 2 + ch, 0:bw], scalar=NRM[:, ch:ch + 1], in1=RB[:, 0:bw],
                                                                              op0=ALU.mult, op1=ALU.mult), r=[("PS", 2 + ch), "NRM", "RB"], w=["QCN"])
        for kc in range(8):
            add("pe", lambda e, kc=kc, b0=b0, bw=bw: e.matmul(PS[:, 5, 0:bw], lhsT=WKV[:, kc, :], rhs=HT[:, kc, b0:b0 + bw], start=(kc == 0), stop=(kc == 7)),
                r=["WKV"] + hk, w=[("PS", 5)])
        add("act", lambda e, bw=bw: e.activation(out=SQ[:, 0, 0:bw], in_=PS[:, 5, 0:bw], func=AF.Square), r=[("PS", 5)], w=[("SQ", 0)])
        add("pe", lambda e, bw=bw: e.matmul(PS[:, 4, 0:bw], lhsT=P.ONB[:, :], rhs=SQ[:, 0, 0:bw], start=True, stop=True), r=["ONB", ("SQ", 0)], w=[("PS", 4)])
        add("dve", lambda e, bw=bw: e.tensor_scalar(out=RB[:, 0:bw], in0=PS[:, 4, 0:bw], scalar1=1.0 / 128, scalar2=RMS_EPS, op0=ALU.mult, op1=ALU.add), r=[("PS", 4)], w=["RB"])
        add("act", lambda e, bw=bw: e.activation(out=RB[:, 0:bw], in_=RB[:, 0:bw], func=AF.Sqrt), r=["RB"], w=["RB"])
        add("dve", lambda e, bw=bw: e.reciprocal(out=RB[:, 0:bw], in_=RB[:, 0:bw]), r=["RB"], w=["RB"])
        add("dve", lambda e, b0=b0, bw=bw: e.scalar_tensor_tensor(out=KVN[:, b0:b0 + bw], in0=PS[:, 5, 0:bw], scalar=NRM[:, 2:3], in1=RB[:, 0:bw], op0=ALU.mult, op1=ALU.mult),
            r=[("PS", 5), "NRM", "RB"], w=["KVN"])
        for kc in range(8):
            add("pe", lambda e, kc=kc, b0=b0, bw=bw: e.matmul(PS[0:96, 6, 0:bw], lhsT=WKR[:, kc, :], rhs=HT[:, kc, b0:b0 + bw], start=(kc == 0), stop=(kc == 7)),
                r=["WKR"] + hk, w=[("PS", 6)])
        if isctx:
            add("act", lambda e, b0=b0, bw=bw: e.activation(out=KR[64:96, b0:b0 + bw], in_=PS[64:96, 6, 0:bw], func=AF.Copy), r=[("PS", 6)], w=["KR"])
        else:
            for kc in range(8):
                add("pe", lambda e, kc=kc, b0=b0, bw=bw: e.matmul(PS[0:96, 0, 0:bw], lhsT=WKRP[:, kc, :], rhs=HT[:, kc, b0:b0 + bw], start=(kc == 0), stop=(kc == 7)),
                    r=["WKRP"] + hk, w=[("PS", 0)])
            add("sp", lambda e, b0=b0, bw=bw: e.dma_start(out=ROPE[64:96, :, 0:bw], in_=rope_in[:, :, b0:b0 + bw]), w=["ROPE"], kind="d")
            add("dve", lambda e, bw=bw: e.tensor_tensor(out=T1[64:96, 0:bw], in0=PS[64:96, 6, 0:bw], in1=ROPE[64:96, 0, 0:bw], op=ALU.mult), r=[("PS", 6), "ROPE"], w=["T1"])
            add("dve", lambda e, bw=bw: e.tensor_tensor(out=T2[64:96, 0:bw], in0=PS[64:96, 0, 0:bw], in1=ROPE[64:96, 1, 0:bw], op=ALU.mult), r=[("PS", 0), "ROPE"], w=["T2"])
            add("dve", lambda e, b0=b0, bw=bw: e.tensor_tensor(out=KR[64:96, b0:b0 + bw], in0=T1[64:96, 0:bw], in1=T2[64:96, 0:bw], op=ALU.add), r=["T1", "T2"], w=["KR"])
    add("sp", lambda e: e.dma_start(out=qcn_o[:, :, :], in_=QCN[:, :, :]), r=["QCN"], kind="d")
    add("sp", lambda e: e.dma_start(out=kvn_o[:, :], in_=KVN[:, :]), r=["KVN"], kind="d")
    add("sp", lambda e: e.dma_start(out=kr_o[:, :], in_=KR[64:96, :]), r=["KR"], kind="d")
    return P.finish()


def build_PB(first, ctx_out, stop=9):
    P = Prog(46 * 1024, 9 * 1024)
    add, PS, X = P.add, P.PS, P.X
    qa_in = P.ext("qa", [128, 4, NTOK], BF16)
    kext_in = P.ext("kext", [128, 4, 22 * 128 + 256], BF16)
    vext_in = P.ext("vext", [128, 24, 640], BF16)
    qcn_in = P.ext("qcn", [128, 2, NTOK], BF16)
    kvn_in = P.ext("kvn_all", [128, NKEY], BF16)
    kr_in = P.ext("kr_all", [32, NKEY], BF16)
    wq_in = P.ext("w_qup", [256, 768])
    wqp_in = P.ext("w_qupp", [256, 768])
    wkv_in = P.ext("w_kvup", [128, 1024])
    wo_in = P.ext("w_out", [D, D])
    ffg = P.ext("ffn_g", [D, FFN])
    ffu = P.ext("ffn_u", [D, FFN])
    ffd = P.ext("ffn_d", [FFN, D])
    nab_in = P.ext("nabias", [128, 7, 8, 128])
    nam_in = P.ext("namask", [NLT, 128, 7, 128])
    rope_in = P.ext("rope", [32, 2, TPC])
    y_out = P.outp("y", [NTOK, D])
    P.load_stream(first)
    P.modulation()
    qtiles = list(range(NT)) if ctx_out else list(range(NLT))

    P.phase()
    QA = P.vb("p (k c) -> p k c", n=4 * NTOK, k=4)
    KW = P.vb("p (k c) -> p k c", n=4 * 896, k=4)
    VW = P.vb("p (s c) -> p s c", n=7 * 640, s=7)
    KAC = P.vb("p (k c) -> p k c", n=4 * 256, k=4)
    VAC = P.vb("p (s c) -> p s c", n=2 * 640, s=2)
    EB = [P.vb("p (h q) -> p h q", n=512, h=4) for _ in range(2)]
    PB_ = [P.vb("p (h q) -> p h q", n=512, h=4) for _ in range(2)]
    MIXT = P.vb("p (k q) -> p k q", n=512, k=4)
    WON = P.vb("p (k c) -> p k c", n=4 * D, k=4)
    NB = P.vb("p (s h q) -> p s h q", n=7 * 8 * 128, s=7, h=8)
    MASK = P.vf("p (s q) -> p s q", n=7 * 128, s=7)
    TS = [P.vf("p (h q) -> p h q", n=512, h=4) for _ in range(2)]
    TMP = P.vf(n=D)
    GBC = P.vf("p (v c) -> p v c", n=2 * D, v=2)
    RC = P.vf(n=8)
    OACC = P.vf("p (h c) -> p h c", n=8 * 66, h=8)
    MIX = P.vf("p (h c) -> p h c", n=512, h=8)
    P.load_gate(GBC, 0)
    add("sp", lambda e: e.dma_start(out=QA[:, :, :], in_=qa_in[:, :, :]), w=["QA"], kind="d")
    add("sp", lambda e: e.dma_start(out=KAC[:, :, :], in_=kext_in[:, :, 22 * 128:22 * 128 + 256]), w=["KAC"], kind="d")
    add("sp", lambda e: e.dma_start(out=VAC[:, :, :], in_=vext_in[:, 22:24, :]), w=["VAC"], kind="d")
    add("pool", lambda e: e.dma_start(out=NB[:, :, :, :], in_=nab_in[:, :, :, :]), w=["NB"], kind="d")
    for pos in range(8):
        hd = 2 * (pos % 4) + pos // 4
        add("pool", lambda e, pos=pos, hd=hd: e.dma_start(out=WON[(pos % 2) * 64:(pos % 2) * 64 + 64, pos // 2, :], in_=wo_in[hd * 64:(hd + 1) * 64, :]), w=["WON"], kind="d")
    gcount = 0
    import os
    _budget = [int(os.environ.get("NABUDGET", "100000000"))]
    _radd = P.S.add

    def add(st, fn, r=(), w=(), kind="c"):
        if _budget[0] <= 0:
            return None
        _budget[0] -= 1
        return _radd(st, fn, r=r, w=w, kind=kind)
    P.add = add
    for T in (qtiles if stop >= 1 else []):
        islat = T < NLT
        keytiles = []
        if islat:
            add("sp", lambda e, T=T: e.dma_start(out=KW[:, :, :], in_=kext_in[:, :, T * 128:(T + 7) * 128]), w=["KW"], kind="d")
            add("sp", lambda e, T=T: e.dma_start(out=VW[:, :, :], in_=vext_in[:, T:T + 7, :]), w=["VW"], kind="d")
            add("sp", lambda e, T=T: e.dma_start(out=MASK[:, :, :], in_=nam_in[T, :, :, :]), w=["MASK"], kind="d")
            keytiles = [("w", s) for s in range(7)]
        keytiles += [("c", 0), ("c", 1)]
        grps = []
        for ki, (kind, s_) in enumerate(keytiles):
            for g in range(2):
                grps.append((ki, kind, s_, g, gcount % 2))
                gcount += 1

        def na_S(grp, T=T):
            ki, kind, s, g, sb_ = grp
            for hh in range(4):
                ch, hp = hh, g * 64
                src, key = (KW, "KW") if kind == "w" else (KAC, "KAC")
                add("pe", lambda e, sb_=sb_, hh=hh, ch=ch, hp=hp, s=s, T=T, src=src: e.matmul(PS[:, sb_, hh * 128:(hh + 1) * 128], lhsT=src[hp:hp + 64, ch, s * 128:(s + 1) * 128],
                                                                                             rhs=QA[hp:hp + 64, ch, T * 128:(T + 1) * 128], start=True, stop=True),
                    r=[key, "QA"], w=[("PS", sb_)])

        def na_E(grp):
            ki, kind, s, g, sb_ = grp
            psv = PS[:, sb_, :].rearrange("p (h q) -> p h q", h=4)
            if kind == "w":
                add("dve", lambda e, sb_=sb_, psv=psv, s=s, g=g: e.scalar_tensor_tensor(out=TS[sb_][:, :, :], in0=psv, scalar=0.125, in1=NB[:, s, 4 * g:4 * g + 4, :],
                                                                                        op0=ALU.mult, op1=ALU.add), r=[("PS", sb_), "NB"], w=[("TS", sb_)])
                add("act", lambda e, sb_=sb_: e.activation(out=EB[sb_][:, :, :], in_=TS[sb_][:, :, :], func=AF.Exp), r=[("TS", sb_)], w=[("EB", sb_)])
                add("dve", lambda e, sb_=sb_, s=s: e.tensor_tensor(out=PB_[sb_][:, :, :], in0=EB[sb_][:, :, :], in1=MASK[:, s:s + 1, :].to_broadcast([128, 4, 128]), op=ALU.mult),
                    r=[("EB", sb_), "MASK"], w=[("PB", sb_)])
            else:
                add("act", lambda e, sb_=sb_, psv=psv: e.activation(out=PB_[sb_][:, :, :], in_=psv, func=AF.Exp, scale=0.125), r=[("PS", sb_)], w=[("PB", sb_)])

        def na_PV(grp):
            ki, kind, s, g, sb_ = grp
            for hh in range(4):
                h = 2 * hh + g
                src, key = (VW, "VW") if kind == "w" else (VAC, "VAC")
                add("pe", lambda e, sb_=sb_, hh=hh, h=h, s=s, src=src: e.matmul(PS[:, 2 + sb_, hh * 66:(hh + 1) * 66], lhsT=PB_[sb_][:, hh, :], rhs=src[:, s, h * 80:h * 80 + 66],
                                                                                   start=True, stop=True), r=[("PB", sb_), key], w=[("PS", 2 + sb_)])
            pov = PS[:, 2 + sb_, 0:264].rearrange("p (h c) -> p h c", h=4)
            if ki == 0:
                add("dve", lambda e, g=g, pov=pov: e.tensor_copy(out=OACC[:, 4 * g:4 * g + 4, :], in_=pov), r=[("PS", 2 + sb_)], w=[("OACC", g)])
            else:
                add("dve", lambda e, g=g, pov=pov: e.tensor_tensor(out=OACC[:, 4 * g:4 * g + 4, :], in0=OACC[:, 4 * g:4 * g + 4, :], in1=pov, op=ALU.add),
                    r=[("PS", 2 + sb_), ("OACC", g)], w=[("OACC", g)])

        na_S(grps[0])
        for n_, grp in enumerate(grps):
            na_E(grp)
            if n_ + 1 < len(grps):
                na_S(grps[n_ + 1])
            na_PV(grp)
        for g in range(2):
            ov = OACC[:, 4 * g:4 * g + 4, :]
            add("dve", lambda e, g=g, ov=ov: e.reciprocal(out=RC[:, 4 * g:4 * g + 4], in_=ov[:, :, 64]), r=[("OACC", g)], w=["RC"])
            add("dve", lambda e, g=g, ov=ov: e.tensor_tensor(out=MIX[:, 4 * g:4 * g + 4, :], in0=ov[:, :, 0:64], in1=RC[:, 4 * g:4 * g + 4].unsqueeze(2).to_broadcast([128, 4, 64]), op=ALU.mult),
                r=[("OACC", g), "RC"], w=["MIX"])
        for c4 in range(4):
            add("pe", lambda e, c4=c4: e.transpose(PS[:, 6, c4 * 128:(c4 + 1) * 128], MIX[:, 2 * c4:2 * c4 + 2, :].rearrange("p h c -> p (h c)"), P.IDF[:, :]),
                r=["MIX", "IDF"], w=[("PS", 6)])
        add("act", lambda e: e.activation(out=MIXT[:, :, :], in_=PS[:, 6, :].rearrange("p (k q) -> p k q", k=4), func=AF.Copy), r=[("PS", 6)], w=["MIXT"])
        for hf in range(2):
            for c4 in range(4):
                add("pe", lambda e, hf=hf, c4=c4: e.matmul(PS[:, 4 + hf, :], lhsT=MIXT[:, c4, :], rhs=WON[:, c4, hf * 512:(hf + 1) * 512], start=(c4 == 0), stop=(c4 == 3)),
                    r=["MIXT", "WON"], w=[("PS", 4 + hf)])
        P.accum(GBC, TMP, T, 4)

    add = _radd
    P.add = _radd
    P.phase()
    KT = P.vb(n=NKEY)
    VH = P.vb("p (t c) -> p t c", n=NKT * 80, t=NKT)
    QCN = P.vb("p (k c) -> p k c", n=2 * NTOK, k=2)
    QT = P.vb(n=NTOK)
    KVB = [P.vb(n=1024) for _ in range(2)]
    PT = [P.vb("p (j q) -> p j q", n=1024, j=2) for _ in range(2)]
    MXT = P.vb(n=512)
    WQ = P.vb("p (k c) -> p k c", n=2 * 768, k=2)
    WQP = P.vb("p (k c) -> p k c", n=2 * 768, k=2)
    WKV = P.vb(n=1024)
    WOH = P.vb(n=D)
    OSB = P.vf(n=512)
    RR = P.vf(n=512)
    T1 = P.vf(n=512)
    T2 = P.vf(n=512)
    TMP = P.vf(n=D)
    GBC = P.vf("p (v c) -> p v c", n=2 * D, v=2)
    ROPE = P.vf("p (a c) -> p a c", n=2 * TPC, a=2)
    P.load_gate(GBC, 0)
    add("sp", lambda e: e.dma_start(out=KT[64:96, :], in_=kr_in[:, :]), w=["KTr"], kind="d")
    add("sp", lambda e: e.dma_start(out=QCN[:, :, :], in_=qcn_in[:, :, :]), w=["QCN"], kind="d")
    add("sp", lambda e: e.dma_start(out=ROPE[64:96, :, :], in_=rope_in[:, :, :]), w=["ROPE"], kind="d")
    add("pool", lambda e: e.dma_start(out=WQ[:, :, :], in_=wq_in.ap().rearrange("(k p) c -> p k c", p=128)), w=["WQ"], kind="d")
    add("pool", lambda e: e.dma_start(out=WQP[:, :, :], in_=wqp_in.ap().rearrange("(k p) c -> p k c", p=128)), w=["WQP"], kind="d")
    add("pool", lambda e: e.dma_start(out=WKV[:, :], in_=wkv_in[:, :]), w=["WKV"], kind="d")
    add("dve", lambda e: e.memset(VH[:, :, :], 1.0), w=["VH"])
    kvblocks = [(k0, min(1024, NKEY - k0)) for k0 in range(0, NKEY, 1024)]
    qblocks = BLOCKS if ctx_out else BLOCKS[:4]
    nkv = 0
    for h in (range(8) if stop >= 2 else []):
        add("pool", lambda e, h=h: e.dma_start(out=WOH[0:64, :], in_=wo_in[512 + h * 64:512 + (h + 1) * 64, :]), w=["WOH"], kind="d")
        for (k0, kw) in kvblocks:
            kb = nkv % 2
            nkv += 1
            add("sp", lambda e, kb=kb, k0=k0, kw=kw: e.dma_start(out=KVB[kb][:, 0:kw], in_=kvn_in[:, k0:k0 + kw]), w=[("KVB", kb)], kind="d")
            for hf in range(0, kw, 512):
                w_ = min(512, kw - hf)
                b = 2 + (hf // 512)
                add("pe", lambda e, kb=kb, hf=hf, w_=w_, b=b, h=h: e.matmul(PS[0:64, b, 0:w_], lhsT=WKV[:, h * 128:h * 128 + 64], rhs=KVB[kb][:, hf:hf + w_], start=True, stop=True),
                    r=["WKV", ("KVB", kb)], w=[("PS", b)])
                add("dve", lambda e, k0=k0, hf=hf, w_=w_, b=b: e.tensor_copy(out=KT[0:64, k0 + hf:k0 + hf + w_], in_=PS[0:64, b, 0:w_]),
                    r=[("PS", b)], w=["KTn"])
            nt_ = kw // 128
            for ti in range(nt_):
                add("pe", lambda e, kb=kb, ti=ti, h=h: e.matmul(PS[:, 0, ti * 64:(ti + 1) * 64], lhsT=KVB[kb][:, ti * 128:(ti + 1) * 128], rhs=WKV[:, h * 128 + 64:h * 128 + 128],
                                                                start=True, stop=True), r=["WKV", ("KVB", kb)], w=[("PS", 0)])
            add("act", lambda e, k0=k0, nt_=nt_: e.activation(out=VH[:, k0 // 128:k0 // 128 + nt_, 0:64], in_=PS[:, 0, 0:nt_ * 64].rearrange("p (t c) -> p t c", t=nt_), func=AF.Copy),
                r=[("PS", 0)], w=["VH"])
        for (b0, bw) in qblocks:
            isctx = b0 >= TPC
            for kc in range(2):
                add("pe", lambda e, kc=kc, b0=b0, bw=bw, h=h: e.matmul(PS[0:96, 0, 0:bw], lhsT=WQ[:, kc, h * 96:(h + 1) * 96], rhs=QCN[:, kc, b0:b0 + bw], start=(kc == 0), stop=(kc == 1)),
                    r=["WQ", "QCN"], w=[("PS", 0)])
            add("act", lambda e, b0=b0, bw=bw: e.activation(out=QT[0:64, b0:b0 + bw], in_=PS[0:64, 0, 0:bw], func=AF.Copy), r=[("PS", 0)], w=["QT"])
            if isctx:
                add("act", lambda e, b0=b0, bw=bw: e.activation(out=QT[64:96, b0:b0 + bw], in_=PS[64:96, 0, 0:bw], func=AF.Copy), r=[("PS", 0)], w=["QT"])
            else:
                for kc in range(2):
                    add("pe", lambda e, kc=kc, b0=b0, bw=bw, h=h: e.matmul(PS[0:96, 1, 0:bw], lhsT=WQP[:, kc, h * 96:(h + 1) * 96], rhs=QCN[:, kc, b0:b0 + bw], start=(kc == 0), stop=(kc == 1)),
                        r=["WQP", "QCN"], w=[("PS", 1)])
                add("dve", lambda e, b0=b0, bw=bw: e.tensor_tensor(out=T1[64:96, 0:bw], in0=PS[64:96, 0, 0:bw], in1=ROPE[64:96, 0, b0:b0 + bw], op=ALU.mult), r=[("PS", 0), "ROPE"], w=["T1"])
                add("dve", lambda e, b0=b0, bw=bw: e.tensor_tensor(out=T2[64:96, 0:bw], in0=PS[64:96, 1, 0:bw], in1=ROPE[64:96, 1, b0:b0 + bw], op=ALU.mult), r=[("PS", 1), "ROPE"], w=["T2"])
                add("dve", lambda e, b0=b0, bw=bw: e.tensor_tensor(out=QT[64:96, b0:b0 + bw], in0=T1[64:96, 0:bw], in1=T2[64:96, 0:bw], op=ALU.add), r=["T1", "T2"], w=["QT"])
        pairs = [(kt, p_) for kt in range(NKT) for p_ in range(2)]

        def emit_S(n):
            kt, p_ = pairs[n]
            a = 2 * (n % 2)
            for j in range(2):
                qb = 2 * p_ + j
                add("pe", lambda e, a=a, j=j, kt=kt, qb=qb: e.matmul(PS[:, a + j, :], lhsT=KT[0:96, kt * 128:(kt + 1) * 128], rhs=QT[0:96, qb * 512:(qb + 1) * 512], start=True, stop=True),
                    r=["KTn", "KTr", "QT"], w=[("PS", a + j)])
        emit_S(0)
        for n, (kt, p_) in enumerate(pairs):
            a = 2 * (n % 2)
            pb = n % 2
            add("act", lambda e, a=a, pb=pb: e.activation(out=PT[pb][:, :, :], in_=PS[:, a:a + 2, :], func=AF.Exp, scale=SM_MLA),
                r=[("PS", a), ("PS", a + 1)], w=[("PT", pb)])
            if n + 1 < len(pairs):
                emit_S(n + 1)
            for j in range(2):
                qb = 2 * p_ + j
                add("pe", lambda e, pb=pb, j=j, kt=kt, qb=qb: e.matmul(PS[0:66, 4 + qb, :], lhsT=VH[:, kt, 0:66], rhs=PT[pb][:, j, :], start=(kt == 0), stop=(kt == NKT - 1)),
                    r=["VH", ("PT", pb)], w=[("PS", 4 + qb)])
        obanks = [(4 + qb, qb * 512, 512) for qb in range(4)]
        for (ob, q0, qw) in obanks + ([(2, TPC, 256)] if ctx_out else []):
            if ob == 2:
                for kt in range(2):
                    sb_ = kt
                    add("pe", lambda e, sb_=sb_, kt=kt: e.matmul(PS[:, sb_, 0:256], lhsT=KT[0:96, kt * 128:(kt + 1) * 128], rhs=QT[0:96, TPC:TPC + 256], start=True, stop=True),
                        r=["KTn", "KTr", "QT"], w=[("PS", sb_)])
                    add("act", lambda e, sb_=sb_: e.activation(out=PT[sb_][:, 0, 0:256], in_=PS[:, sb_, 0:256], func=AF.Exp, scale=SM_MLA), r=[("PS", sb_)], w=[("PT", sb_)])
                    add("pe", lambda e, sb_=sb_, kt=kt: e.matmul(PS[0:66, 2, 0:256], lhsT=VH[:, kt, 0:66], rhs=PT[sb_][:, 0, 0:256], start=(kt == 0), stop=(kt == 1)),
                        r=["VH", ("PT", sb_)], w=[("PS", 2)])
            add("act", lambda e, ob=ob, qw=qw: e.activation(out=OSB[0:64, 0:qw], in_=PS[0:64, ob, 0:qw], func=AF.Copy), r=[("PS", ob)], w=["OSB"])
            add("dve", lambda e, ob=ob, qw=qw: e.reciprocal(out=RR[64:65, 0:qw], in_=PS[64:65, ob, 0:qw]), r=[("PS", ob)], w=["RR"])
            add("pe", lambda e, ob=ob, qw=qw: e.matmul(PS[0:64, ob, 0:qw], lhsT=P.ONF[64:65, 0:64], rhs=RR[64:65, 0:qw], start=True, stop=True),
                r=["RR", "ONF", "OSB"], w=[("PS", ob)])
            add("dve", lambda e, ob=ob, qw=qw: e.tensor_tensor(out=MXT[0:64, 0:qw], in0=OSB[0:64, 0:qw], in1=PS[0:64, ob, 0:qw], op=ALU.mult), r=["OSB", ("PS", ob)], w=["MXT"])
            for ti in range(qw // 128):
                t = q0 // 128 + ti
                for hf in range(2):
                    add("pe", lambda e, ti=ti, hf=hf: e.matmul(PS[:, hf, :], lhsT=MXT[0:64, ti * 128:(ti + 1) * 128], rhs=WOH[0:64, hf * 512:(hf + 1) * 512], start=True, stop=True),
                        r=["MXT", "WOH"], w=[("PS", hf)])
                P.accum(GBC, TMP, t, 0)

    P.phase()
    HT = P.vb("p (k c) -> p k c", n=8 * NTOK, k=8)
    bufs = P.ffn_bufs(2)
    GBC = P.vf("p (v c) -> p v c", n=2 * D, v=2)
    LNV = P.vf("p (v c) -> p v c", n=2 * D, v=2)
    tiles = list(range(NT)) if ctx_out else list(range(NLT))
    if stop >= 3:
        P.layer_norm(LNV, bufs[5][0], 0, tiles, tkey=("TMPA", 0))
    if stop >= 4:
        P.mod_cols(3, 4)
        P.load_gate(GBC, 1)
        P.make_HT(HT, tiles)
        P.swiglu(bufs, HT, GBC, ffg, ffu, ffd, FFN, tiles)
        P.layer_norm(LNV, bufs[5][0], 1, tiles, tkey=("TMPA", 0))
    P.store_stream(y_out)
    return P.finish()


def build_PC(final, ctx_live, stop=9):
    P = Prog(48 * 1024, 9 * 1024)
    add, PS, X = P.add, P.PS, P.X
    halo_in = P.ext("halo", [8, 2, D])
    asame_in = P.ext("a_same", [128, 5, 4, 128])
    aprev_in = P.ext("a_prev", [128, 4, 128])
    anext_in = P.ext("a_next", [128, 4, 128])
    ahp_in = P.ext("a_hp", [8, 4, 128])
    ahn_in = P.ext("a_hn", [8, 4, 128])
    pw_in = P.ext("pool_w", [4, 256, 256])
    rt_in = P.ext("router", [D, NEXP])
    mg = P.ext("moe_g", [NEXP * D, EXPD])
    mu = P.ext("moe_u", [NEXP * D, EXPD])
    md = P.ext("moe_d", [NEXP * EXPD, D])
    y_out = P.outp("y", [NTOK, D])
    P.load_stream(False)
    P.modulation()
    tiles = list(range(NT)) if ctx_live else list(range(NLT))
    P.phase()
    XB = P.vb("p (t c) -> p t c", n=NT * D, t=NT)
    MT = P.vb("p (k c) -> p k c", n=8 * NTOK, k=8)
    WP = P.vb("p (g k d) -> p g k d", n=4 * 2 * 256, g=4, k=2)
    ASAME = P.vb("p (a g q) -> p a g q", n=5 * 4 * 128, a=5, g=4)
    APREV = P.vb("p (g q) -> p g q", n=512, g=4)
    ANEXT = P.vb("p (g q) -> p g q", n=512, g=4)
    AHP = P.vb("p (g q) -> p g q", n=512, g=4)
    AHN = P.vb("p (g q) -> p g q", n=512, g=4)
    HALO = P.vb("p (a c) -> p a c", n=2 * D, a=2)
    GBC = P.vf("p (v c) -> p v c", n=2 * D, v=2)
    PSC = P.vf(n=D)
    TMP = P.vf(n=D)
    LNV = P.vf("p (v c) -> p v c", n=2 * D, v=2)
    P.mod_cols(0, 1)
    P.load_gate(GBC, 0)
    add("sp", lambda e: e.dma_start(out=PSC[:, :], in_=P.ln_in[4:5, :].to_broadcast([128, D])), w=["PSC"], kind="d")
    add("pool", lambda e: e.dma_start(out=ASAME[:, :, :, :], in_=asame_in[:, :, :, :]), w=["ATAB"], kind="d")
    add("pool", lambda e: e.dma_start(out=APREV[:, :, :], in_=aprev_in[:, :, :]), w=["ATAB"], kind="d")
    add("pool", lambda e: e.dma_start(out=ANEXT[:, :, :], in_=anext_in[:, :, :]), w=["ATAB"], kind="d")
    add("pool", lambda e: e.dma_start(out=AHP[0:8, :, :], in_=ahp_in[:, :, :]), w=["ATAB"], kind="d")
    add("pool", lambda e: e.dma_start(out=AHN[0:8, :, :], in_=ahn_in[:, :, :]), w=["ATAB"], kind="d")
    add("pool", lambda e: e.dma_start(out=HALO[0:8, :, :], in_=halo_in[:, :, :]), w=["ATAB"], kind="d")
    add("pool", lambda e: e.dma_start(out=WP[:, :, :, :], in_=pw_in.ap().rearrange("g (k p) d -> p g k d", p=128)), w=["WP"], kind="d")
    for t in tiles:
        add("act", lambda e, t=t: e.activation(out=XB[:, t, :], in_=X[:, t, :], func=AF.Copy), r=[("X", t)], w=[("XB", t)])
    nb = 0
    for t in tiles:
        for half in range(2):
            b = nb % 2
            nb += 1
            for q in range(4):
                kc = half * 4 + q
                g = kc // 2
                cs = slice(kc * 128, (kc + 1) * 128)
                if t < NLT:
                    cls = 1 if t == 0 else (2 if t == NLT - 1 else 0)
                    srcs = [(XB[:, t, cs], ASAME[:, cls, g, :], ("XB", t))]
                    srcs.append((XB[:, t - 1, cs], APREV[:, g, :], ("XB", t - 1)) if t > 0 else (HALO[0:8, 0, cs], AHP[0:8, g, :], "ATAB"))
                    srcs.append((XB[:, t + 1, cs], ANEXT[:, g, :], ("XB", t + 1)) if t < NLT - 1 else (HALO[0:8, 1, cs], AHN[0:8, g, :], "ATAB"))
                elif t == NLT:
                    srcs = [(XB[:, t, cs], ASAME[:, 3, g, :], ("XB", t)), (XB[:, t + 1, cs], ANEXT[:, g, :], ("XB", t + 1))]
                else:
                    srcs = [(XB[:, t, cs], ASAME[:, 4, g, :], ("XB", t)), (XB[:, t - 1, cs], APREV[:, g, :], ("XB", t - 1))]
                for si, (l_, r_, key) in enumerate(srcs):
                    add("pe", lambda e, b=b, q=q, l_=l_, r_=r_, si=si, ns=len(srcs): e.matmul(PS[:, b, q * 128:(q + 1) * 128], lhsT=l_, rhs=r_, start=(si == 0), stop=(si == ns - 1)),
                        r=[key, "ATAB"], w=[("PS", b)])
            v = 0 if t < NLT else 1
            for q in range(4):
                kc = half * 4 + q
                add("act", lambda e, t=t, kc=kc, b=b, q=q, v=v: e.activation(out=MT[:, kc, t * 128:(t + 1) * 128], in_=PS[:, b, q * 128:(q + 1) * 128],
                                                                              func=AF.Copy, scale=P.SCL[:, v, kc:kc + 1]), r=[("PS", b), "SCL"], w=[("MT", t)])
    for t in tiles:
        for g in range(4):
            for k in range(2):
                add("pe", lambda e, t=t, g=g, k=k: e.matmul(PS[:, 2 + g // 2, (g % 2) * 256:(g % 2 + 1) * 256], lhsT=MT[:, 2 * g + k, t * 128:(t + 1) * 128], rhs=WP[:, g, k, :],
                                                            start=(k == 0), stop=(k == 1)), r=[("MT", t), "WP"], w=[("PS", 2 + g // 2)])
        v = 0 if t < NLT else 1
        src = PS[:, 2:4, :].rearrange("p a b -> p (a b)")
        add("dve", lambda e, v=v, src=src, G=GBC, T_=TMP: e.tensor_tensor(out=T_[:, :], in0=src, in1=G[:, v, :], op=ALU.mult), r=[("PS", 2), ("PS", 3), ("GBC", v)], w=["TMPA"])
        add("dve", lambda e, T_=TMP, P_=PSC: e.tensor_tensor(out=T_[:, :], in0=T_[:, :], in1=P_[:, :], op=ALU.mult), r=["TMPA", "PSC"], w=["TMPA"])
        add("pool", lambda e, t=t, T_=TMP: e.tensor_tensor(out=X[:, t, :], in0=X[:, t, :], in1=T_[:, :], op=ALU.add), r=[("X", t), "TMPA"], w=[("X", t)])
    if stop >= 2:
        P.layer_norm(LNV, TMP, 0, tiles)
    P.phase()
    HT = P.vb("p (k c) -> p k c", n=8 * NTOK, k=8)
    bufs = P.ffn_bufs(2)
    GBC = P.vf("p (v c) -> p v c", n=2 * D, v=2)
    LNV = P.vf("p (v c) -> p v c", n=2 * D, v=2)
    GATES = P.vf("p (t e) -> p t e", n=NT * 8, t=NT)
    H32 = P.vf("p (k c) -> p k c", n=8 * 128, k=8)
    RT = P.vf("p (k e) -> p k e", n=64, k=8)
    LG = P.vf(n=8)
    L2 = P.vf(n=8)
    E1 = P.vf(n=8)
    E2 = P.vf(n=8)
    MS = P.vf(n=8)
    if stop >= 3:
        P.mod_cols(3, 4)
        P.load_gate(GBC, 1)
        add("sp", lambda e: e.dma_start(out=RT[:, :, :], in_=rt_in.ap().rearrange("(k p) e -> p k e", p=128)), w=["RT"], kind="d")
        for t in tiles:
            v = 0 if t < NLT else 1
            for half in range(2):
                b = half
                for q in range(4):
                    kc = half * 4 + q
                    add("pe", lambda e, t=t, kc=kc, b=b, q=q: e.transpose(PS[:, b, q * 128:(q + 1) * 128], X[:, t, kc * 128:(kc + 1) * 128], P.IDF[:, :]),
                        r=[("X", t), "IDF"], w=[("PS", b)])
                for q in range(4):
                    kc = half * 4 + q
                    add("act", lambda e, t=t, kc=kc, b=b, q=q, v=v: e.activation(out=HT[:, kc, t * 128:(t + 1) * 128], in_=PS[:, b, q * 128:(q + 1) * 128],
                                                                                  func=AF.Identity, bias=P.SHF[:, v, kc:kc + 1], scale=P.SCL[:, v, kc:kc + 1]),
                        r=[("PS", b), "SCL", "SHF"], w=[("HT", t)])
                    add("act", lambda e, kc=kc, b=b, q=q, v=v: e.activation(out=H32[:, kc, :], in_=PS[:, b, q * 128:(q + 1) * 128],
                                                                            func=AF.Identity, bias=P.SHF[:, v, kc:kc + 1], scale=P.SCL[:, v, kc:kc + 1]),
                        r=[("PS", b), "SCL", "SHF"], w=["H32"])
            for kc in range(8):
                add("pe", lambda e, kc=kc: e.matmul(PS[:, 6, 0:8], lhsT=H32[:, kc, :], rhs=RT[:, kc, :], start=(kc == 0), stop=(kc == 7)), r=["H32", "RT"], w=[("PS", 6)])
            add("dve", lambda e: e.tensor_copy(out=LG[:, :], in_=PS[:, 6, 0:8]), r=[("PS", 6)], w=["LG"])
            add("dve", lambda e: e.tensor_reduce(out=MS[:, 0:1], in_=LG[:, :], axis=AX.X, op=ALU.max), r=["LG"], w=["MS"])
            add("dve", lambda e: e.tensor_scalar(out=E1[:, :], in0=LG[:, :], scalar1=MS[:, 0:1], scalar2=None, op0=ALU.is_equal), r=["LG", "MS"], w=["E1"])
            add("dve", lambda e: e.scalar_tensor_tensor(out=L2[:, :], in0=E1[:, :], scalar=-1e30, in1=LG[:, :], op0=ALU.mult, op1=ALU.add), r=["E1", "LG"], w=["L2"])
            add("dve", lambda e: e.tensor_reduce(out=MS[:, 1:2], in_=L2[:, :], axis=AX.X, op=ALU.max), r=["L2"], w=["MS"])
            add("dve", lambda e: e.tensor_scalar(out=E2[:, :], in0=L2[:, :], scalar1=MS[:, 1:2], scalar2=None, op0=ALU.is_equal), r=["L2", "MS"], w=["E2"])
            add("dve", lambda e: e.tensor_tensor(out=MS[:, 2:3], in0=MS[:, 1:2], in1=MS[:, 0:1], op=ALU.subtract), r=["MS"], w=["MS"])
            add("act", lambda e: e.activation(out=MS[:, 3:4], in_=MS[:, 2:3], func=AF.Exp), r=["MS"], w=["MS"])
            add("dve", lambda e: e.tensor_scalar(out=MS[:, 4:5], in0=MS[:, 3:4], scalar1=1.0, scalar2=None, op0=ALU.add), r=["MS"], w=["MS"])
            add("dve", lambda e: e.reciprocal(out=MS[:, 5:6], in_=MS[:, 4:5]), r=["MS"], w=["MS"])
            add("dve", lambda e: e.tensor_tensor(out=MS[:, 6:7], in0=MS[:, 3:4], in1=MS[:, 5:6], op=ALU.mult), r=["MS"], w=["MS"])
            add("dve", lambda e: e.tensor_scalar(out=E1[:, :], in0=E1[:, :], scalar1=MS[:, 5:6], scalar2=None, op0=ALU.mult), r=["E1", "MS"], w=["E1"])
            add("dve", lambda e, t=t: e.scalar_tensor_tensor(out=GATES[:, t, :], in0=E2[:, :], scalar=MS[:, 6:7], in1=E1[:, :], op0=ALU.mult, op1=ALU.add),
                r=["E2", "E1", "MS"], w=["GATES"])
        cnt = 0
        for ex in (range(NEXP) if stop >= 4 else []):
            cnt = P.swiglu(bufs, HT, GBC, mg, mu, md, EXPD, tiles, e_off=ex, gates=lambda t, ex=ex: (GATES[:, t, ex:ex + 1], "GATES"), cnt0=cnt)
        P.layer_norm(LNV, bufs[5][0], 1, tiles, final=final, tkey=("TMPA", 0))
    P.store_stream(y_out)
    return P.finish()


_PROGS = {}


def _prog(key, fn):
    if key not in _PROGS:
        _PROGS[key] = fn()
    return _PROGS[key]


def _rope_perm():
    d = np.arange(32)
    dd = d % 16
    return np.where(dd < 8, d + 8, d - 8)


def _rope_tables(c):
    t = c * TPC + np.arange(TPC)
    row = (t // 64).astype(np.float32)
    col = (t % 64).astype(np.float32)
    inv = (1.0 / (np.float32(10000.0) ** (np.arange(8, dtype=np.float32) / np.float32(8)))).astype(np.float32)
    tab = np.zeros((32, 2, TPC), np.float32)
    for d in range(32):
        pos = row if d < 16 else col
        dd = d % 16
        ang = (pos * inv[dd % 8]).astype(np.float32)
        tab[d, 0] = np.cos(ang)
        tab[d, 1] = -np.sin(ang) if dd < 8 else np.sin(ang)
    return tab


def _na_bias_table(rel_bias):
    kr, kc = np.divmod(np.arange(128), 64)
    out = np.zeros((128, 7, 8, 128), np.float32)
    for s in range(7):
        drow = 2 * (s - 3) + kr[:, None] - kr[None, :] + 7
        dcol = kc[:, None] - kc[None, :] + 15
        ok = (drow >= 0) & (drow < 15) & (dcol >= 0) & (dcol < 31)
        g = rel_bias[:, np.clip(drow, 0, 14), np.clip(dcol, 0, 30)]
        out[:, s] = np.where(ok[None], g, 0.0).transpose(1, 0, 2)
    return out


def _na_mask(c):
    kr, kc = np.divmod(np.arange(128), 64)
    m = np.zeros((NLT, 128, 7, 128), np.float32)
    for T in range(NLT):
        qrow = 32 * c + 2 * T + kr
        rs = np.clip(qrow - 4, 0, 256 - 8)
        cs = np.clip(kc - 8, 0, 64 - 16)
        for s in range(7):
            ktg = 16 * c + T + s - 3
            if ktg < 0 or ktg >= S // 128:
                continue
            krow = 2 * ktg + kr
            ok = (krow[:, None] >= rs[None, :]) & (krow[:, None] < rs[None, :] + 8) & (kc[:, None] >= cs[None, :]) & (kc[:, None] < cs[None, :] + 16)
            m[T, :, s, :] = ok
    return m


def _common(inp, i, xs):
    maps = []
    for c in range(NCORE):
        m = {}
        m["x"] = xs[c]
        m["cvec"] = np.ascontiguousarray(np.stack([inp["c"][0], inp["c_ctx"]]).reshape(2, 8, 128).transpose(2, 0, 1))
        m["mod_w"] = np.ascontiguousarray(inp["mod_w"][i])
        m["mod_b"] = np.ascontiguousarray(inp["mod_b"][i][None])
        m["mod_bc"] = np.ascontiguousarray(inp["mod_b"][i].reshape(48, 128).T)
        m["ln"] = np.ascontiguousarray(np.stack([inp["ln1_g"][i], inp["ln1_b"][i], inp["ln2_g"][i], inp["ln2_b"][i], inp["pool_scale"][i // 2]]))
        m["ident"] = np.eye(128, dtype=np.float32)
        maps.append(m)
    return maps


def run_PA(inp, i, xs, first):
    j = i // 2
    nc = _prog(("PA", first), lambda: build_PA(first))
    maps = _common(inp, i, xs)
    perm = _rope_perm()
    w_in = np.ascontiguousarray(inp["attn_w_in"][j])
    wkr = np.zeros((D, 96), np.float32)
    wkr[:, 64:] = w_in[:, 1920:1952]
    wkrp = np.zeros((D, 96), np.float32)
    wkrp[:, 64:] = w_in[:, 1920:1952][:, perm]
    nrm = np.ascontiguousarray(np.stack([inp["mla_q_norm"][j][:128], inp["mla_q_norm"][j][128:], inp["mla_kv_norm"][j]], axis=1))
    for c, m in enumerate(maps):
        m.update(w_in=w_in, wkr=wkr, wkrp=wkrp, nrm=nrm, rope=_rope_tables(c))
    return run_bass_kernel_spmd(nc, maps, core_ids=list(range(NCORE))).results


def pb_maps(inp, i, xs, pa):
    j = i // 2
    maps = _common(inp, i, xs)
    perm = _rope_perm()
    wq = np.ascontiguousarray(inp["mla_w_q_up"][j])
    wq3 = wq.reshape(256, 8, 96)
    wqp = np.zeros((256, 8, 96), np.float32)
    wqp[:, :, 64:] = wq3[:, :, 64:][:, :, perm]
    nab = np.ascontiguousarray(_na_bias_table(inp["na_rel_bias"][j])[:, :, [0, 2, 4, 6, 1, 3, 5, 7], :])
    bf = ml_dtypes.bfloat16
    kvn_all = np.concatenate([pa[0]["kvn"][:, TPC:]] + [pa[c]["kvn"][:, :TPC] for c in range(NCORE)], axis=1)
    kr_all = np.concatenate([pa[0]["kr"][:, TPC:]] + [pa[c]["kr"][:, :TPC] for c in range(NCORE)], axis=1)
    for c, m in enumerate(maps):
        ka, va = pa[c]["ka"], pa[c]["va"]
        zk = np.zeros((128, 4, 384), bf)
        zv = np.zeros((128, 3, 640), bf)
        kprev = pa[c - 1]["ka"][:, :, TPC - 384:TPC] if c > 0 else zk
        knext = pa[c + 1]["ka"][:, :, 0:384] if c < NCORE - 1 else zk
        vprev = pa[c - 1]["va"][:, NLT - 3:NLT] if c > 0 else zv
        vnext = pa[c + 1]["va"][:, 0:3] if c < NCORE - 1 else zv
        m["kext"] = np.ascontiguousarray(np.concatenate([kprev, ka[:, :, :TPC], knext, ka[:, :, TPC:]], axis=2))
        m["vext"] = np.ascontiguousarray(np.concatenate([vprev, va[:, :NLT], vnext, va[:, NLT:]], axis=1))
        m["qa"] = pa[c]["qa"]
        m["qcn"] = pa[c]["qcn"]
        m["kvn_all"] = np.ascontiguousarray(kvn_all)
        m["kr_all"] = np.ascontiguousarray(kr_all)
        m["w_qup"] = wq
        m["w_qupp"] = np.ascontiguousarray(wqp.reshape(256, 768))
        m["w_kvup"] = np.ascontiguousarray(inp["mla_w_kv_up"][j])
        m["w_out"] = np.ascontiguousarray(inp["attn_w_out"][j])
        m["ffn_g"] = np.ascontiguousarray(inp["ffn_w_gate"][j])
        m["ffn_u"] = np.ascontiguousarray(inp["ffn_w_up"][j])
        m["ffn_d"] = np.ascontiguousarray(inp["ffn_w_down"][j])
        m["nabias"] = nab
        m["namask"] = _na_mask(c)
        m["rope"] = _rope_tables(c)
    return maps


def run_PB(inp, i, xs, pa, first, ctx_out):
    nc = _prog(("PB", first, ctx_out), lambda: build_PB(first, ctx_out))
    return run_bass_kernel_spmd(nc, pb_maps(inp, i, xs, pa), core_ids=list(range(NCORE))).results


def _pool_tables(c):
    halves = (1, 2, 4, 8)
    p = np.arange(128)
    same = np.zeros((128, 5, 4, 128), np.float32)
    prev = np.zeros((128, 4, 128), np.float32)
    nxt = np.zeros((128, 4, 128), np.float32)
    hp = np.zeros((8, 4, 128), np.float32)
    hn = np.zeros((8, 4, 128), np.float32)
    eye = np.eye(128, dtype=np.float32)
    for g, h in enumerate(halves):
        src, dst = p[:, None], p[None, :]
        inwin = (src >= dst - h) & (src < dst + h)
        gen = inwin / np.float32(2 * h) - eye
        cnt_first = (np.minimum(p + h, 128 + h) - np.maximum(p - h, 0)).astype(np.float32)
        first = inwin / cnt_first[None, :] - eye
        cnt_last = (np.minimum(p + h, 128) - (p - h)).astype(np.float32)
        last = inwin / cnt_last[None, :] - eye
        same[:, 0, g] = gen
        same[:, 1, g] = first if c == 0 else gen
        same[:, 2, g] = last if c == NCORE - 1 else gen
        same[:, 3, g] = first
        same[:, 4, g] = last
        prev[:, g] = ((src - 128) >= dst - h) / np.float32(2 * h)
        nxt[:, g] = ((src + 128) < dst + h) / np.float32(2 * h)
        j = np.arange(8)[:, None]
        hp[:, g] = ((j - 8) >= dst - h) / np.float32(2 * h)
        hn[:, g] = ((j + 128) < dst + h) / np.float32(2 * h)
    return dict(a_same=same, a_prev=prev, a_next=nxt, a_hp=hp, a_hn=hn)


def pc_maps(inp, i, xs):
    j = i // 2
    maps = _common(inp, i, xs)
    for c, m in enumerate(maps):
        halo = np.zeros((8, 2, D), np.float32)
        if c > 0:
            halo[:, 0] = xs[c - 1][TPC - 8:TPC]
        if c < NCORE - 1:
            halo[:, 1] = xs[c + 1][0:8]
        m["halo"] = halo
        m.update(_pool_tables(c))
        m["pool_w"] = np.ascontiguousarray(inp["pool_w"][j])
        m["router"] = np.ascontiguousarray(inp["moe_router"][j])
        m["moe_g"] = inp["moe_w_gate"][j].reshape(NEXP * D, EXPD)
        m["moe_u"] = inp["moe_w_up"][j].reshape(NEXP * D, EXPD)
        m["moe_d"] = inp["moe_w_down"][j].reshape(NEXP * EXPD, D)
    return maps


def run_PC(inp, i, xs, final, ctx_live):
    nc = _prog(("PC", final, ctx_live), lambda: build_PC(final, ctx_live))
    return run_bass_kernel_spmd(nc, pc_maps(inp, i, xs), core_ids=list(range(NCORE))).results


def kernel(**inp):
    inp = {k: np.asarray(v) for k, v in inp.items()}
    xs = [np.ascontiguousarray(np.concatenate([inp["x"][0, c * TPC:(c + 1) * TPC], inp["ctx"][0]], axis=0)) for c in range(NCORE)]
    pa = run_PA(inp, 0, xs, True)
    res = run_PB(inp, 0, xs, pa, True, True)
    xs = [res[c]["y"] for c in range(NCORE)]
    res = run_PC(inp, 1, xs, False, True)
    xs = [res[c]["y"] for c in range(NCORE)]
    pa = run_PA(inp, 2, xs, False)
    res = run_PB(inp, 2, xs, pa, False, False)
    xs = [res[c]["y"] for c in range(NCORE)]
    res = run_PC(inp, 3, xs, True, False)
    out = np.concatenate([res[c]["y"][:TPC] for c in range(NCORE)], axis=0)
    return out[None].astype(np.float32)
```

```python
import numpy as np
import ml_dtypes
from contextlib import ExitStack
import concourse.bass as bass
import concourse.mybir as mybir
from concourse.bass_utils import run_bass_kernel_spmd

F32 = mybir.dt.float32
BF16 = mybir.dt.bfloat16
AF = mybir.ActivationFunctionType
ALU = mybir.AluOpType
AX = mybir.AxisListType

NCORE = 8
D = 1024
S = 16384
TPC = S // NCORE
NLT = TPC // 128
NCT = 2
NT = NLT + NCT
NTOK = NT * 128
DEPTH = 4
ALPHA = (2 * DEPTH) ** 0.25
FFN = 2816
EXPD = 3584
NEXP = 8
LN_EPS = 1e-5
RMS_EPS = 1e-6
NKEY = 256 + S
NKT = NKEY // 128
SM_MLA = 96 ** -0.5
BLOCKS = [(0, 512), (512, 512), (1024, 512), (1536, 512), (2048, 256)]


class Sched:
    def __init__(self):
        self.ops = []
        self.lastw = {}
        self.readers = {}
        self.bar = set()

    def add(self, st, fn, r=(), w=(), kind="c"):
        i = len(self.ops)
        deps = set(self.bar)
        for k in r:
            if k in self.lastw:
                deps.add(self.lastw[k])
        for k in w:
            if k in self.lastw:
                deps.add(self.lastw[k])
            for j in self.readers.get(k, ()):
                deps.add(j)
        self.ops.append(dict(st=st, fn=fn, kind=kind, deps=deps, inc=False, sv=None))
        for k in r:
            self.readers.setdefault(k, []).append(i)
        for k in w:
            self.lastw[k] = i
            self.readers[k] = []
        return i

    def barrier(self):
        last = {}
        dl = {}
        for i, o in enumerate(self.ops):
            if o["kind"] == "c":
                last[o["st"]] = i
            else:
                dl.setdefault(o["st"], []).append(i)
        self.bar = set(last.values())
        for lst in dl.values():
            self.bar.update(lst[-8:])

    def emit(self, nc, es):
        ops = self.ops
        NP = 8
        streams = ["pe", "act", "dve", "pool", "sp"]
        dma_idx = {s: [] for s in streams}
        for i, o in enumerate(ops):
            if o["kind"] == "d":
                lst = dma_idx[o["st"]]
                if len(lst) >= NP:
                    o["deps"].add(lst[-NP])
                lst.append(i)
            best = {}
            keep = []
            for j in o["deps"]:
                p = ops[j]
                if p["kind"] == "c":
                    if p["st"] == "pe" and o["st"] == "pe" and o["kind"] == "c":
                        continue
                    if p["st"] not in best or best[p["st"]] < j:
                        best[p["st"]] = j
                else:
                    keep.append(j)
            for j in best.values():
                ops[j]["inc"] = True
                keep.append(j)
            o["deps"] = keep
        csem = {s: es.enter_context(nc.semaphore("c_" + s)) for s in streams}
        dsem = {s: [es.enter_context(nc.semaphore("d_%s%d" % (s, k))) for k in range(NP)] for s in ("pool", "sp")}
        cnt = {s: 0 for s in streams}
        dcnt = {s: 0 for s in streams}
        for o in ops:
            if o["kind"] == "c":
                if o["inc"]:
                    cnt[o["st"]] += 1
                    o["sv"] = (csem[o["st"]], cnt[o["st"]], 1)
            else:
                k = dcnt[o["st"]]
                dcnt[o["st"]] += 1
                o["sv"] = (dsem[o["st"]][k % NP], 16 * (k // NP + 1), 16)
        bystream = {s: [o for o in ops if o["st"] == s] for s in streams}

        def run(eng, st):
            lst = bystream[st]
            waited = {}
            for o in lst:
                for j in o["deps"]:
                    sem, val, _ = ops[j]["sv"]
                    key = id(sem)
                    if waited.get(key, 0) < val:
                        eng.wait_ge(sem, val)
                        waited[key] = val
                ins = o["fn"](eng)
                if o["sv"] is not None:
                    ins.then_inc(o["sv"][0], o["sv"][2])
            if st in dsem:
                k = dcnt[st]
                for q in range(NP):
                    n = (k - q + NP - 1) // NP if k > q else 0
                    if n > 0:
                        eng.wait_ge(dsem[st][q], 16 * n)

        with nc.Block() as block:
            @block.tensor
            def _(e):
                run(e, "pe")

            @block.scalar
            def _(e):
                run(e, "act")

            @block.vector
            def _(e):
                run(e, "dve")

            @block.gpsimd
            def _(e):
                run(e, "pool")

            @block.sync
            def _(e):
                run(e, "sp")


class Prog:
    def __init__(self, arb_elems, arf_elems):
        self.nc = nc = bass.Bass("TRN2", target_bir_lowering=False)
        self.es = es = ExitStack()
        self.S = Sched()
        self.add = self.S.add
        self.x_in = self.ext("x", [NTOK, D])
        self.cvec_in = self.ext("cvec", [128, 2, 8])
        self.modw_in = self.ext("mod_w", [D, 6 * D])
        self.modb_in = self.ext("mod_b", [1, 6 * D])
        self.modbc_in = self.ext("mod_bc", [128, 48])
        self.ln_in = self.ext("ln", [5, D])
        self.ident_in = self.ext("ident", [128, 128])
        self.grow = nc.dram_tensor("grow", [2, 2, D], F32)
        self.X = self.sb("X", [128, NT, D], F32)
        self.ARB = self.sb("ARB", [128, arb_elems], BF16)
        self.ARF = self.sb("ARF", [128, arf_elems], F32)
        self.MCOL = self.sb("MCOL", [128, 2, 48], F32)
        self.MBCOL = self.sb("MBCOL", [128, 48], F32)
        self.SC = self.sb("SC", [128, 2, 8], F32)
        self.SCB = self.sb("SCB", [128, 8, 2], BF16)
        self.IDF = self.sb("IDF", [128, 128], F32)
        self.IDB = self.sb("IDB", [128, 128], BF16)
        self.ONB = self.sb("ONB", [128, 128], BF16)
        self.ONF = self.sb("ONF", [128, 64], F32)
        self.STAT = self.sb("STAT", [128, 8], F32)
        self.SCL = self.sb("SCL", [128, 2, 8], F32)
        self.SHF = self.sb("SHF", [128, 2, 8], F32)
        self.PS = es.enter_context(nc.psum_tensor("PS", [128, 8, 512], F32))
        self.bo = 0
        self.fo = 0
        add = self.add
        add("sp", lambda e: e.dma_start(out=self.IDF[:, :], in_=self.ident_in[:, :]), w=["IDF"], kind="d")
        add("dve", lambda e: e.tensor_copy(out=self.IDB[:, :], in_=self.IDF[:, :]), r=["IDF"], w=["IDB"])
        add("dve", lambda e: e.memset(self.ONB[:, :], 1.0), w=["ONB"])
        add("dve", lambda e: e.memset(self.ONF[:, :], 1.0), w=["ONF"])

    def ext(self, name, shape, dt=F32):
        return self.nc.dram_tensor(name, list(shape), dt, kind="ExternalInput")

    def outp(self, name, shape, dt=F32):
        return self.nc.dram_tensor(name, list(shape), dt, kind="ExternalOutput")

    def sb(self, name, shape, dt):
        return self.es.enter_context(self.nc.sbuf_tensor(name, list(shape), dt))

    def phase(self):
        self.S.barrier()
        self.bo = 0
        self.fo = 0

    def vb(self, pattern=None, n=None, **kw):
        v = self.ARB[:, self.bo:self.bo + n]
        self.bo += n
        assert self.bo <= self.ARB.shape[1], ("ARB overflow", self.bo)
        return v.rearrange(pattern, **kw) if pattern else v

    def vf(self, pattern=None, n=None, **kw):
        v = self.ARF[:, self.fo:self.fo + n]
        self.fo += n
        assert self.fo <= self.ARF.shape[1], ("ARF overflow", self.fo)
        return v.rearrange(pattern, **kw) if pattern else v

    def load_stream(self, first):
        X, add = self.X, self.add
        for t in range(NT):
            add("sp", lambda e, t=t: e.dma_start(out=X[:, t, :], in_=self.x_in[t * 128:(t + 1) * 128, :]), w=[("X", t)], kind="d")
            if first:
                add("dve", lambda e, t=t: e.tensor_scalar(out=X[:, t, :], in0=X[:, t, :], scalar1=ALPHA, scalar2=None, op0=ALU.mult),
                    r=[("X", t)], w=[("X", t)])

    def store_stream(self, y_out):
        for t in range(NT):
            self.add("sp", lambda e, t=t: e.dma_start(out=y_out[t * 128:(t + 1) * 128, :], in_=self.X[:, t, :]), r=[("X", t)], kind="d")

    def modulation(self, mw=None, mb=None, mbc=None):
        add, PS = self.add, self.PS
        SC, SCB, MCOL, MBCOL = self.SC, self.SCB, self.MCOL, self.MBCOL
        self.phase()
        MW = self.vb("p (k c) -> p k c", n=8 * 512, k=8)
        SCBC = self.vb("p (v k m) -> p v k m", n=2 * 8 * 128, v=2, k=8)
        GR = self.vf(n=512)
        MBR = self.vf(n=2 * D)
        mw = self.modw_in if mw is None else mw
        mb = self.modb_in if mb is None else mb
        mbc = self.modbc_in if mbc is None else mbc
        add("sp", lambda e: e.dma_start(out=SC[:, :, :], in_=self.cvec_in[:, :, :]), w=["SC"], kind="d")
        add("act", lambda e: e.activation(out=SC[:, :, :], in_=SC[:, :, :], func=AF.Silu), r=["SC"], w=["SC"])
        add("dve", lambda e: e.tensor_copy(out=SCB[:, :, :], in_=SC[:, :, :].rearrange("p v k -> p k v")), r=["SC"], w=["SCB"])
        for v in range(2):
            add("dve", lambda e, v=v: e.tensor_copy(out=SCBC[:, v, :, :], in_=SC[:, v, :].unsqueeze(2).to_broadcast([128, 8, 128])),
                r=["SC"], w=[("SCBC", v)])
        add("sp", lambda e: e.dma_start(out=MBCOL[:, :], in_=mbc[:, :]), w=["MBCOL"], kind="d")
        for cb in range(12):
            add("pool", lambda e, cb=cb: e.dma_start(out=MW[:, :, :], in_=mw.ap().rearrange("(k p) c -> p k c", p=128)[:, :, cb * 512:(cb + 1) * 512]),
                w=["MW"], kind="d")
            for jj in range(4):
                for kc in range(8):
                    add("pe", lambda e, jj=jj, kc=kc: e.matmul(PS[:, 6, jj * 2:jj * 2 + 2], lhsT=MW[:, kc, jj * 128:(jj + 1) * 128], rhs=SCB[:, kc, :],
                                                             start=(kc == 0), stop=(kc == 7)),
                        r=["MW", "SCB"], w=[("PS", 6)])
            add("dve", lambda e, cb=cb: e.tensor_tensor(out=MCOL[:, :, cb * 4:(cb + 1) * 4].rearrange("p v j -> p j v"),
                                                       in0=PS[:, 6, 0:8].rearrange("p (j v) -> p j v", v=2),
                                                       in1=MBCOL[:, cb * 4:(cb + 1) * 4].unsqueeze(2).to_broadcast([128, 4, 2]), op=ALU.add),
                r=[("PS", 6), "MBCOL"], w=["MCOL"])
        for gi, which in enumerate((2, 5)):
            add("sp", lambda e, which=which: e.dma_start(out=MBR[0:1, 0:D], in_=mb[0:1, which * D:(which + 1) * D]), w=["MBR"], kind="d")
            for hf in range(2):
                add("pool", lambda e, which=which, hf=hf: e.dma_start(out=MW[:, :, :], in_=mw.ap().rearrange("(k p) c -> p k c", p=128)[:, :, which * D + hf * 512: which * D + (hf + 1) * 512]),
                    w=["MW"], kind="d")
                for v in range(2):
                    for kc in range(8):
                        add("pe", lambda e, v=v, kc=kc: e.matmul(PS[:, 5, :], lhsT=SCBC[:, v, kc, :], rhs=MW[:, kc, :], start=(kc == 0), stop=(kc == 7)),
                            r=["MW", ("SCBC", v)], w=[("PS", 5)])
                    add("dve", lambda e, hf=hf: e.tensor_tensor(out=GR[0:1, :], in0=PS[0:1, 5, :], in1=MBR[0:1, hf * 512:(hf + 1) * 512], op=ALU.add),
                        r=[("PS", 5), "MBR"], w=["GR"])
                    add("sp", lambda e, gi=gi, v=v, hf=hf: e.dma_start(out=self.grow[gi, v:v + 1, hf * 512:(hf + 1) * 512], in_=GR[0:1, :]),
                        r=["GR"], w=[("grow", gi)], kind="d")

    def load_gate(self, GBC, gi):
        for v in range(2):
            self.add("sp", lambda e, v=v: e.dma_start(out=GBC[:, v, :], in_=self.grow[gi, v:v + 1, :].to_broadcast([128, D])),
                     r=[("grow", gi)], w=[("GBC", v)], kind="d")

    def mod_cols(self, shift_idx, scale_idx):
        self.add("dve", lambda e: e.tensor_scalar(out=self.SCL[:, :, :], in0=self.MCOL[:, :, scale_idx * 8:(scale_idx + 1) * 8], scalar1=1.0, scalar2=1.0 / ALPHA,
                                                  op0=ALU.add, op1=ALU.mult), r=["MCOL"], w=["SCL"])
        self.add("dve", lambda e: e.tensor_copy(out=self.SHF[:, :, :], in_=self.MCOL[:, :, shift_idx * 8:(shift_idx + 1) * 8]), r=["MCOL"], w=["SHF"])

    def make_HT(self, HT, tiles, with_shift=True):
        add, PS, X = self.add, self.PS, self.X
        for t in tiles:
            v = 0 if t < NLT else 1
            for half in range(2):
                b = half
                for q in range(4):
                    kc = half * 4 + q
                    add("pe", lambda e, t=t, kc=kc, b=b, q=q: e.transpose(PS[:, b, q * 128:(q + 1) * 128], X[:, t, kc * 128:(kc + 1) * 128], self.IDF[:, :]),
                        r=[("X", t), "IDF"], w=[("PS", b)])
                for q in range(4):
                    kc = half * 4 + q
                    if with_shift:
                        add("act", lambda e, t=t, kc=kc, b=b, q=q, v=v: e.activation(out=HT[:, kc, t * 128:(t + 1) * 128], in_=PS[:, b, q * 128:(q + 1) * 128],
                                                                                      func=AF.Identity, bias=self.SHF[:, v, kc:kc + 1], scale=self.SCL[:, v, kc:kc + 1]),
                            r=[("PS", b), "SCL", "SHF"], w=[("HT", t)])
                    else:
                        add("act", lambda e, t=t, kc=kc, b=b, q=q, v=v: e.activation(out=HT[:, kc, t * 128:(t + 1) * 128], in_=PS[:, b, q * 128:(q + 1) * 128],
                                                                                      func=AF.Copy, scale=self.SCL[:, v, kc:kc + 1]),
                            r=[("PS", b), "SCL"], w=[("HT", t)])

    def layer_norm(self, LNV, TMP, which, tiles, final=False, tkey="TMPA"):
        add, X, STAT = self.add, self.X, self.STAT
        a = 1.0 if final else ALPHA
        row = which * 2
        add("sp", lambda e: e.dma_start(out=LNV[:, 0, :], in_=self.ln_in[row:row + 1, :].to_broadcast([128, D])), w=[("LNV", 0)], kind="d")
        add("sp", lambda e: e.dma_start(out=LNV[:, 1, :], in_=self.ln_in[row + 1:row + 2, :].to_broadcast([128, D])), w=[("LNV", 1)], kind="d")
        if a != 1.0:
            add("dve", lambda e: e.tensor_scalar(out=LNV[:, :, :], in0=LNV[:, :, :], scalar1=a, scalar2=None, op0=ALU.mult),
                r=[("LNV", 0), ("LNV", 1)], w=[("LNV", 0), ("LNV", 1)])
        for t in tiles:
            k = ("X", t)
            add("dve", lambda e, t=t: e.tensor_reduce(out=STAT[:, 0:1], in_=X[:, t, :], axis=AX.X, op=ALU.add), r=[k], w=["STAT"])
            add("dve", lambda e: e.tensor_scalar(out=STAT[:, 1:2], in0=STAT[:, 0:1], scalar1=-1.0 / D, scalar2=None, op0=ALU.mult), r=["STAT"], w=["STAT"])
            add("dve", lambda e, t=t: e.tensor_scalar(out=X[:, t, :], in0=X[:, t, :], scalar1=STAT[:, 1:2], scalar2=None, op0=ALU.add), r=[k, "STAT"], w=[k])
            add("dve", lambda e, t=t: e.tensor_tensor(out=TMP[:, :], in0=X[:, t, :], in1=X[:, t, :], op=ALU.mult), r=[k], w=[tkey])
            add("dve", lambda e: e.tensor_reduce(out=STAT[:, 2:3], in_=TMP[:, :], axis=AX.X, op=ALU.add), r=[tkey], w=["STAT"])
            add("dve", lambda e: e.tensor_scalar(out=STAT[:, 3:4], in0=STAT[:, 2:3], scalar1=1.0 / D, scalar2=LN_EPS, op0=ALU.mult, op1=ALU.add), r=["STAT"], w=["STAT"])
            add("act", lambda e: e.activation(out=STAT[:, 5:6], in_=STAT[:, 3:4], func=AF.Sqrt), r=["STAT"], w=["STAT"])
            add("dve", lambda e: e.reciprocal(out=STAT[:, 4:5], in_=STAT[:, 5:6]), r=["STAT"], w=["STAT"])
            add("dve", lambda e, t=t: e.scalar_tensor_tensor(out=X[:, t, :], in0=X[:, t, :], scalar=STAT[:, 4:5], in1=LNV[:, 0, :], op0=ALU.mult, op1=ALU.mult),
                r=[k, "STAT", ("LNV", 0)], w=[k])
            add("dve", lambda e, t=t: e.tensor_tensor(out=X[:, t, :], in0=X[:, t, :], in1=LNV[:, 1, :], op=ALU.add), r=[k, ("LNV", 1)], w=[k])

    def accum(self, GBC, TMP, t, b0, gate=None, tkey="TMPA"):
        v = 0 if t < NLT else 1
        PS, X = self.PS, self.X
        src = PS[:, b0:b0 + 2, :].rearrange("p a b -> p (a b)")
        if gate is None:
            self.add("dve", lambda e: e.tensor_tensor(out=TMP[:, :], in0=src, in1=GBC[:, v, :], op=ALU.mult),
                     r=[("PS", b0), ("PS", b0 + 1), ("GBC", v)], w=[tkey])
        else:
            gap, gkey = gate
            self.add("dve", lambda e: e.scalar_tensor_tensor(out=TMP[:, :], in0=src, scalar=gap, in1=GBC[:, v, :], op0=ALU.mult, op1=ALU.mult),
                     r=[("PS", b0), ("PS", b0 + 1), ("GBC", v), gkey], w=[tkey])
        self.add("pool", lambda e: e.tensor_tensor(out=X[:, t, :], in0=X[:, t, :], in1=TMP[:, :], op=ALU.add), r=[("X", t), tkey], w=[("X", t)])

    def swiglu(self, bufs, HT, GBC, wg, wu, wd, F, tiles, e_off=0, gates=None, cnt0=0):
        add, PS = self.add, self.PS
        WA, WB, WD, AT, SG, TMP = bufs
        nbuf = len(WA)
        nfb = (F + 511) // 512
        ntok = len(tiles) * 128
        t0 = tiles[0] * 128
        blocks = [(b0, min(512, ntok - b0)) for b0 in range(0, ntok, 512)]
        cnt = cnt0
        hkeys = [("HT", tt) for tt in tiles]
        for fb in range(nfb):
            f0 = fb * 512
            fw = min(512, F - f0)
            nfc = fw // 128
            par = cnt % nbuf
            cnt += 1
            add("pool", lambda e, f0=f0, fw=fw, par=par: e.dma_start(out=WA[par][:, :, 0:fw], in_=wg.ap()[e_off * D:(e_off + 1) * D, :].rearrange("(k p) c -> p k c", p=128)[:, :, f0:f0 + fw]),
                w=[("WA", par)], kind="d")
            add("pool", lambda e, f0=f0, fw=fw, par=par: e.dma_start(out=WB[par][:, :, 0:fw], in_=wu.ap()[e_off * D:(e_off + 1) * D, :].rearrange("(k p) c -> p k c", p=128)[:, :, f0:f0 + fw]),
                w=[("WB", par)], kind="d")
            add("pool", lambda e, f0=f0, nfc=nfc, par=par: e.dma_start(out=WD[par][:, 0:nfc, :], in_=wd.ap()[e_off * F + f0:e_off * F + f0 + nfc * 128, :].rearrange("(k p) c -> p k c", p=128)),
                w=[("WD", par)], kind="d")
            for bi, (b0, bw) in enumerate(blocks):
                ap_ = bi % 2
                for fc in range(nfc):
                    for kc in range(8):
                        add("pe", lambda e, fc=fc, kc=kc, par=par, b0=b0, bw=bw: e.matmul(PS[:, 4, 0:bw], lhsT=WA[par][:, kc, fc * 128:(fc + 1) * 128], rhs=HT[:, kc, t0 + b0:t0 + b0 + bw],
                                                                                           start=(kc == 0), stop=(kc == 7)),
                            r=[("WA", par)] + hkeys, w=[("PS", 4)])
                    for kc in range(8):
                        add("pe", lambda e, fc=fc, kc=kc, par=par, b0=b0, bw=bw: e.matmul(PS[:, 5, 0:bw], lhsT=WB[par][:, kc, fc * 128:(fc + 1) * 128], rhs=HT[:, kc, t0 + b0:t0 + b0 + bw],
                                                                                           start=(kc == 0), stop=(kc == 7)),
                            r=[("WB", par)] + hkeys, w=[("PS", 5)])
                    sp_ = fc % 2
                    add("act", lambda e, sp_=sp_, bw=bw: e.activation(out=SG[sp_][:, 0:bw], in_=PS[:, 4, 0:bw], func=AF.Silu), r=[("PS", 4)], w=[("SG", sp_)])
                    add("dve", lambda e, sp_=sp_, ap_=ap_, fc=fc, bw=bw: e.tensor_tensor(out=AT[ap_][:, fc, 0:bw], in0=PS[:, 5, 0:bw], in1=SG[sp_][:, 0:bw], op=ALU.mult),
                        r=[("PS", 5), ("SG", sp_)], w=[("AT", ap_)])
                for ti in range(bw // 128):
                    t = tiles[0] + b0 // 128 + ti
                    yp = ti % 2
                    for hf in range(2):
                        for fc in range(nfc):
                            add("pe", lambda e, ap_=ap_, fc=fc, ti=ti, par=par, hf=hf, yp=yp, nfc=nfc: e.matmul(PS[:, 2 * yp + hf, :], lhsT=AT[ap_][:, fc, ti * 128:(ti + 1) * 128], rhs=WD[par][:, fc, hf * 512:(hf + 1) * 512],
                                                                                                       start=(fc == 0), stop=(fc == nfc - 1)),
                                r=[("AT", ap_), ("WD", par)], w=[("PS", 2 * yp + hf)])
                    self.accum(GBC, TMP[yp], t, 2 * yp, gate=None if gates is None else gates(t), tkey=("TMPA", yp))
        return cnt

    def ffn_bufs(self, nbuf):
        WA = [self.vb("p (k c) -> p k c", n=8 * 512, k=8) for _ in range(nbuf)]
        WB = [self.vb("p (k c) -> p k c", n=8 * 512, k=8) for _ in range(nbuf)]
        WD = [self.vb("p (k c) -> p k c", n=4 * D, k=4) for _ in range(nbuf)]
        AT = [self.vb("p (k c) -> p k c", n=4 * 512, k=4) for _ in range(2)]
        SG = [self.vf(n=512) for _ in range(2)]
        TMP = [self.vf(n=D) for _ in range(2)]
        return WA, WB, WD, AT, SG, TMP

    def finish(self):
        self.S.emit(self.nc, self.es)
        self.es.close()
        return self.nc


def build_PA(first):
    P = Prog(44 * 1024, 6 * 1024)
    P.load_stream(first)
    P.modulation()
    pa_body(P)
    return P.finish()


def pa_body(P):
    add, PS = P.add, P.PS
    w_in = P.ext("w_in", [D, 1952])
    wkr_in = P.ext("wkr", [D, 96])
    wkrp_in = P.ext("wkrp", [D, 96])
    nrm_in = P.ext("nrm", [128, 3])
    rope_in = P.ext("rope", [32, 2, TPC])
    qa_o = P.outp("qa", [128, 4, NTOK], BF16)
    ka_o = P.outp("ka", [128, 4, NTOK], BF16)
    va_o = P.outp("va", [128, NT, 640], BF16)
    qcn_o = P.outp("qcn", [128, 2, NTOK], BF16)
    kvn_o = P.outp("kvn", [128, NTOK], BF16)
    kr_o = P.outp("kr", [32, NTOK], BF16)
    P.mod_cols(0, 1)
    P.phase()
    HT = P.vb("p (k c) -> p k c", n=8 * NTOK, k=8)
    WQ = P.vb("p (k c) -> p k c", n=8 * 512, k=8)
    WS = P.vb("p (k c) -> p k c", n=8 * 256, k=8)
    WKV = P.vb("p (k c) -> p k c", n=8 * 128, k=8)
    WKR = P.vb("p (k c) -> p k c", n=8 * 96, k=8)
    WKRP = P.vb("p (k c) -> p k c", n=8 * 96, k=8)
    OUTB = P.vb("p (k c) -> p k c", n=2 * NTOK, k=2)
    VAO = P.vb("p (t h c) -> p t h c", n=2 * 640, t=2, h=8)
    QCN = P.vb("p (k c) -> p k c", n=2 * NTOK, k=2)
    KVN = P.vb(n=NTOK)
    KR = P.vb(n=NTOK)
    SQ = P.vb("p (k c) -> p k c", n=2 * 512, k=2)
    NRM = P.vf(n=3)
    RB = P.vf(n=512)
    T1 = P.vf(n=512)
    T2 = P.vf(n=512)
    ROPE = P.vf("p (a c) -> p a c", n=2 * 512, a=2)
    P.make_HT(HT, list(range(NT)))
    hk = [("HT", t) for t in range(NT)]
    w3 = w_in.ap().rearrange("(k p) c -> p k c", p=128)
    add("sp", lambda e: e.dma_start(out=NRM[:, :], in_=nrm_in[:, :]), w=["NRM"], kind="d")
    add("pool", lambda e: e.dma_start(out=WS[:, :, :], in_=w3[:, :, 1536:1792]), w=["WS"], kind="d")
    add("pool", lambda e: e.dma_start(out=WKV[:, :, :], in_=w3[:, :, 1792:1920]), w=["WKV"], kind="d")
    add("pool", lambda e: e.dma_start(out=WKR[:, :, :], in_=wkr_in.ap().rearrange("(k p) c -> p k c", p=128)), w=["WKR"], kind="d")
    add("pool", lambda e: e.dma_start(out=WKRP[:, :, :], in_=wkrp_in.ap().rearrange("(k p) c -> p k c", p=128)), w=["WKRP"], kind="d")
    add("dve", lambda e: e.memset(VAO[:, :, :, :], 1.0), w=[("VAO", 0), ("VAO", 1)])
    for wi, dst in ((0, qa_o), (1, ka_o)):
        add("pool", lambda e, wi=wi: e.dma_start(out=WQ[:, :, :], in_=w3[:, :, wi * 512:(wi + 1) * 512]), w=["WQ"], kind="d")
        n = 0
        for jc in range(4):
            for (b0, bw) in BLOCKS:
                b = n % 2
                n += 1
                for kc in range(8):
                    add("pe", lambda e, jc=jc, kc=kc, b=b, b0=b0, bw=bw: e.matmul(PS[:, b, 0:bw], lhsT=WQ[:, kc, jc * 128:(jc + 1) * 128], rhs=HT[:, kc, b0:b0 + bw],
                                                                                   start=(kc == 0), stop=(kc == 7)), r=["WQ"] + hk, w=[("PS", b)])
                add("act", lambda e, jc=jc, b=b, b0=b0, bw=bw: e.activation(out=OUTB[:, jc % 2, b0:b0 + bw], in_=PS[:, b, 0:bw], func=AF.Copy),
                    r=[("PS", b)], w=[("OUTB", jc % 2)])
            add("sp", lambda e, dst=dst, jc=jc: e.dma_start(out=dst[:, jc, :], in_=OUTB[:, jc % 2, :]), r=[("OUTB", jc % 2)], kind="d")
    add("pool", lambda e: e.dma_start(out=WQ[:, :, :], in_=w3[:, :, 1024:1536]), w=["WQ"], kind="d")
    for t in range(NT):
        b = t % 2
        for kc in range(8):
            add("pe", lambda e, t=t, kc=kc, b=b: e.matmul(PS[:, b, :], lhsT=HT[:, kc, t * 128:(t + 1) * 128], rhs=WQ[:, kc, :], start=(kc == 0), stop=(kc == 7)),
                r=["WQ", ("HT", t)], w=[("PS", b)])
        add("dve", lambda e, t=t, b=b: e.tensor_copy(out=VAO[:, b, :, 0:64], in_=PS[:, b, :].rearrange("p (h c) -> p h c", h=8)), r=[("PS", b)], w=[("VAO", b)])
        add("sp", lambda e, t=t, b=b: e.dma_start(out=va_o[:, t, :], in_=VAO[:, b, :, :].rearrange("p h c -> p (h c)")), r=[("VAO", b)], kind="d")
    for (b0, bw) in BLOCKS:
        isctx = b0 >= TPC
        for ch in range(2):
            for kc in range(8):
                add("pe", lambda e, ch=ch, kc=kc, b0=b0, bw=bw: e.matmul(PS[:, 2 + ch, 0:bw], lhsT=WS[:, kc, ch * 128:(ch + 1) * 128], rhs=HT[:, kc, b0:b0 + bw],
                                                                          start=(kc == 0), stop=(kc == 7)), r=["WS"] + hk, w=[("PS", 2 + ch)])
            add("act", lambda e, ch=ch, bw=bw: e.activation(out=SQ[:, ch, 0:bw], in_=PS[:, 2 + ch, 0:bw], func=AF.Square), r=[("PS", 2 + ch)], w=[("SQ", ch)])
        for ch in range(2):
            add("pe", lambda e, ch=ch, bw=bw: e.matmul(PS[:, 4, 0:bw], lhsT=P.ONB[:, :], rhs=SQ[:, ch, 0:bw], start=(ch == 0), stop=(ch == 1)),
                r=["ONB", ("SQ", ch)], w=[("PS", 4)])
        add("dve", lambda e, bw=bw: e.tensor_scalar(out=RB[:, 0:bw], in0=PS[:, 4, 0:bw], scalar1=1.0 / 256, scalar2=RMS_EPS, op0=ALU.mult, op1=ALU.add), r=[("PS", 4)], w=["RB"])
        add("act", lambda e, bw=bw: e.activation(out=RB[:, 0:bw], in_=RB[:, 0:bw], func=AF.Sqrt), r=["RB"], w=["RB"])
        add("dve", lambda e, bw=bw: e.reciprocal(out=RB[:, 0:bw], in_=RB[:, 0:bw]), r=["RB"], w=["RB"])
        for ch in range(2):
            add("dve", lambda e, ch=ch, b0=b0, bw=bw: e.scalar_tensor_tensor(out=QCN[:, ch, b0:b0 + bw], in0=PS[:, 2 + ch, 0:bw], scalar=NRM[:, ch:ch + 1], in1=RB[:, 0:bw],
                                                                              op0=ALU.mult, op1=ALU.mult), r=[("PS", 2 + ch), "NRM", "RB"], w=["QCN"])
        for kc in range(8):
            add("pe", lambda e, kc=kc, b0=b0, bw=bw: e.matmul(PS[:, 5, 0:bw], lhsT=WKV[:, kc, :], rhs=HT[:, kc, b0:b0 + bw], start=(kc == 0), stop=(kc == 7)),
                r=["WKV"] + hk, w=[("PS", 5)])
        add("act", lambda e, bw=bw: e.activation(out=SQ[:, 0, 0:bw], in_=PS[:, 5, 0:bw], func=AF.Square), r=[("PS", 5)], w=[("SQ", 0)])
        add("pe", lambda e, bw=bw: e.matmul(PS[:, 4, 0:bw], lhsT=P.ONB[:, :], rhs=SQ[:, 0, 0:bw], start=True, stop=True), r=["ONB", ("SQ", 0)], w=[("PS", 4)])
        add("dve", lambda e, bw=bw: e.tensor_scalar(out=RB[:, 0:bw], in0=PS[:, 4, 0:bw], scalar1=1.0 / 128, scalar2=RMS_EPS, op0=ALU.mult, op1=ALU.add), r=[("PS", 4)], w=["RB"])
        add("act", lambda e, bw=bw: e.activation(out=RB[:, 0:bw], in_=RB[:, 0:bw], func=AF.Sqrt), r=["RB"], w=["RB"])
        add("dve", lambda e, bw=bw: e.reciprocal(out=RB[:, 0:bw], in_=RB[:, 0:bw]), r=["RB"], w=["RB"])
        add("dve", lambda e, b0=b0, bw=bw: e.scalar_tensor_tensor(out=KVN[:, b0:b0 + bw], in0=PS[:, 5, 0:bw], scalar=NRM[:, 2:3], in1=RB[:, 0:bw], op0=ALU.mult, op1=ALU.mult),
            r=[("PS", 5), "NRM", "RB"], w=["KVN"])
        for kc in range(8):
            add("pe", lambda e, kc=kc, b0=b0, bw=bw: e.matmul(PS[0:96, 6, 0:bw], lhsT=WKR[:, kc, :], rhs=HT[:, kc, b0:b0 + bw], start=(kc == 0), stop=(kc == 7)),
                r=["WKR"] + hk, w=[("PS", 6)])
        if isctx:
            add("act", lambda e, b0=b0, bw=bw: e.activation(out=KR[64:96, b0:b0 + bw], in_=PS[64:96, 6, 0:bw], func=AF.Copy), r=[("PS", 6)], w=["KR"])
        else:
            for kc in range(8):
                add("pe", lambda e, kc=kc, b0=b0, bw=bw: e.matmul(PS[0:96, 0, 0:bw], lhsT=WKRP[:, kc, :], rhs=HT[:, kc, b0:b0 + bw], start=(kc == 0), stop=(kc == 7)),
                    r=["WKRP"] + hk, w=[("PS", 0)])
            add("sp", lambda e, b0=b0, bw=bw: e.dma_start(out=ROPE[64:96, :, 0:bw], in_=rope_in[:, :, b0:b0 + bw]), w=["ROPE"], kind="d")
            add("dve", lambda e, bw=bw: e.tensor_tensor(out=T1[64:96, 0:bw], in0=PS[64:96, 6, 0:bw], in1=ROPE[64:96, 0, 0:bw], op=ALU.mult), r=[("PS", 6), "ROPE"], w=["T1"])
            add("dve", lambda e, bw=bw: e.tensor_tensor(out=T2[64:96, 0:bw], in0=PS[64:96, 0, 0:bw], in1=ROPE[64:96, 1, 0:bw], op=ALU.mult), r=[("PS", 0), "ROPE"], w=["T2"])
            add("dve", lambda e, b0=b0, bw=bw: e.tensor_tensor(out=KR[64:96, b0:b0 + bw], in0=T1[64:96, 0:bw], in1=T2[64:96, 0:bw], op=ALU.add), r=["T1", "T2"], w=["KR"])
    add("sp", lambda e: e.dma_start(out=qcn_o[:, :, :], in_=QCN[:, :, :]), r=["QCN"], kind="d")
    add("sp", lambda e: e.dma_start(out=kvn_o[:, :], in_=KVN[:, :]), r=["KVN"], kind="d")
    add("sp", lambda e: e.dma_start(out=kr_o[:, :], in_=KR[64:96, :]), r=["KR"], kind="d")


def build_PB(first, ctx_out, stop=9):
    P = Prog(46 * 1024, 9 * 1024)
    add, PS, X = P.add, P.PS, P.X
    qa_in = P.ext("qa", [128, 4, NTOK], BF16)
    kext_in = P.ext("kext", [128, 4, 22 * 128 + 256], BF16)
    vext_in = P.ext("vext", [128, 24, 640], BF16)
    qcn_in = P.ext("qcn", [128, 2, NTOK], BF16)
    kvn_in = P.ext("kvn_all", [128, NKEY], BF16)
    kr_in = P.ext("kr_all", [32, NKEY], BF16)
    wq_in = P.ext("w_qup", [256, 768])
    wqp_in = P.ext("w_qupp", [256, 768])
    wkv_in = P.ext("w_kvup", [128, 1024])
    wo_in = P.ext("w_out", [D, D])
    ffg = P.ext("ffn_g", [D, FFN])
    ffu = P.ext("ffn_u", [D, FFN])
    ffd = P.ext("ffn_d", [FFN, D])
    nab_in = P.ext("nabias", [128, 7, 8, 128])
    nam_in = P.ext("namask", [NLT, 128, 7, 128])
    rope_in = P.ext("rope", [32, 2, TPC])
    y_out = P.outp("y", [NTOK, D])
    P.load_stream(first)
    P.modulation()
    qtiles = list(range(NT)) if ctx_out else list(range(NLT))

    P.phase()
    QA = P.vb("p (k c) -> p k c", n=4 * NTOK, k=4)
    KW = P.vb("p (k c) -> p k c", n=4 * 896, k=4)
    VW = P.vb("p (s c) -> p s c", n=7 * 640, s=7)
    KAC = P.vb("p (k c) -> p k c", n=4 * 256, k=4)
    VAC = P.vb("p (s c) -> p s c", n=2 * 640, s=2)
    EB = [P.vb("p (h q) -> p h q", n=512, h=4) for _ in range(2)]
    PB_ = [P.vb("p (h q) -> p h q", n=512, h=4) for _ in range(2)]
    MIXT = P.vb("p (k q) -> p k q", n=512, k=4)
    WON = P.vb("p (k c) -> p k c", n=4 * D, k=4)
    NB = P.vb("p (s h q) -> p s h q", n=7 * 8 * 128, s=7, h=8)
    MASK = P.vf("p (s q) -> p s q", n=7 * 128, s=7)
    TS = [P.vf("p (h q) -> p h q", n=512, h=4) for _ in range(2)]
    TMP = P.vf(n=D)
    GBC = P.vf("p (v c) -> p v c", n=2 * D, v=2)
    RC = P.vf(n=8)
    OACC = P.vf("p (h c) -> p h c", n=8 * 66, h=8)
    MIX = P.vf("p (h c) -> p h c", n=512, h=8)
    P.load_gate(GBC, 0)
    add("sp", lambda e: e.dma_start(out=QA[:, :, :], in_=qa_in[:, :, :]), w=["QA"], kind="d")
    add("sp", lambda e: e.dma_start(out=KAC[:, :, :], in_=kext_in[:, :, 22 * 128:22 * 128 + 256]), w=["KAC"], kind="d")
    add("sp", lambda e: e.dma_start(out=VAC[:, :, :], in_=vext_in[:, 22:24, :]), w=["VAC"], kind="d")
    add("pool", lambda e: e.dma_start(out=NB[:, :, :, :], in_=nab_in[:, :, :, :]), w=["NB"], kind="d")
    for pos in range(8):
        hd = 2 * (pos % 4) + pos // 4
        add("pool", lambda e, pos=pos, hd=hd: e.dma_start(out=WON[(pos % 2) * 64:(pos % 2) * 64 + 64, pos // 2, :], in_=wo_in[hd * 64:(hd + 1) * 64, :]), w=["WON"], kind="d")
    gcount = 0
    import os
    _budget = [int(os.environ.get("NABUDGET", "100000000"))]
    _radd = P.S.add

    def add(st, fn, r=(), w=(), kind="c"):
        if _budget[0] <= 0:
            return None
        _budget[0] -= 1
        return _radd(st, fn, r=r, w=w, kind=kind)
    P.add = add
    for T in (qtiles if (stop >= 1 and not os.environ.get('SKIPATT')) else []):
        islat = T < NLT
        keytiles = []
        if islat:
            add("sp", lambda e, T=T: e.dma_start(out=KW[:, :, :], in_=kext_in[:, :, T * 128:(T + 7) * 128]), w=["KW"], kind="d")
            add("sp", lambda e, T=T: e.dma_start(out=VW[:, :, :], in_=vext_in[:, T:T + 7, :]), w=["VW"], kind="d")
            add("sp", lambda e, T=T: e.dma_start(out=MASK[:, :, :], in_=nam_in[T, :, :, :]), w=["MASK"], kind="d")
            keytiles = [("w", s) for s in range(7)]
        keytiles += [("c", 0), ("c", 1)]
        grps = []
        for ki, (kind, s_) in enumerate(keytiles):
            for g in range(2):
                grps.append((ki, kind, s_, g, gcount % 2))
                gcount += 1

        def na_S(grp, T=T):
            ki, kind, s, g, sb_ = grp
            for hh in range(4):
                ch, hp = hh, g * 64
                src, key = (KW, "KW") if kind == "w" else (KAC, "KAC")
                add("pe", lambda e, sb_=sb_, hh=hh, ch=ch, hp=hp, s=s, T=T, src=src: e.matmul(PS[:, sb_, hh * 128:(hh + 1) * 128], lhsT=src[hp:hp + 64, ch, s * 128:(s + 1) * 128],
                                                                                             rhs=QA[hp:hp + 64, ch, T * 128:(T + 1) * 128], start=True, stop=True),
                    r=[key, "QA"], w=[("PS", sb_)])

        def na_E(grp):
            ki, kind, s, g, sb_ = grp
            psv = PS[:, sb_, :].rearrange("p (h q) -> p h q", h=4)
            if kind == "w":
                add("dve", lambda e, sb_=sb_, psv=psv, s=s, g=g: e.scalar_tensor_tensor(out=TS[sb_][:, :, :], in0=psv, scalar=0.125, in1=NB[:, s, 4 * g:4 * g + 4, :],
                                                                                        op0=ALU.mult, op1=ALU.add), r=[("PS", sb_), "NB"], w=[("TS", sb_)])
                add("act", lambda e, sb_=sb_: e.activation(out=EB[sb_][:, :, :], in_=TS[sb_][:, :, :], func=AF.Exp), r=[("TS", sb_)], w=[("EB", sb_)])
                add("dve", lambda e, sb_=sb_, s=s: e.tensor_tensor(out=PB_[sb_][:, :, :], in0=EB[sb_][:, :, :], in1=MASK[:, s:s + 1, :].to_broadcast([128, 4, 128]), op=ALU.mult),
                    r=[("EB", sb_), "MASK"], w=[("PB", sb_)])
            else:
                add("act", lambda e, sb_=sb_, psv=psv: e.activation(out=PB_[sb_][:, :, :], in_=psv, func=AF.Exp, scale=0.125), r=[("PS", sb_)], w=[("PB", sb_)])

        def na_PV(grp):
            ki, kind, s, g, sb_ = grp
            for hh in range(4):
                h = 2 * hh + g
                src, key = (VW, "VW") if kind == "w" else (VAC, "VAC")
                add("pe", lambda e, sb_=sb_, hh=hh, h=h, s=s, src=src: e.matmul(PS[:, 2 + sb_, hh * 66:(hh + 1) * 66], lhsT=PB_[sb_][:, hh, :], rhs=src[:, s, h * 80:h * 80 + 66],
                                                                                   start=True, stop=True), r=[("PB", sb_), key], w=[("PS", 2 + sb_)])
            pov = PS[:, 2 + sb_, 0:264].rearrange("p (h c) -> p h c", h=4)
            if ki == 0:
                add("dve", lambda e, g=g, pov=pov: e.tensor_copy(out=OACC[:, 4 * g:4 * g + 4, :], in_=pov), r=[("PS", 2 + sb_)], w=[("OACC", g)])
            else:
                add("dve", lambda e, g=g, pov=pov: e.tensor_tensor(out=OACC[:, 4 * g:4 * g + 4, :], in0=OACC[:, 4 * g:4 * g + 4, :], in1=pov, op=ALU.add),
                    r=[("PS", 2 + sb_), ("OACC", g)], w=[("OACC", g)])

        na_S(grps[0])
        for n_, grp in enumerate(grps):
            na_E(grp)
            if n_ + 1 < len(grps):
                na_S(grps[n_ + 1])
            na_PV(grp)
        for g in range(2):
            ov = OACC[:, 4 * g:4 * g + 4, :]
            add("dve", lambda e, g=g, ov=ov: e.reciprocal(out=RC[:, 4 * g:4 * g + 4], in_=ov[:, :, 64]), r=[("OACC", g)], w=["RC"])
            add("dve", lambda e, g=g, ov=ov: e.tensor_tensor(out=MIX[:, 4 * g:4 * g + 4, :], in0=ov[:, :, 0:64], in1=RC[:, 4 * g:4 * g + 4].unsqueeze(2).to_broadcast([128, 4, 64]), op=ALU.mult),
                r=[("OACC", g), "RC"], w=["MIX"])
        for c4 in range(4):
            add("pe", lambda e, c4=c4: e.transpose(PS[:, 6, c4 * 128:(c4 + 1) * 128], MIX[:, 2 * c4:2 * c4 + 2, :].rearrange("p h c -> p (h c)"), P.IDF[:, :]),
                r=["MIX", "IDF"], w=[("PS", 6)])
        add("act", lambda e: e.activation(out=MIXT[:, :, :], in_=PS[:, 6, :].rearrange("p (k q) -> p k q", k=4), func=AF.Copy), r=[("PS", 6)], w=["MIXT"])
        for hf in range(2):
            for c4 in range(4):
                add("pe", lambda e, hf=hf, c4=c4: e.matmul(PS[:, 4 + hf, :], lhsT=MIXT[:, c4, :], rhs=WON[:, c4, hf * 512:(hf + 1) * 512], start=(c4 == 0), stop=(c4 == 3)),
                    r=["MIXT", "WON"], w=[("PS", 4 + hf)])
        P.accum(GBC, TMP, T, 4)

    add = _radd
    P.add = _radd
    P.phase()
    KT = P.vb(n=NKEY)
    VH = P.vb("p (t c) -> p t c", n=NKT * 80, t=NKT)
    QCN = P.vb("p (k c) -> p k c", n=2 * NTOK, k=2)
    QT = P.vb(n=NTOK)
    KVB = [P.vb(n=1024) for _ in range(2)]
    PT = [P.vb("p (j q) -> p j q", n=1024, j=2) for _ in range(2)]
    MXT = P.vb(n=512)
    WQ = P.vb("p (k c) -> p k c", n=2 * 768, k=2)
    WQP = P.vb("p (k c) -> p k c", n=2 * 768, k=2)
    WKV = P.vb(n=1024)
    WOH = P.vb(n=D)
    OSB = P.vf(n=512)
    RR = P.vf(n=512)
    T1 = P.vf(n=512)
    T2 = P.vf(n=512)
    TMP = P.vf(n=D)
    GBC = P.vf("p (v c) -> p v c", n=2 * D, v=2)
    ROPE = P.vf("p (a c) -> p a c", n=2 * TPC, a=2)
    P.load_gate(GBC, 0)
    add("sp", lambda e: e.dma_start(out=KT[64:96, :], in_=kr_in[:, :]), w=["KTr"], kind="d")
    add("sp", lambda e: e.dma_start(out=QCN[:, :, :], in_=qcn_in[:, :, :]), w=["QCN"], kind="d")
    add("sp", lambda e: e.dma_start(out=ROPE[64:96, :, :], in_=rope_in[:, :, :]), w=["ROPE"], kind="d")
    add("pool", lambda e: e.dma_start(out=WQ[:, :, :], in_=wq_in.ap().rearrange("(k p) c -> p k c", p=128)), w=["WQ"], kind="d")
    add("pool", lambda e: e.dma_start(out=WQP[:, :, :], in_=wqp_in.ap().rearrange("(k p) c -> p k c", p=128)), w=["WQP"], kind="d")
    add("pool", lambda e: e.dma_start(out=WKV[:, :], in_=wkv_in[:, :]), w=["WKV"], kind="d")
    add("dve", lambda e: e.memset(VH[:, :, :], 1.0), w=["VH"])
    kvblocks = [(k0, min(1024, NKEY - k0)) for k0 in range(0, NKEY, 1024)]
    qblocks = BLOCKS if ctx_out else BLOCKS[:4]
    nkv = 0
    for h in (range(8) if (stop >= 2 and not os.environ.get('SKIPATT')) else []):
        add("pool", lambda e, h=h: e.dma_start(out=WOH[0:64, :], in_=wo_in[512 + h * 64:512 + (h + 1) * 64, :]), w=["WOH"], kind="d")
        for (k0, kw) in kvblocks:
            kb = nkv % 2
            nkv += 1
            add("sp", lambda e, kb=kb, k0=k0, kw=kw: e.dma_start(out=KVB[kb][:, 0:kw], in_=kvn_in[:, k0:k0 + kw]), w=[("KVB", kb)], kind="d")
            for hf in range(0, kw, 512):
                w_ = min(512, kw - hf)
                b = 2 + (hf // 512)
                add("pe", lambda e, kb=kb, hf=hf, w_=w_, b=b, h=h: e.matmul(PS[0:64, b, 0:w_], lhsT=WKV[:, h * 128:h * 128 + 64], rhs=KVB[kb][:, hf:hf + w_], start=True, stop=True),
                    r=["WKV", ("KVB", kb)], w=[("PS", b)])
                add("dve", lambda e, k0=k0, hf=hf, w_=w_, b=b: e.tensor_copy(out=KT[0:64, k0 + hf:k0 + hf + w_], in_=PS[0:64, b, 0:w_]),
                    r=[("PS", b)], w=["KTn"])
            nt_ = kw // 128
            for ti in range(nt_):
                add("pe", lambda e, kb=kb, ti=ti, h=h: e.matmul(PS[:, 0, ti * 64:(ti + 1) * 64], lhsT=KVB[kb][:, ti * 128:(ti + 1) * 128], rhs=WKV[:, h * 128 + 64:h * 128 + 128],
                                                                start=True, stop=True), r=["WKV", ("KVB", kb)], w=[("PS", 0)])
            add("act", lambda e, k0=k0, nt_=nt_: e.activation(out=VH[:, k0 // 128:k0 // 128 + nt_, 0:64], in_=PS[:, 0, 0:nt_ * 64].rearrange("p (t c) -> p t c", t=nt_), func=AF.Copy),
                r=[("PS", 0)], w=["VH"])
        for (b0, bw) in qblocks:
            isctx = b0 >= TPC
            for kc in range(2):
                add("pe", lambda e, kc=kc, b0=b0, bw=bw, h=h: e.matmul(PS[0:96, 0, 0:bw], lhsT=WQ[:, kc, h * 96:(h + 1) * 96], rhs=QCN[:, kc, b0:b0 + bw], start=(kc == 0), stop=(kc == 1)),
                    r=["WQ", "QCN"], w=[("PS", 0)])
            add("act", lambda e, b0=b0, bw=bw: e.activation(out=QT[0:64, b0:b0 + bw], in_=PS[0:64, 0, 0:bw], func=AF.Copy), r=[("PS", 0)], w=["QT"])
            if isctx:
                add("act", lambda e, b0=b0, bw=bw: e.activation(out=QT[64:96, b0:b0 + bw], in_=PS[64:96, 0, 0:bw], func=AF.Copy), r=[("PS", 0)], w=["QT"])
            else:
                for kc in range(2):
                    add("pe", lambda e, kc=kc, b0=b0, bw=bw, h=h: e.matmul(PS[0:96, 1, 0:bw], lhsT=WQP[:, kc, h * 96:(h + 1) * 96], rhs=QCN[:, kc, b0:b0 + bw], start=(kc == 0), stop=(kc == 1)),
                        r=["WQP", "QCN"], w=[("PS", 1)])
                add("dve", lambda e, b0=b0, bw=bw: e.tensor_tensor(out=T1[64:96, 0:bw], in0=PS[64:96, 0, 0:bw], in1=ROPE[64:96, 0, b0:b0 + bw], op=ALU.mult), r=[("PS", 0), "ROPE"], w=["T1"])
                add("dve", lambda e, b0=b0, bw=bw: e.tensor_tensor(out=T2[64:96, 0:bw], in0=PS[64:96, 1, 0:bw], in1=ROPE[64:96, 1, b0:b0 + bw], op=ALU.mult), r=[("PS", 1), "ROPE"], w=["T2"])
                add("dve", lambda e, b0=b0, bw=bw: e.tensor_tensor(out=QT[64:96, b0:b0 + bw], in0=T1[64:96, 0:bw], in1=T2[64:96, 0:bw], op=ALU.add), r=["T1", "T2"], w=["QT"])
        pairs = [(kt, p_) for kt in range(NKT) for p_ in range(2)]

        def emit_S(n):
            kt, p_ = pairs[n]
            a = 2 * (n % 2)
            for j in range(2):
                qb = 2 * p_ + j
                add("pe", lambda e, a=a, j=j, kt=kt, qb=qb: e.matmul(PS[:, a + j, :], lhsT=KT[0:96, kt * 128:(kt + 1) * 128], rhs=QT[0:96, qb * 512:(qb + 1) * 512], start=True, stop=True),
                    r=["KTn", "KTr", "QT"], w=[("PS", a + j)])
        emit_S(0)
        for n, (kt, p_) in enumerate(pairs):
            a = 2 * (n % 2)
            pb = n % 2
            add("act", lambda e, a=a, pb=pb: e.activation(out=PT[pb][:, :, :], in_=PS[:, a:a + 2, :], func=AF.Exp, scale=SM_MLA),
                r=[("PS", a), ("PS", a + 1)], w=[("PT", pb)])
            if n + 1 < len(pairs):
                emit_S(n + 1)
            for j in range(2):
                qb = 2 * p_ + j
                add("pe", lambda e, pb=pb, j=j, kt=kt, qb=qb: e.matmul(PS[0:66, 4 + qb, :], lhsT=VH[:, kt, 0:66], rhs=PT[pb][:, j, :], start=(kt == 0), stop=(kt == NKT - 1)),
                    r=["VH", ("PT", pb)], w=[("PS", 4 + qb)])
        obanks = [(4 + qb, qb * 512, 512) for qb in range(4)]
        for (ob, q0, qw) in obanks + ([(2, TPC, 256)] if ctx_out else []):
            if ob == 2:
                for kt in range(2):
                    sb_ = kt
                    add("pe", lambda e, sb_=sb_, kt=kt: e.matmul(PS[:, sb_, 0:256], lhsT=KT[0:96, kt * 128:(kt + 1) * 128], rhs=QT[0:96, TPC:TPC + 256], start=True, stop=True),
                        r=["KTn", "KTr", "QT"], w=[("PS", sb_)])
                    add("act", lambda e, sb_=sb_: e.activation(out=PT[sb_][:, 0, 0:256], in_=PS[:, sb_, 0:256], func=AF.Exp, scale=SM_MLA), r=[("PS", sb_)], w=[("PT", sb_)])
                    add("pe", lambda e, sb_=sb_, kt=kt: e.matmul(PS[0:66, 2, 0:256], lhsT=VH[:, kt, 0:66], rhs=PT[sb_][:, 0, 0:256], start=(kt == 0), stop=(kt == 1)),
                        r=["VH", ("PT", sb_)], w=[("PS", 2)])
            add("act", lambda e, ob=ob, qw=qw: e.activation(out=OSB[0:64, 0:qw], in_=PS[0:64, ob, 0:qw], func=AF.Copy), r=[("PS", ob)], w=["OSB"])
            add("dve", lambda e, ob=ob, qw=qw: e.reciprocal(out=RR[64:65, 0:qw], in_=PS[64:65, ob, 0:qw]), r=[("PS", ob)], w=["RR"])
            add("pe", lambda e, ob=ob, qw=qw: e.matmul(PS[0:64, ob, 0:qw], lhsT=P.ONF[64:65, 0:64], rhs=RR[64:65, 0:qw], start=True, stop=True),
                r=["RR", "ONF", "OSB"], w=[("PS", ob)])
            add("dve", lambda e, ob=ob, qw=qw: e.tensor_tensor(out=MXT[0:64, 0:qw], in0=OSB[0:64, 0:qw], in1=PS[0:64, ob, 0:qw], op=ALU.mult), r=["OSB", ("PS", ob)], w=["MXT"])
            for ti in range(qw // 128):
                t = q0 // 128 + ti
                for hf in range(2):
                    add("pe", lambda e, ti=ti, hf=hf: e.matmul(PS[:, hf, :], lhsT=MXT[0:64, ti * 128:(ti + 1) * 128], rhs=WOH[0:64, hf * 512:(hf + 1) * 512], start=True, stop=True),
                        r=["MXT", "WOH"], w=[("PS", hf)])
                P.accum(GBC, TMP, t, 0)

    P.phase()
    HT = P.vb("p (k c) -> p k c", n=8 * NTOK, k=8)
    bufs = P.ffn_bufs(2)
    GBC = P.vf("p (v c) -> p v c", n=2 * D, v=2)
    LNV = P.vf("p (v c) -> p v c", n=2 * D, v=2)
    tiles = list(range(NT)) if ctx_out else list(range(NLT))
    if stop >= 3:
        P.layer_norm(LNV, bufs[5][0], 0, tiles, tkey=("TMPA", 0))
    if stop >= 4:
        P.mod_cols(3, 4)
        P.load_gate(GBC, 1)
        P.make_HT(HT, tiles)
        P.swiglu(bufs, HT, GBC, ffg, ffu, ffd, FFN, tiles)
        P.layer_norm(LNV, bufs[5][0], 1, tiles, tkey=("TMPA", 0))
    P.store_stream(y_out)
    return P.finish()


def build_PC(final, ctx_live, stop=9, with_pa=False):
    P = Prog(48 * 1024, 9 * 1024)
    add, PS, X = P.add, P.PS, P.X
    halo_in = P.ext("halo", [8, 2, D])
    asame_in = P.ext("a_same", [128, 5, 4, 128])
    aprev_in = P.ext("a_prev", [128, 4, 128])
    anext_in = P.ext("a_next", [128, 4, 128])
    ahp_in = P.ext("a_hp", [8, 4, 128])
    ahn_in = P.ext("a_hn", [8, 4, 128])
    pw_in = P.ext("pool_w", [4, 256, 256])
    rt_in = P.ext("router", [D, NEXP])
    mg = P.ext("moe_g", [NEXP * D, EXPD])
    mu = P.ext("moe_u", [NEXP * D, EXPD])
    md = P.ext("moe_d", [NEXP * EXPD, D])
    y_out = P.outp("y", [NTOK, D])
    P.load_stream(False)
    P.modulation()
    tiles = list(range(NT)) if ctx_live else list(range(NLT))
    P.phase()
    XB = P.vb("p (t c) -> p t c", n=NT * D, t=NT)
    MT = P.vb("p (k c) -> p k c", n=8 * NTOK, k=8)
    WP = P.vb("p (g k d) -> p g k d", n=4 * 2 * 256, g=4, k=2)
    ASAME = P.vb("p (a g q) -> p a g q", n=5 * 4 * 128, a=5, g=4)
    APREV = P.vb("p (g q) -> p g q", n=512, g=4)
    ANEXT = P.vb("p (g q) -> p g q", n=512, g=4)
    AHP = P.vb("p (g q) -> p g q", n=512, g=4)
    AHN = P.vb("p (g q) -> p g q", n=512, g=4)
    HALO = P.vb("p (a c) -> p a c", n=2 * D, a=2)
    GBC = P.vf("p (v c) -> p v c", n=2 * D, v=2)
    PSC = P.vf(n=D)
    TMP = P.vf(n=D)
    LNV = P.vf("p (v c) -> p v c", n=2 * D, v=2)
    P.mod_cols(0, 1)
    P.load_gate(GBC, 0)
    add("sp", lambda e: e.dma_start(out=PSC[:, :], in_=P.ln_in[4:5, :].to_broadcast([128, D])), w=["PSC"], kind="d")
    add("pool", lambda e: e.dma_start(out=ASAME[:, :, :, :], in_=asame_in[:, :, :, :]), w=["ATAB"], kind="d")
    add("pool", lambda e: e.dma_start(out=APREV[:, :, :], in_=aprev_in[:, :, :]), w=["ATAB"], kind="d")
    add("pool", lambda e: e.dma_start(out=ANEXT[:, :, :], in_=anext_in[:, :, :]), w=["ATAB"], kind="d")
    add("pool", lambda e: e.dma_start(out=AHP[0:8, :, :], in_=ahp_in[:, :, :]), w=["ATAB"], kind="d")
    add("pool", lambda e: e.dma_start(out=AHN[0:8, :, :], in_=ahn_in[:, :, :]), w=["ATAB"], kind="d")
    add("pool", lambda e: e.dma_start(out=HALO[0:8, :, :], in_=halo_in[:, :, :]), w=["ATAB"], kind="d")
    add("pool", lambda e: e.dma_start(out=WP[:, :, :, :], in_=pw_in.ap().rearrange("g (k p) d -> p g k d", p=128)), w=["WP"], kind="d")
    for t in tiles:
        add("act", lambda e, t=t: e.activation(out=XB[:, t, :], in_=X[:, t, :], func=AF.Copy), r=[("X", t)], w=[("XB", t)])
    nb = 0
    for t in tiles:
        for half in range(2):
            b = nb % 2
            nb += 1
            for q in range(4):
                kc = half * 4 + q
                g = kc // 2
                cs = slice(kc * 128, (kc + 1) * 128)
                if t < NLT:
                    cls = 1 if t == 0 else (2 if t == NLT - 1 else 0)
                    srcs = [(XB[:, t, cs], ASAME[:, cls, g, :], ("XB", t))]
                    srcs.append((XB[:, t - 1, cs], APREV[:, g, :], ("XB", t - 1)) if t > 0 else (HALO[0:8, 0, cs], AHP[0:8, g, :], "ATAB"))
                    srcs.append((XB[:, t + 1, cs], ANEXT[:, g, :], ("XB", t + 1)) if t < NLT - 1 else (HALO[0:8, 1, cs], AHN[0:8, g, :], "ATAB"))
                elif t == NLT:
                    srcs = [(XB[:, t, cs], ASAME[:, 3, g, :], ("XB", t)), (XB[:, t + 1, cs], ANEXT[:, g, :], ("XB", t + 1))]
                else:
                    srcs = [(XB[:, t, cs], ASAME[:, 4, g, :], ("XB", t)), (XB[:, t - 1, cs], APREV[:, g, :], ("XB", t - 1))]
                for si, (l_, r_, key) in enumerate(srcs):
                    add("pe", lambda e, b=b, q=q, l_=l_, r_=r_, si=si, ns=len(srcs): e.matmul(PS[:, b, q * 128:(q + 1) * 128], lhsT=l_, rhs=r_, start=(si == 0), stop=(si == ns - 1)),
                        r=[key, "ATAB"], w=[("PS", b)])
            v = 0 if t < NLT else 1
            for q in range(4):
                kc = half * 4 + q
                add("act", lambda e, t=t, kc=kc, b=b, q=q, v=v: e.activation(out=MT[:, kc, t * 128:(t + 1) * 128], in_=PS[:, b, q * 128:(q + 1) * 128],
                                                                              func=AF.Copy, scale=P.SCL[:, v, kc:kc + 1]), r=[("PS", b), "SCL"], w=[("MT", t)])
    for t in tiles:
        for g in range(4):
            for k in range(2):
                add("pe", lambda e, t=t, g=g, k=k: e.matmul(PS[:, 2 + g // 2, (g % 2) * 256:(g % 2 + 1) * 256], lhsT=MT[:, 2 * g + k, t * 128:(t + 1) * 128], rhs=WP[:, g, k, :],
                                                            start=(k == 0), stop=(k == 1)), r=[("MT", t), "WP"], w=[("PS", 2 + g // 2)])
        v = 0 if t < NLT else 1
        src = PS[:, 2:4, :].rearrange("p a b -> p (a b)")
        add("dve", lambda e, v=v, src=src, G=GBC, T_=TMP: e.tensor_tensor(out=T_[:, :], in0=src, in1=G[:, v, :], op=ALU.mult), r=[("PS", 2), ("PS", 3), ("GBC", v)], w=["TMPA"])
        add("dve", lambda e, T_=TMP, P_=PSC: e.tensor_tensor(out=T_[:, :], in0=T_[:, :], in1=P_[:, :], op=ALU.mult), r=["TMPA", "PSC"], w=["TMPA"])
        add("pool", lambda e, t=t, T_=TMP: e.tensor_tensor(out=X[:, t, :], in0=X[:, t, :], in1=T_[:, :], op=ALU.add), r=[("X", t), "TMPA"], w=[("X", t)])
    if stop >= 2:
        P.layer_norm(LNV, TMP, 0, tiles)
    P.phase()
    HT = P.vb("p (k c) -> p k c", n=8 * NTOK, k=8)
    bufs = P.ffn_bufs(2)
    GBC = P.vf("p (v c) -> p v c", n=2 * D, v=2)
    LNV = P.vf("p (v c) -> p v c", n=2 * D, v=2)
    GATES = P.vf("p (t e) -> p t e", n=NT * 8, t=NT)
    H32 = P.vf("p (k c) -> p k c", n=8 * 128, k=8)
    RT = P.vf("p (k e) -> p k e", n=64, k=8)
    LG = P.vf(n=8)
    L2 = P.vf(n=8)
    E1 = P.vf(n=8)
    E2 = P.vf(n=8)
    MS = P.vf(n=8)
    if stop >= 3:
        P.mod_cols(3, 4)
        P.load_gate(GBC, 1)
        add("sp", lambda e: e.dma_start(out=RT[:, :, :], in_=rt_in.ap().rearrange("(k p) e -> p k e", p=128)), w=["RT"], kind="d")
        for t in tiles:
            v = 0 if t < NLT else 1
            for half in range(2):
                b = half
                for q in range(4):
                    kc = half * 4 + q
                    add("pe", lambda e, t=t, kc=kc, b=b, q=q: e.transpose(PS[:, b, q * 128:(q + 1) * 128], X[:, t, kc * 128:(kc + 1) * 128], P.IDF[:, :]),
                        r=[("X", t), "IDF"], w=[("PS", b)])
                for q in range(4):
                    kc = half * 4 + q
                    add("act", lambda e, t=t, kc=kc, b=b, q=q, v=v: e.activation(out=HT[:, kc, t * 128:(t + 1) * 128], in_=PS[:, b, q * 128:(q + 1) * 128],
                                                                                  func=AF.Identity, bias=P.SHF[:, v, kc:kc + 1], scale=P.SCL[:, v, kc:kc + 1]),
                        r=[("PS", b), "SCL", "SHF"], w=[("HT", t)])
                    add("act", lambda e, kc=kc, b=b, q=q, v=v: e.activation(out=H32[:, kc, :], in_=PS[:, b, q * 128:(q + 1) * 128],
                                                                            func=AF.Identity, bias=P.SHF[:, v, kc:kc + 1], scale=P.SCL[:, v, kc:kc + 1]),
                        r=[("PS", b), "SCL", "SHF"], w=["H32"])
            for kc in range(8):
                add("pe", lambda e, kc=kc: e.matmul(PS[:, 6, 0:8], lhsT=H32[:, kc, :], rhs=RT[:, kc, :], start=(kc == 0), stop=(kc == 7)), r=["H32", "RT"], w=[("PS", 6)])
            add("dve", lambda e: e.tensor_copy(out=LG[:, :], in_=PS[:, 6, 0:8]), r=[("PS", 6)], w=["LG"])
            add("dve", lambda e: e.tensor_reduce(out=MS[:, 0:1], in_=LG[:, :], axis=AX.X, op=ALU.max), r=["LG"], w=["MS"])
            add("dve", lambda e: e.tensor_scalar(out=E1[:, :], in0=LG[:, :], scalar1=MS[:, 0:1], scalar2=None, op0=ALU.is_equal), r=["LG", "MS"], w=["E1"])
            add("dve", lambda e: e.scalar_tensor_tensor(out=L2[:, :], in0=E1[:, :], scalar=-1e30, in1=LG[:, :], op0=ALU.mult, op1=ALU.add), r=["E1", "LG"], w=["L2"])
            add("dve", lambda e: e.tensor_reduce(out=MS[:, 1:2], in_=L2[:, :], axis=AX.X, op=ALU.max), r=["L2"], w=["MS"])
            add("dve", lambda e: e.tensor_scalar(out=E2[:, :], in0=L2[:, :], scalar1=MS[:, 1:2], scalar2=None, op0=ALU.is_equal), r=["L2", "MS"], w=["E2"])
            add("dve", lambda e: e.tensor_tensor(out=MS[:, 2:3], in0=MS[:, 1:2], in1=MS[:, 0:1], op=ALU.subtract), r=["MS"], w=["MS"])
            add("act", lambda e: e.activation(out=MS[:, 3:4], in_=MS[:, 2:3], func=AF.Exp), r=["MS"], w=["MS"])
            add("dve", lambda e: e.tensor_scalar(out=MS[:, 4:5], in0=MS[:, 3:4], scalar1=1.0, scalar2=None, op0=ALU.add), r=["MS"], w=["MS"])
            add("dve", lambda e: e.reciprocal(out=MS[:, 5:6], in_=MS[:, 4:5]), r=["MS"], w=["MS"])
            add("dve", lambda e: e.tensor_tensor(out=MS[:, 6:7], in0=MS[:, 3:4], in1=MS[:, 5:6], op=ALU.mult), r=["MS"], w=["MS"])
            add("dve", lambda e: e.tensor_scalar(out=E1[:, :], in0=E1[:, :], scalar1=MS[:, 5:6], scalar2=None, op0=ALU.mult), r=["E1", "MS"], w=["E1"])
            add("dve", lambda e, t=t: e.scalar_tensor_tensor(out=GATES[:, t, :], in0=E2[:, :], scalar=MS[:, 6:7], in1=E1[:, :], op0=ALU.mult, op1=ALU.add),
                r=["E2", "E1", "MS"], w=["GATES"])
        cnt = 0
        for ex in (range(NEXP) if stop >= 4 else []):
            cnt = P.swiglu(bufs, HT, GBC, mg, mu, md, EXPD, tiles, e_off=ex, gates=lambda t, ex=ex: (GATES[:, t, ex:ex + 1], "GATES"), cnt0=cnt)
        P.layer_norm(LNV, bufs[5][0], 1, tiles, final=final, tkey=("TMPA", 0))
    P.store_stream(y_out)
    if with_pa:
        mw2 = P.ext("mod_w2", [D, 6 * D])
        mb2 = P.ext("mod_b2", [1, 6 * D])
        mbc2 = P.ext("mod_bc2", [128, 48])
        P.modulation(mw2, mb2, mbc2)
        pa_body(P)
    return P.finish()


_PROGS = {}


def _prog(key, fn):
    if key not in _PROGS:
        _PROGS[key] = fn()
    return _PROGS[key]


def _rope_perm():
    d = np.arange(32)
    dd = d % 16
    return np.where(dd < 8, d + 8, d - 8)


def _rope_tables(c):
    t = c * TPC + np.arange(TPC)
    row = (t // 64).astype(np.float32)
    col = (t % 64).astype(np.float32)
    inv = (1.0 / (np.float32(10000.0) ** (np.arange(8, dtype=np.float32) / np.float32(8)))).astype(np.float32)
    tab = np.zeros((32, 2, TPC), np.float32)
    for d in range(32):
        pos = row if d < 16 else col
        dd = d % 16
        ang = (pos * inv[dd % 8]).astype(np.float32)
        tab[d, 0] = np.cos(ang)
        tab[d, 1] = -np.sin(ang) if dd < 8 else np.sin(ang)
    return tab


def _na_bias_table(rel_bias):
    kr, kc = np.divmod(np.arange(128), 64)
    out = np.zeros((128, 7, 8, 128), np.float32)
    for s in range(7):
        drow = 2 * (s - 3) + kr[:, None] - kr[None, :] + 7
        dcol = kc[:, None] - kc[None, :] + 15
        ok = (drow >= 0) & (drow < 15) & (dcol >= 0) & (dcol < 31)
        g = rel_bias[:, np.clip(drow, 0, 14), np.clip(dcol, 0, 30)]
        out[:, s] = np.where(ok[None], g, 0.0).transpose(1, 0, 2)
    return out


def _na_mask(c):
    kr, kc = np.divmod(np.arange(128), 64)
    m = np.zeros((NLT, 128, 7, 128), np.float32)
    for T in range(NLT):
        qrow = 32 * c + 2 * T + kr
        rs = np.clip(qrow - 4, 0, 256 - 8)
        cs = np.clip(kc - 8, 0, 64 - 16)
        for s in range(7):
            ktg = 16 * c + T + s - 3
            if ktg < 0 or ktg >= S // 128:
                continue
            krow = 2 * ktg + kr
            ok = (krow[:, None] >= rs[None, :]) & (krow[:, None] < rs[None, :] + 8) & (kc[:, None] >= cs[None, :]) & (kc[:, None] < cs[None, :] + 16)
            m[T, :, s, :] = ok
    return m


def _common(inp, i, xs):
    maps = []
    for c in range(NCORE):
        m = {}
        m["x"] = xs[c]
        m["cvec"] = np.ascontiguousarray(np.stack([inp["c"][0], inp["c_ctx"]]).reshape(2, 8, 128).transpose(2, 0, 1))
        m["mod_w"] = np.ascontiguousarray(inp["mod_w"][i])
        m["mod_b"] = np.ascontiguousarray(inp["mod_b"][i][None])
        m["mod_bc"] = np.ascontiguousarray(inp["mod_b"][i].reshape(48, 128).T)
        m["ln"] = np.ascontiguousarray(np.stack([inp["ln1_g"][i], inp["ln1_b"][i], inp["ln2_g"][i], inp["ln2_b"][i], inp["pool_scale"][i // 2]]))
        m["ident"] = np.eye(128, dtype=np.float32)
        maps.append(m)
    return maps


def _pa_inputs(inp, j, maps):
    perm = _rope_perm()
    w_in = np.ascontiguousarray(inp["attn_w_in"][j])
    wkr = np.zeros((D, 96), np.float32)
    wkr[:, 64:] = w_in[:, 1920:1952]
    wkrp = np.zeros((D, 96), np.float32)
    wkrp[:, 64:] = w_in[:, 1920:1952][:, perm]
    nrm = np.ascontiguousarray(np.stack([inp["mla_q_norm"][j][:128], inp["mla_q_norm"][j][128:], inp["mla_kv_norm"][j]], axis=1))
    for c, m in enumerate(maps):
        m.update(w_in=w_in, wkr=wkr, wkrp=wkrp, nrm=nrm, rope=_rope_tables(c))


def run_PA(inp, i, xs, first):
    nc = _prog(("PA", first), lambda: build_PA(first))
    maps = _common(inp, i, xs)
    _pa_inputs(inp, i // 2, maps)
    return run_bass_kernel_spmd(nc, maps, core_ids=list(range(NCORE))).results


def pb_maps(inp, i, xs, pa):
    j = i // 2
    maps = _common(inp, i, xs)
    perm = _rope_perm()
    wq = np.ascontiguousarray(inp["mla_w_q_up"][j])
    wq3 = wq.reshape(256, 8, 96)
    wqp = np.zeros((256, 8, 96), np.float32)
    wqp[:, :, 64:] = wq3[:, :, 64:][:, :, perm]
    nab = np.ascontiguousarray(_na_bias_table(inp["na_rel_bias"][j])[:, :, [0, 2, 4, 6, 1, 3, 5, 7], :])
    bf = ml_dtypes.bfloat16
    kvn_all = np.concatenate([pa[0]["kvn"][:, TPC:]] + [pa[c]["kvn"][:, :TPC] for c in range(NCORE)], axis=1)
    kr_all = np.concatenate([pa[0]["kr"][:, TPC:]] + [pa[c]["kr"][:, :TPC] for c in range(NCORE)], axis=1)
    for c, m in enumerate(maps):
        ka, va = pa[c]["ka"], pa[c]["va"]
        zk = np.zeros((128, 4, 384), bf)
        zv = np.zeros((128, 3, 640), bf)
        kprev = pa[c - 1]["ka"][:, :, TPC - 384:TPC] if c > 0 else zk
        knext = pa[c + 1]["ka"][:, :, 0:384] if c < NCORE - 1 else zk
        vprev = pa[c - 1]["va"][:, NLT - 3:NLT] if c > 0 else zv
        vnext = pa[c + 1]["va"][:, 0:3] if c < NCORE - 1 else zv
        m["kext"] = np.ascontiguousarray(np.concatenate([kprev, ka[:, :, :TPC], knext, ka[:, :, TPC:]], axis=2))
        m["vext"] = np.ascontiguousarray(np.concatenate([vprev, va[:, :NLT], vnext, va[:, NLT:]], axis=1))
        m["qa"] = pa[c]["qa"]
        m["qcn"] = pa[c]["qcn"]
        m["kvn_all"] = np.ascontiguousarray(kvn_all)
        m["kr_all"] = np.ascontiguousarray(kr_all)
        m["w_qup"] = wq
        m["w_qupp"] = np.ascontiguousarray(wqp.reshape(256, 768))
        m["w_kvup"] = np.ascontiguousarray(inp["mla_w_kv_up"][j])
        m["w_out"] = np.ascontiguousarray(inp["attn_w_out"][j])
        m["ffn_g"] = np.ascontiguousarray(inp["ffn_w_gate"][j])
        m["ffn_u"] = np.ascontiguousarray(inp["ffn_w_up"][j])
        m["ffn_d"] = np.ascontiguousarray(inp["ffn_w_down"][j])
        m["nabias"] = nab
        m["namask"] = _na_mask(c)
        m["rope"] = _rope_tables(c)
    return maps


def run_PB(inp, i, xs, pa, first, ctx_out):
    nc = _prog(("PB", first, ctx_out), lambda: build_PB(first, ctx_out))
    return run_bass_kernel_spmd(nc, pb_maps(inp, i, xs, pa), core_ids=list(range(NCORE))).results


def _pool_tables(c):
    halves = (1, 2, 4, 8)
    p = np.arange(128)
    same = np.zeros((128, 5, 4, 128), np.float32)
    prev = np.zeros((128, 4, 128), np.float32)
    nxt = np.zeros((128, 4, 128), np.float32)
    hp = np.zeros((8, 4, 128), np.float32)
    hn = np.zeros((8, 4, 128), np.float32)
    eye = np.eye(128, dtype=np.float32)
    for g, h in enumerate(halves):
        src, dst = p[:, None], p[None, :]
        inwin = (src >= dst - h) & (src < dst + h)
        gen = inwin / np.float32(2 * h) - eye
        cnt_first = (np.minimum(p + h, 128 + h) - np.maximum(p - h, 0)).astype(np.float32)
        first = inwin / cnt_first[None, :] - eye
        cnt_last = (np.minimum(p + h, 128) - (p - h)).astype(np.float32)
        last = inwin / cnt_last[None, :] - eye
        same[:, 0, g] = gen
        same[:, 1, g] = first if c == 0 else gen
        same[:, 2, g] = last if c == NCORE - 1 else gen
        same[:, 3, g] = first
        same[:, 4, g] = last
        prev[:, g] = ((src - 128) >= dst - h) / np.float32(2 * h)
        nxt[:, g] = ((src + 128) < dst + h) / np.float32(2 * h)
        j = np.arange(8)[:, None]
        hp[:, g] = ((j - 8) >= dst - h) / np.float32(2 * h)
        hn[:, g] = ((j + 128) < dst + h) / np.float32(2 * h)
    return dict(a_same=same, a_prev=prev, a_next=nxt, a_hp=hp, a_hn=hn)


def pc_maps(inp, i, xs):
    j = i // 2
    maps = _common(inp, i, xs)
    for c, m in enumerate(maps):
        halo = np.zeros((8, 2, D), np.float32)
        if c > 0:
            halo[:, 0] = xs[c - 1][TPC - 8:TPC]
        if c < NCORE - 1:
            halo[:, 1] = xs[c + 1][0:8]
        m["halo"] = halo
        m.update(_pool_tables(c))
        m["pool_w"] = np.ascontiguousarray(inp["pool_w"][j])
        m["router"] = np.ascontiguousarray(inp["moe_router"][j])
        m["moe_g"] = inp["moe_w_gate"][j].reshape(NEXP * D, EXPD)
        m["moe_u"] = inp["moe_w_up"][j].reshape(NEXP * D, EXPD)
        m["moe_d"] = inp["moe_w_down"][j].reshape(NEXP * EXPD, D)
    return maps


def run_PC(inp, i, xs, final, ctx_live, with_pa=False):
    nc = _prog(("PC", final, ctx_live, with_pa), lambda: build_PC(final, ctx_live, with_pa=with_pa))
    maps = pc_maps(inp, i, xs)
    if with_pa:
        _pa_inputs(inp, (i + 1) // 2, maps)
        for m in maps:
            m["mod_w2"] = np.ascontiguousarray(inp["mod_w"][i + 1])
            m["mod_b2"] = np.ascontiguousarray(inp["mod_b"][i + 1][None])
            m["mod_bc2"] = np.ascontiguousarray(inp["mod_b"][i + 1].reshape(48, 128).T)
    return run_bass_kernel_spmd(nc, maps, core_ids=list(range(NCORE))).results


def kernel(**inp):
    inp = {k: np.asarray(v) for k, v in inp.items()}
    xs = [np.ascontiguousarray(np.concatenate([inp["x"][0, c * TPC:(c + 1) * TPC], inp["ctx"][0]], axis=0)) for c in range(NCORE)]
    pa = run_PA(inp, 0, xs, True)
    res = run_PB(inp, 0, xs, pa, True, True)
    xs = [res[c]["y"] for c in range(NCORE)]
    res = run_PC(inp, 1, xs, False, True, with_pa=True)
    xs = [res[c]["y"] for c in range(NCORE)]
    res = run_PB(inp, 2, xs, res, False, False)
    xs = [res[c]["y"] for c in range(NCORE)]
    res = run_PC(inp, 3, xs, True, False)
    out = np.concatenate([res[c]["y"][:TPC] for c in range(NCORE)], axis=0)
    return out[None].astype(np.float32)
```
